# Optimizing a Trainium2 kernel written in Bass

```python
import jax, jax.numpy as jnp
from jax import lax
import numpy as np

D_MODEL = 1024
BATCH = 16
SEQ = 2048
DEPTH = 2

CTX_LEN = 256
GRID_W = 64
ROPE_THETA = 10000.0
NORM_EPS = 1e-6
ATTN_QBLK = 128

MLA_HEADS = 8
MLA_Q_RANK = 384
MLA_KV_RANK = 256
MLA_NOPE = 64
MLA_ROPE = 32
MLA_V = 64
SWA_HEADS = 8
SWA_KV_HEADS = 2
SWA_GROUP = SWA_HEADS // SWA_KV_HEADS
SWA_HEAD_DIM = 64
SWA_WINDOW = 128
SWA_BLK = SWA_WINDOW
DN_HEADS = 8
DN_HEAD_DIM = 64
DN_CONV = 5
DN_CHUNK = 64
N_BRANCH = 3
BRANCH_W = MLA_HEADS * MLA_V
N_EXPERTS = 32
TOP_K = 4
D_EXPERT = 1024
SWIGLU_LIMIT = 7.0
SWIGLU_ALPHA = 1.702
MOE_BLK = 256

IN_SIZES = (
    MLA_Q_RANK,
    MLA_KV_RANK,
    MLA_ROPE,
    SWA_HEADS * SWA_HEAD_DIM,
    SWA_KV_HEADS * SWA_HEAD_DIM,
    SWA_KV_HEADS * SWA_HEAD_DIM,
    3 * DN_HEADS * DN_HEAD_DIM,
    DN_HEADS * DN_HEAD_DIM,
    2 * DN_HEADS,
    2 * DN_HEADS,
    N_BRANCH * D_MODEL,
)
D_IN = sum(IN_SIZES)

kernel_name = "hybrid_mla_swa_deltanet_moe_dit"


def _rmsnorm(x, g):
    xf = x.astype(jnp.float32)
    y = xf * lax.rsqrt(jnp.mean(xf * xf, axis=-1, keepdims=True) + NORM_EPS)
    return (y * g.astype(jnp.float32)).astype(x.dtype)


def _l2norm(x):
    xf = x.astype(jnp.float32)
    return (xf * lax.rsqrt(jnp.sum(xf * xf, axis=-1, keepdims=True) + NORM_EPS)).astype(x.dtype)


def _modulate(h, shift, scale):
    return h * (1 + scale) + shift


def _split(x, sizes):
    return jnp.split(x, np.cumsum(sizes)[:-1].tolist(), axis=-1)


def _axial_rope_tables(n_tok, rot_dim):
    rows = n_tok // GRID_W
    row = jnp.broadcast_to(jnp.arange(rows)[:, None], (rows, GRID_W)).reshape(-1).astype(jnp.float32)
    col = jnp.broadcast_to(jnp.arange(GRID_W)[None, :], (rows, GRID_W)).reshape(-1).astype(jnp.float32)
    n_freq = rot_dim // 4
    inv_freq = ROPE_THETA ** (-jnp.arange(n_freq, dtype=jnp.float32) / n_freq)
    ang = jnp.concatenate([row[:, None] * inv_freq, col[:, None] * inv_freq], axis=-1)
    return jnp.cos(ang), jnp.sin(ang)


def _apply_rope(x, cos, sin):
    xf = x.astype(jnp.float32)
    x1, x2 = jnp.split(xf, 2, axis=-1)
    cs, sn = cos[None, :, None, :], sin[None, :, None, :]
    return jnp.concatenate([x1 * cs - x2 * sn, x2 * cs + x1 * sn], axis=-1).astype(x.dtype)


def _dense_block_attention(q, k, v, scale):
    B, S, H, dk = q.shape
    nb = S // ATTN_QBLK
    qb = jnp.moveaxis(q.reshape(B, nb, ATTN_QBLK, H, dk), 1, 0)

    def one(qblk):
        s = jnp.einsum('bqhd,bthd->bhqt', qblk, k).astype(jnp.float32) * scale
        p = jax.nn.softmax(s, axis=-1).astype(v.dtype)
        return jnp.einsum('bhqt,bthe->bqhe', p, v)

    o = lax.map(one, qb)
    return jnp.moveaxis(o, 0, 1).reshape(B, S, H, v.shape[-1])


def _mla_branch(cq, ckv, kr, cq_c, ckv_c, kr_c, q_norm_g, w_q_up, kv_norm_g, w_kv_up, cos, sin, ctx_out):
    scale = (MLA_NOPE + MLA_ROPE) ** -0.5

    def queries(cqx):
        B, T = cqx.shape[:2]
        return (_rmsnorm(cqx, q_norm_g) @ w_q_up).reshape(B, T, MLA_HEADS, MLA_NOPE + MLA_ROPE)

    def keys_values(ckvx, krx):
        B, T = ckvx.shape[:2]
        kv = (_rmsnorm(ckvx, kv_norm_g) @ w_kv_up).reshape(B, T, MLA_HEADS, MLA_NOPE + MLA_V)
        k_nope, v = kv[..., :MLA_NOPE], kv[..., MLA_NOPE:]
        k_rope = jnp.broadcast_to(krx, (B, T, MLA_HEADS, MLA_ROPE))
        return jnp.concatenate([k_nope, k_rope], axis=-1), v

    B, S = cq.shape[:2]
    q = queries(cq)
    q = jnp.concatenate([q[..., :MLA_NOPE], _apply_rope(q[..., MLA_NOPE:], cos, sin)], axis=-1)
    k, v = keys_values(ckv, _apply_rope(kr[:, :, None, :], cos, sin))
    kc, vc = keys_values(ckv_c, kr_c[:, :, None, :])
    o = _dense_block_attention(q, jnp.concatenate([kc, k], axis=1), jnp.concatenate([vc, v], axis=1), scale)
    o = o.reshape(B, S, BRANCH_W)
    if not ctx_out:
        return o, None
    oc = _dense_block_attention(queries(cq_c), kc, vc, scale)
    return o, oc.reshape(cq_c.shape[0], cq_c.shape[1], BRANCH_W)


def _sink_attend(q, segs, sink, scale):
    scores = []
    for k, _, m in segs:
        s = jnp.einsum('bqngd,btnd->bngqt', q, k).astype(jnp.float32) * scale
        if m is not None:
            s = jnp.where(m, s, -jnp.inf)
        scores.append(s)
    B, N, G, Q = scores[0].shape[:4]
    sink_col = jnp.broadcast_to(sink[None, :, :, None, None], (B, N, G, Q, 1))
    p = jax.nn.softmax(jnp.concatenate(scores + [sink_col], axis=-1), axis=-1)
    out = None
    off = 0
    for (k, v, _), s in zip(segs, scores):
        t = s.shape[-1]
        o = jnp.einsum('bngqt,btnd->bqngd', p[..., off:off + t].astype(v.dtype), v)
        out = o if out is None else out + o
        off += t
    return out


def _swa_branch(sq, sk, sv, sq_c, sk_c, sv_c, sink, cos, sin, ctx_out):
    B, S = sq.shape[:2]
    T = sq_c.shape[1]
    dh = SWA_HEAD_DIM
    scale = dh ** -0.5
    sink_g = sink.astype(jnp.float32).reshape(SWA_KV_HEADS, SWA_GROUP)
    q = _apply_rope(sq.reshape(B, S, SWA_HEADS, dh), cos, sin).reshape(B, S, SWA_KV_HEADS, SWA_GROUP, dh)
    k = _apply_rope(sk.reshape(B, S, SWA_KV_HEADS, dh), cos, sin)
    v = sv.reshape(B, S, SWA_KV_HEADS, dh)
    kc = sk_c.reshape(B, T, SWA_KV_HEADS, dh)
    vc = sv_c.reshape(B, T, SWA_KV_HEADS, dh)
    pad = ((0, 0), (SWA_WINDOW, SWA_WINDOW), (0, 0), (0, 0))
    kpad, vpad = jnp.pad(k, pad), jnp.pad(v, pad)
    rel = (jnp.arange(3 * SWA_BLK) - SWA_BLK)[None, :] - jnp.arange(SWA_BLK)[:, None]
    band = jnp.abs(rel) <= SWA_WINDOW

    def block(i):
        start = i * SWA_BLK
        qb = lax.dynamic_slice_in_dim(q, start, SWA_BLK, 1)
        kb = lax.dynamic_slice_in_dim(kpad, start, 3 * SWA_BLK, 1)
        vb = lax.dynamic_slice_in_dim(vpad, start, 3 * SWA_BLK, 1)
        kpos = start - SWA_BLK + jnp.arange(3 * SWA_BLK)
        mask = band & ((kpos >= 0) & (kpos < S))[None, :]
        return _sink_attend(qb, [(kc, vc, None), (kb, vb, mask)], sink_g, scale)

    o = lax.map(block, jnp.arange(S // SWA_BLK))
    o = jnp.moveaxis(o, 0, 1).reshape(B, S, BRANCH_W)
    if not ctx_out:
        return o, None
    qc = sq_c.reshape(B, T, SWA_KV_HEADS, SWA_GROUP, dh)
    oc = _sink_attend(qc, [(kc, vc, None)], sink_g, scale).reshape(B, T, BRANCH_W)
    return o, oc


def _short_conv(u, w):
    C = u.shape[-1]
    y = lax.conv_general_dilated(u, w[:, None, :].astype(u.dtype), window_strides=(1,),
                                 padding=[(DN_CONV // 2, DN_CONV // 2)],
                                 dimension_numbers=('NWC', 'WIO', 'NWC'), feature_group_count=C)
    return jax.nn.silu(y)


def _gated_delta_chunked(q, k, v, g, beta, s0, with_output):
    out_dtype = v.dtype
    B, T, H, dk = q.shape
    dv = v.shape[-1]
    C = DN_CHUNK
    n = T // C

    def chunks(x):
        x = x.astype(jnp.float32).reshape((B, n, C, H) + x.shape[3:])
        return jnp.moveaxis(x, 3, 2)

    q, k, v, g, beta = chunks(q), chunks(k), chunks(v), chunks(g), chunks(beta)
    gcum = jnp.cumsum(g, axis=-1)
    causal = jnp.tril(jnp.ones((C, C), bool))
    strict = jnp.tril(jnp.ones((C, C), bool), -1)
    diff = gcum[..., :, None] - gcum[..., None, :]
    decay = jnp.where(causal, jnp.exp(jnp.where(causal, diff, 0.0)), 0.0)
    kb = k * beta[..., None]
    lmat = jnp.where(strict, jnp.einsum('bnhid,bnhjd->bnhij', kb, k) * decay, 0.0)
    amat = lmat + jnp.eye(C, dtype=jnp.float32)
    rhs = jnp.concatenate([v * beta[..., None], kb * jnp.exp(gcum)[..., None]], axis=-1)
    sol = lax.linalg.triangular_solve(amat, rhs, left_side=True, lower=True, unit_diagonal=True)
    u, w = sol[..., :dv], sol[..., dv:]
    k_dec = k * jnp.exp(gcum[..., -1:] - gcum)[..., None]
    g_last = jnp.exp(gcum[..., -1])
    to_scan = lambda x: jnp.moveaxis(x, 1, 0)

    def state_update(S, w_i, u_i, kd_i, gl_i):
        v_new = u_i - jnp.einsum('bhck,bhkv->bhcv', w_i, S)
        S_next = S * gl_i[..., None, None] + jnp.einsum('bhck,bhcv->bhkv', kd_i, v_new)
        return S_next, v_new

    if not with_output:
        def step_state(S, xs):
            S_next, _ = state_update(S, *xs)
            return S_next, None
        s_fin, _ = lax.scan(step_state, s0, tuple(map(to_scan, (w, u, k_dec, g_last))))
        return s_fin, None

    a_intra = jnp.einsum('bnhid,bnhjd->bnhij', q, k) * decay
    q_dec = q * jnp.exp(gcum)[..., None]

    def step(S, xs):
        w_i, u_i, kd_i, gl_i, qd_i, a_i = xs
        S_next, v_new = state_update(S, w_i, u_i, kd_i, gl_i)
        o = jnp.einsum('bhck,bhkv->bhcv', qd_i, S) + jnp.einsum('bhij,bhjv->bhiv', a_i, v_new)
        return S_next, o

    s_fin, o = lax.scan(step, s0, tuple(map(to_scan, (w, u, k_dec, g_last, q_dec, a_intra))))
    o = jnp.moveaxis(jnp.moveaxis(o, 0, 1), 2, 3).reshape(B, T, H, dv).astype(out_dtype)
    return s_fin, o


def _deltanet_branch(dqkv, dz, dbeta, da, dqkv_c, dz_c, dbeta_c, da_c, conv_w, a_log, dt_bias, norm_g, ctx_out):
    H, dk = DN_HEADS, DN_HEAD_DIM

    def heads(u):
        B, T = u.shape[:2]
        q, k, v = jnp.split(_short_conv(u, conv_w), 3, axis=-1)
        q = _l2norm(q.reshape(B, T, H, dk)) * (dk ** -0.5)
        k = _l2norm(k.reshape(B, T, H, dk))
        return q, k, v.reshape(B, T, H, dk)

    def gates(braw, araw, d):
        beta = jax.nn.sigmoid(braw[..., d * H:(d + 1) * H].astype(jnp.float32))
        g = -jnp.exp(a_log[d].astype(jnp.float32)) * jax.nn.softplus(
            araw[..., d * H:(d + 1) * H].astype(jnp.float32) + dt_bias[d].astype(jnp.float32))
        return g, beta

    def out_gate(o, z):
        B, T = o.shape[:2]
        return (_rmsnorm(o, norm_g) * jax.nn.silu(z.reshape(B, T, H, dk))).reshape(B, T, H * dk)

    q, k, v = heads(dqkv)
    qc, kc, vc = heads(dqkv_c)
    s0 = jnp.zeros((q.shape[0], H, dk, dk), jnp.float32)
    o, oc = None, None
    for d in range(2):
        g, beta = gates(dbeta, da, d)
        g_c, beta_c = gates(dbeta_c, da_c, d)
        seq, cseq = (q, k, v, g, beta), (qc, kc, vc, g_c, beta_c)
        if d == 1:
            seq = tuple(jnp.flip(t, axis=1) for t in seq)
            cseq = tuple(jnp.flip(t, axis=1) for t in cseq)
        s_ctx, oc_d = _gated_delta_chunked(*cseq, s0, with_output=ctx_out)
        _, o_d = _gated_delta_chunked(*seq, s_ctx, with_output=True)
        if d == 1:
            o_d = jnp.flip(o_d, axis=1)
            oc_d = jnp.flip(oc_d, axis=1) if ctx_out else None
        o = o_d if o is None else o + o_d
        if ctx_out:
            oc = oc_d if oc is None else oc + oc_d
    o = out_gate(o, dz)
    return o, (out_gate(oc, dz_c) if ctx_out else None)


def _merge(branches, gate_raw, w_branch, w_out):
    gate_parts = jnp.split(gate_raw, N_BRANCH, axis=-1)
    y = None
    for i in range(N_BRANCH):
        yi = jax.nn.sigmoid(gate_parts[i]) * (branches[i] @ w_branch[i])
        y = yi if y is None else y + yi
    return y @ w_out


def _hybrid_mixer(h, hc, w_in, mla_q_norm_g, mla_w_q_up, mla_kv_norm_g, mla_w_kv_up, swa_sink,
                  dn_conv_w, dn_a_log, dn_dt_bias, dn_norm_g, w_branch, w_out, ctx_out):
    S = h.shape[1]
    cos_m, sin_m = _axial_rope_tables(S, MLA_ROPE)
    cos_s, sin_s = _axial_rope_tables(S, SWA_HEAD_DIM)
    cq, ckv, kr, sq, sk, sv, dqkv, dz, dbeta, da, gate_raw = _split(h @ w_in, IN_SIZES)
    cq_c, ckv_c, kr_c, sq_c, sk_c, sv_c, dqkv_c, dz_c, dbeta_c, da_c, gate_raw_c = _split(hc @ w_in, IN_SIZES)
    o_a, oc_a = _mla_branch(cq, ckv, kr, cq_c, ckv_c, kr_c, mla_q_norm_g, mla_w_q_up, mla_kv_norm_g,
                            mla_w_kv_up, cos_m, sin_m, ctx_out)
    o_b, oc_b = _swa_branch(sq, sk, sv, sq_c, sk_c, sv_c, swa_sink, cos_s, sin_s, ctx_out)
    o_c, oc_c = _deltanet_branch(dqkv, dz, dbeta, da, dqkv_c, dz_c, dbeta_c, da_c, dn_conv_w, dn_a_log,
                                 dn_dt_bias, dn_norm_g, ctx_out)
    y = _merge((o_a, o_b, o_c), gate_raw, w_branch, w_out)
    yc = _merge((oc_a, oc_b, oc_c), gate_raw_c, w_branch, w_out) if ctx_out else None
    return y, yc


def _moe_ffn(h, router_w, router_b, w_gu, b_gu, w_dn, b_dn):
    N, D = h.shape
    logits = (h @ router_w).astype(jnp.float32) + router_b.astype(jnp.float32)
    top_v, top_e = lax.top_k(logits, TOP_K)
    gate = jax.nn.softmax(top_v, axis=-1)
    A = N * TOP_K
    flat_e = top_e.reshape(A)
    order = jnp.argsort(flat_e)
    e_sorted = flat_e[order]
    tok_sorted = (order // TOP_K).astype(jnp.int32)
    gate_sorted = gate.reshape(A)[order]
    counts = jnp.zeros((N_EXPERTS,), jnp.int32).at[flat_e].add(1)
    padded = (counts + MOE_BLK - 1) // MOE_BLK * MOE_BLK
    pad_end = jnp.cumsum(padded)
    pad_start = pad_end - padded
    cnt_start = jnp.cumsum(counts) - counts
    slot = pad_start[e_sorted] + jnp.arange(A, dtype=jnp.int32) - cnt_start[e_sorted]
    n_blocks = -(-A // MOE_BLK) + N_EXPERTS
    P = n_blocks * MOE_BLK
    slot_tok = jnp.full((P,), N, jnp.int32).at[slot].set(tok_sorted)
    slot_gate = jnp.zeros((P,), jnp.float32).at[slot].set(gate_sorted)
    block_e = jnp.minimum(jnp.searchsorted(pad_end, jnp.arange(n_blocks, dtype=jnp.int32) * MOE_BLK, side='right'),
                          N_EXPERTS - 1)
    h_pad = jnp.concatenate([h, jnp.zeros((1, D), h.dtype)], axis=0)

    def run(args):
        idx, e = args
        xb = h_pad[idx]
        gu = xb @ w_gu[e] + b_gu[e]
        g_, u_ = gu[:, 0::2], gu[:, 1::2]
        g_ = jnp.minimum(g_, SWIGLU_LIMIT)
        u_ = jnp.clip(u_, -SWIGLU_LIMIT, SWIGLU_LIMIT)
        act = (u_ + 1) * (g_ * jax.nn.sigmoid(SWIGLU_ALPHA * g_))
        return act @ w_dn[e] + b_dn[e]

    yb = lax.map(run, (slot_tok.reshape(n_blocks, MOE_BLK), block_e)).reshape(P, D)
    y = jnp.zeros((N + 1, D), h.dtype).at[slot_tok].add(yb * slot_gate[:, None].astype(yb.dtype))
    return y[:N]


def setup_inputs(seed: int = 0) -> dict:
    key = jax.random.key(seed)
    ks = jax.random.split(key, 32)
    f32 = jnp.float32
    L, D, E, F, H = DEPTH, D_MODEL, N_EXPERTS, D_EXPERT, DN_HEADS

    def nrm(i, shape, scale):
        return jax.random.normal(ks[i], shape, f32) * scale

    def gain(i, shape):
        return 1.0 + nrm(i, shape, 0.02)

    dt = jnp.exp(jax.random.uniform(ks[16], (L, 2, H), f32, float(np.log(1e-3)), float(np.log(1e-1))))
    return {
        "x": nrm(0, (BATCH, SEQ, D), 1.0),
        "c": nrm(1, (BATCH, D), 1.0),
        "ctx": nrm(2, (BATCH, CTX_LEN, D), 1.0),
        "c_ctx": nrm(3, (D,), 1.0),
        "w_mod": nrm(4, (L, D, 6 * D), 0.5 * D ** -0.5),
        "b_mod": nrm(5, (L, 6 * D), 0.01),
        "norm1_g": gain(6, (L, D)),
        "norm2_g": gain(7, (L, D)),
        "w_in": nrm(8, (L, D, D_IN), D ** -0.5),
        "mla_q_norm_g": gain(9, (L, MLA_Q_RANK)),
        "mla_w_q_up": nrm(10, (L, MLA_Q_RANK, MLA_HEADS * (MLA_NOPE + MLA_ROPE)), MLA_Q_RANK ** -0.5),
        "mla_kv_norm_g": gain(11, (L, MLA_KV_RANK)),
        "mla_w_kv_up": nrm(12, (L, MLA_KV_RANK, MLA_HEADS * (MLA_NOPE + MLA_V)), MLA_KV_RANK ** -0.5),
        "swa_sink": nrm(13, (L, SWA_HEADS), 0.5),
        "dn_conv_w": nrm(14, (L, DN_CONV, 3 * DN_HEADS * DN_HEAD_DIM), DN_CONV ** -0.5),
        "dn_a_log": jnp.log(jax.random.uniform(ks[15], (L, 2, H), f32, 1.0, 8.0)),
        "dn_dt_bias": dt + jnp.log(-jnp.expm1(-dt)),
        "dn_norm_g": gain(17, (L, DN_HEAD_DIM)),
        "w_branch": nrm(18, (L, N_BRANCH, BRANCH_W, D), BRANCH_W ** -0.5),
        "w_out": nrm(19, (L, D, D), D ** -0.5),
        "router_w": nrm(20, (L, D, E), D ** -0.5),
        "router_b": nrm(21, (L, E), 0.01),
        "exp_w_gu": nrm(22, (L, E, D, 2 * F), D ** -0.5),
        "exp_b_gu": nrm(23, (L, E, 2 * F), 0.01),
        "exp_w_dn": nrm(24, (L, E, F, D), F ** -0.5),
        "exp_b_dn": nrm(25, (L, E, D), 0.01),
        "final_norm_g": gain(26, (D,)),
    }


def reference(x, c, ctx, c_ctx, w_mod, b_mod, norm1_g, norm2_g, w_in, mla_q_norm_g, mla_w_q_up,
              mla_kv_norm_g, mla_w_kv_up, swa_sink, dn_conv_w, dn_a_log, dn_dt_bias, dn_norm_g,
              w_branch, w_out, router_w, router_b, exp_w_gu, exp_b_gu, exp_w_dn, exp_b_dn, final_norm_g):
    B, S, D = x.shape
    T = ctx.shape[1]
    for l in range(DEPTH):
        last = l == DEPTH - 1
        mod = jax.nn.silu(c) @ w_mod[l] + b_mod[l]
        mod_c = jax.nn.silu(c_ctx) @ w_mod[l] + b_mod[l]
        sh1, sc1, g1, sh2, sc2, g2 = [m[:, None, :] for m in jnp.split(mod, 6, axis=-1)]
        sh1c, sc1c, g1c, sh2c, sc2c, g2c = jnp.split(mod_c, 6, axis=-1)

        h = _modulate(_rmsnorm(x, norm1_g[l]), sh1, sc1)
        hc = _modulate(_rmsnorm(ctx, norm1_g[l]), sh1c, sc1c)
        y, yc = _hybrid_mixer(h, hc, w_in[l], mla_q_norm_g[l], mla_w_q_up[l], mla_kv_norm_g[l], mla_w_kv_up[l],
                              swa_sink[l], dn_conv_w[l], dn_a_log[l], dn_dt_bias[l], dn_norm_g[l],
                              w_branch[l], w_out[l], ctx_out=not last)
        x = x + g1 * y
        h = _modulate(_rmsnorm(x, norm2_g[l]), sh2, sc2)
        moe_p = (router_w[l], router_b[l], exp_w_gu[l], exp_b_gu[l], exp_w_dn[l], exp_b_dn[l])
        if last:
            x = x + g2 * _moe_ffn(h.reshape(B * S, D), *moe_p).reshape(B, S, D)
        else:
            ctx = ctx + g1c * yc
            hc = _modulate(_rmsnorm(ctx, norm2_g[l]), sh2c, sc2c)
            f = _moe_ffn(jnp.concatenate([h.reshape(B * S, D), hc.reshape(B * T, D)], axis=0), *moe_p)
            x = x + g2 * f[:B * S].reshape(B, S, D)
            ctx = ctx + g2c * f[B * S:].reshape(B, T, D)
    return _rmsnorm(x, final_norm_g)
```

```python
import numpy as np
import concourse.bass as bass
import concourse.mybir as mybir
from concourse.bass_utils import run_bass_kernel_spmd
from concourse.alu_op_type import AluOpType as ALU

F32 = mybir.dt.float32
BF16 = mybir.dt.bfloat16
AF = mybir.ActivationFunctionType
AX = mybir.AxisListType

D = 1024
DEPTH = 2
NB = 2
TC = 256
TL = 2048
TA = TC + TL
EPS = 1e-6
NCORES = 8


class Buf:
    __slots__ = ("w", "r")

    def __init__(self):
        self.w = None
        self.r = []


class T:
    def __init__(self, t, psum=False):
        self.t = t
        self.b = Buf()
        self.psum = psum

    def __getitem__(self, k):
        return self.t[k]


class Prog:
    def __init__(self, debug=False, as_input=()):
        self.as_input = set(as_input)
        self.nc = bass.Bass("TRN2", target_bir_lowering=False)
        nc = self.nc
        self.debug = debug
        self.eng = {"pe": nc.tensor, "act": nc.scalar, "dve": nc.vector, "pool": nc.gpsimd, "sp": nc.sync}
        self.esem = {k: nc.alloc_semaphore("sem_" + k) for k in ("pe", "act", "dve", "pool")}
        self.ecnt = {k: 0 for k in self.esem}
        self.waited = {k: {} for k in self.eng}
        self.dsems = [nc.alloc_semaphore("dsem%d" % i) for i in range(48)]
        self.dcnt = [0] * len(self.dsems)
        self.dnext = 0
        self.semid = {}
        self.n_inst = 0
        self.out_events = []
        self.scopes = []
        self.uid = 0

    def sb(self, name, shape, dt=F32):
        self.uid += 1
        name = "%s_%d" % (name, self.uid)
        if self.scopes:
            return T(self.scopes[-1].enter_context(self.nc.sbuf_tensor(name, list(shape), dt)))
        return T(self.nc.alloc_sbuf_tensor(name, list(shape), dt))

    def ps(self, name, shape, dt=F32):
        self.uid += 1
        name = "%s_%d" % (name, self.uid)
        if self.scopes:
            return T(self.scopes[-1].enter_context(self.nc.psum_tensor(name, list(shape), dt)), psum=True)
        return T(self.nc.alloc_psum_tensor(name, list(shape), dt), psum=True)

    def scope(self):
        return _Scope(self)

    def barrier(self):
        evs = [(self.dsems[i], self.dcnt[i]) for i in range(len(self.dsems)) if self.dcnt[i] > 0]
        evs += [(self.esem[k], self.ecnt[k]) for k in self.esem if self.ecnt[k] > 0]
        for en in self.eng:
            self._wait(en, evs)

    def dram(self, name, shape, dt=F32, kind="Internal"):
        if self.debug and kind == "Internal":
            kind = "ExternalInput" if name in self.as_input else "ExternalOutput"
        return T(self.nc.dram_tensor(name, list(shape), dt, kind=kind))

    def _wait(self, en, evs):
        e = self.eng[en]
        w = self.waited[en]
        best = {}
        for ev in evs:
            if ev is None:
                continue
            sem, val = ev
            k = id(sem)
            if w.get(k, 0) >= val:
                continue
            if k not in best or best[k][1] < val:
                best[k] = (sem, val)
        for k, (sem, val) in best.items():
            e.wait_ge(sem, val)
            w[k] = val

    def _deps(self, en, R, W):
        evs = []
        for b in R:
            evs.append(b.b.w)
            if b.psum:
                own = self.esem.get(en)
                evs.extend(ev for ev in b.b.r if ev[0] is not own)
        for b in W:
            evs.append(b.b.w)
            evs.extend(b.b.r)
        if en == "pe":
            s = self.esem["pe"]
            evs = [ev for ev in evs if ev is not None and ev[0] is not s]
        return evs

    def _commit(self, ev, R, W):
        for b in R:
            b.b.r.append(ev)
            if len(b.b.r) > 6:
                d = {}
                for s, v in b.b.r:
                    if id(s) not in d or d[id(s)][1] < v:
                        d[id(s)] = (s, v)
                b.b.r = list(d.values())
        for b in W:
            b.b.w = ev
            b.b.r = []

    def op(self, en, fn, R=(), W=()):
        self._wait(en, self._deps(en, R, W))
        inst = fn(self.eng[en])
        self.ecnt[en] += 1
        inst.then_inc(self.esem[en], 1)
        self._commit((self.esem[en], self.ecnt[en]), R, W)
        self.n_inst += 1
        return inst

    def dma(self, out, in_, R=(), W=(), q="sp", is_output=False, **kw):
        i = self.dnext
        self.dnext = (self.dnext + 1) % len(self.dsems)
        sem = self.dsems[i]
        evs = self._deps(q, R, W)
        if self.dcnt[i] > 0:
            evs.append((sem, self.dcnt[i]))
        self._wait(q, evs)
        inst = self.eng[q].dma_start(out=out, in_=in_, **kw)
        self.dcnt[i] += 16
        inst.then_inc(sem, 16)
        ev = (sem, self.dcnt[i])
        self._commit(ev, R, W)
        if is_output:
            self.out_events.append(ev)
        self.n_inst += 1
        return inst

    def finish(self):
        evs = [(self.dsems[i], self.dcnt[i]) for i in range(len(self.dsems)) if self.dcnt[i] > 0]
        evs += [(self.esem[k], self.ecnt[k]) for k in self.esem if self.ecnt[k] > 0]
        self._wait("sp", evs)

    def mm(self, out, lhsT, rhs, start, stop, R, W):
        return self.op("pe", lambda e: e.matmul(out, lhsT, rhs, start=start, stop=stop), R, W)

    def tr(self, out, in_, ident, R, W):
        return self.op("pe", lambda e: e.transpose(out, in_, ident), R, W)

    def act(self, out, in_, func, R, W, en="act", **kw):
        return self.op(en, lambda e: e.activation(out=out, in_=in_, func=func, **kw), R, W)

    def tt(self, out, in0, in1, op, R, W, en="dve"):
        return self.op(en, lambda e: e.tensor_tensor(out=out, in0=in0, in1=in1, op=op), R, W)

    def ts(self, out, in0, s1, s2, op0, op1, R, W, en="dve"):
        return self.op(en, lambda e: e.tensor_scalar(out=out, in0=in0, scalar1=s1, scalar2=s2, op0=op0, op1=op1), R, W)

    def stt(self, out, in0, scalar, in1, op0, op1, R, W):
        return self.op("dve", lambda e: e.scalar_tensor_tensor(out=out, in0=in0, scalar=scalar, in1=in1, op0=op0, op1=op1), R, W)

    def copy(self, out, in_, R, W, en="dve"):
        if en == "act":
            return self.op("act", lambda e: e.copy(out=out, in_=in_), R, W)
        return self.op(en, lambda e: e.tensor_copy(out=out, in_=in_), R, W)


class _Scope:
    def __init__(self, P):
        self.P = P

    def __enter__(self):
        import contextlib
        self.es = contextlib.ExitStack()
        self.P.scopes.append(self.es)
        return self

    def __exit__(self, *a):
        self.P.barrier()
        self.P.scopes.pop()
        self.es.close()
        return False


NFM = 6592
NTM = 672
FM_OFF = dict(cq=0, ckv=384, sqA=640, sqB=1152, skA=1664, skB=1792, dqkv=1920, gate=3456, krA=6528, krB=6560)
TM_OFF = dict(sv=0, dz=128, dbeta=640, da=656)
IN_OFF = dict(cq=0, ckv=384, kr=640, sq=672, sk=1184, sv=1312, dqkv=1440, dz=2976, dbeta=3488, da=3504, gate=3520)


def host_w_in_layout(w_in):
    o = IN_OFF
    idx = []
    idx += list(range(o["cq"], o["cq"] + 384))
    idx += list(range(o["ckv"], o["ckv"] + 256))
    idx += list(range(o["sq"], o["sq"] + 512))
    for h in range(8):
        b = o["sq"] + 64 * h
        idx += list(range(b + 32, b + 64)) + list(range(b, b + 32))
    idx += list(range(o["sk"], o["sk"] + 128))
    for h in range(2):
        b = o["sk"] + 64 * h
        idx += list(range(b + 32, b + 64)) + list(range(b, b + 32))
    idx += list(range(o["dqkv"], o["dqkv"] + 1536))
    idx += list(range(o["gate"], o["gate"] + 3072))
    idx += list(range(o["kr"], o["kr"] + 32))
    idx += list(range(o["kr"] + 16, o["kr"] + 32)) + list(range(o["kr"], o["kr"] + 16))
    assert len(idx) == NFM
    w_fm = np.ascontiguousarray(w_in[:, :, idx])
    idt = list(range(o["sv"], o["sv"] + 128)) + list(range(o["dz"], o["dz"] + 512)) + list(range(o["dbeta"], o["dbeta"] + 32))
    w_tm = np.ascontiguousarray(w_in[:, :, idt])
    return w_fm, w_tm


class Ctx:
    pass


def declare_io(P, C):
    nc = P.nc
    def inp(name, shape):
        return T(nc.dram_tensor(name, list(shape), F32, kind="ExternalInput"))
    C.x = inp("x", [NB, TL, D])
    C.ctx = inp("ctx", [NB, TC, D])
    C.cT = inp("cT", [128, 8, 3])
    C.w_mod = inp("w_mod", [DEPTH, D, 6 * D])
    C.b_mod = inp("b_mod", [DEPTH, 6 * D])
    C.norm1_g = inp("norm1_g", [DEPTH, D])
    C.norm2_g = inp("norm2_g", [DEPTH, D])
    C.w_fm = inp("w_fm", [DEPTH, D, NFM])
    C.w_tm = inp("w_tm", [DEPTH, D, NTM])
    C.ident = inp("ident", [128, 128])
    C.mod = P.dram("mod", [DEPTH, 3, 6 * D])
    C.proj = P.dram("proj", [NB, NFM, TA])
    C.projT = P.dram("projT", [NB, TA, NTM])


def stage_consts(P, C):
    C.ident_sb = P.sb("ident_sb", [128, 128])
    P.dma(C.ident_sb[:], C.ident[:, :], R=[C.ident], W=[C.ident_sb])
    C.ones_sb = P.sb("ones_sb", [128, 128])
    P.op("dve", lambda e: e.memset(C.ones_sb[:], 1.0), W=[C.ones_sb])


def stage_mod(P, C, l):
    with P.scope():
        _stage_mod(P, C, l)


def _stage_mod(P, C, l):
    C.scT = P.sb("scT", [128, 8, 3])
    C.modrow = P.sb("modrow", [3, 6 * D])
    C.wm = [P.sb("wm%d" % i, [128, 8, 512]) for i in range(2)]
    C.bm = P.sb("bm", [3, 6 * D])
    C.gbc = P.sb("gbc", [3, 2, D])
    C.ps_mod = [P.ps("ps_mod%d" % i, [128, 512]) for i in range(2)]
    P.dma(C.scT[:], C.cT[:, :, :], R=[C.cT], W=[C.scT])
    P.act(C.scT[:], C.scT[:], AF.Silu, R=[C.scT], W=[C.scT])
    P.dma(C.bm[:], C.b_mod[l, :].partition_broadcast(3), R=[C.b_mod], W=[C.bm])
    P.dma(C.gbc[:, 0, :], C.norm1_g[l, :].partition_broadcast(3), R=[C.norm1_g], W=[C.gbc])
    P.dma(C.gbc[:, 1, :], C.norm2_g[l, :].partition_broadcast(3), R=[C.norm2_g], W=[C.gbc])
    wv = C.w_mod.t[l].rearrange("(c p) n -> p c n", p=128)
    for j in range(12):
        wt = C.wm[j % 2]
        ps = C.ps_mod[j % 2]
        P.dma(wt[:], wv[:, :, j * 512:(j + 1) * 512], R=[C.w_mod], W=[wt])
        for c in range(8):
            P.mm(ps[0:3, :], C.scT[:, c, :], wt[:, c, :], c == 0, c == 7, R=[C.scT, wt], W=[ps])
        P.tt(C.modrow[:, j * 512:(j + 1) * 512], ps[0:3, :], C.bm[:, j * 512:(j + 1) * 512], ALU.add, R=[ps, C.bm], W=[C.modrow])
    for slot, gi in ((1, 0), (4, 1)):
        sl = C.modrow[:, slot * D:(slot + 1) * D]
        P.stt(sl, sl, 1.0, C.gbc[:, gi, :], ALU.add, ALU.mult, R=[C.modrow, C.gbc], W=[C.modrow])
    P.dma(C.mod[l], C.modrow[:], R=[C.modrow], W=[C.mod])


def token_src(C, l, b, t0, n):
    if l == 0:
        if t0 < TC:
            return C.ctx, C.ctx[b, t0:t0 + n, :]
        return C.x, C.x[b, t0 - TC:t0 - TC + n, :]
    return C.xres, C.xres[b, t0:t0 + n, :]


def blocks():
    out = [(0, TC)]
    for i in range(TL // 512):
        out.append((TC + i * 512, 512))
    return out


def load_mod_bc(P, C, l, row, slots, dst):
    for s, d in zip(slots, dst):
        P.dma(d[:], C.mod[l, row, s * D:(s + 1) * D].partition_broadcast(128), R=[C.mod], W=[d])


def norm_mod_T(P, C, src_t, src_ap, A_bc, sh_bc, hT, col0, it):
    xt = C.xt[it % 2]
    ht = C.ht[it % 2]
    st = C.st[it % 2]
    ps = C.ps_tr[it % 2]
    P.dma(xt[:], src_ap, R=[src_t], W=[xt])
    P.act(ht[:], xt[:], AF.Square, R=[xt], W=[ht, st], accum_out=st[:, 0:1])
    P.ts(st[:, 1:2], st[:, 0:1], 1.0 / D, EPS, ALU.mult, ALU.add, R=[st], W=[st])
    P.act(st[:, 2:3], st[:, 1:2], AF.Sqrt, R=[st], W=[st])
    P.op("dve", lambda e: e.reciprocal(out=st[:, 3:4], in_=st[:, 2:3]), R=[st], W=[st])
    P.stt(ht[:], xt[:], st[:, 3:4], A_bc[:], ALU.mult, ALU.mult, R=[xt, st, A_bc], W=[ht])
    P.tt(ht[:], ht[:], sh_bc[:], ALU.add, R=[ht, sh_bc], W=[ht])
    for c in range(8):
        P.tr(ps[:, c * 128:(c + 1) * 128], ht[:, c * 128:(c + 1) * 128], C.ident_sb[:], R=[ht, C.ident_sb], W=[ps])
    P.copy(hT[:, :, col0:col0 + 128], ps[:].rearrange("p (c t) -> p c t", c=8), R=[ps], W=[hT], en="act" if it % 2 else "dve")


def stage_proj(P, C, l, do_blocks=None):
    with P.scope():
        _stage_proj(P, C, l, do_blocks)


def _stage_proj(P, C, l, do_blocks=None):
    C.xt = [P.sb("xt%d" % i, [128, D]) for i in range(2)]
    C.ht = [P.sb("ht%d" % i, [128, D]) for i in range(2)]
    C.st = [P.sb("st%d" % i, [128, 4]) for i in range(2)]
    C.ps_tr = [P.ps("ps_tr%d" % i, [128, 1024]) for i in range(2)]
    C.hT = [P.sb("hT%d" % i, [128, 8, 512]) for i in range(2)]
    C.A_bc = [P.sb("A_bc%d" % i, [128, D]) for i in range(3)]
    C.sh_bc = [P.sb("sh_bc%d" % i, [128, D]) for i in range(3)]
    C.wfm = [P.sb("wfm%d" % i, [128, 8, 512]) for i in range(2)]
    C.wtm = P.sb("wtm", [128, 8, NTM])
    C.ps_mm = [P.ps("ps_mm%d" % i, [128, 512]) for i in range(2)]
    C.ot = [P.sb("ot%d" % i, [128, 512]) for i in range(3)]
    for row in range(3):
        load_mod_bc(P, C, l, row, (1, 0), (C.A_bc[row], C.sh_bc[row]))
    P.dma(C.wtm[:], C.w_tm.t[l].rearrange("(c p) n -> p c n", p=128), R=[C.w_tm], W=[C.wtm])
    wv = C.w_fm.t[l].rearrange("(c p) n -> p c n", p=128)
    it = 0
    ib = 0
    ig = 0
    io = 0
    for b in range(NB):
        for (t0, n) in blocks():
            if do_blocks is not None and (b, t0) not in do_blocks:
                continue
            row = 2 if t0 < TC else b
            hT = C.hT[ib % 2]
            ib += 1
            for i in range(n // 128):
                src_t, src_ap = token_src(C, l, b, t0 + i * 128, 128)
                norm_mod_T(P, C, src_t, src_ap, C.A_bc[row], C.sh_bc[row], hT, i * 128, it)
                it += 1
            for g in range((NFM + 511) // 512):
                c0 = g * 512
                cw = min(512, NFM - c0)
                wt = C.wfm[ig % 2]
                ig += 1
                P.dma(wt[:, :, 0:cw], wv[:, :, c0:c0 + cw], R=[C.w_fm], W=[wt])
                for j in range((cw + 127) // 128):
                    m = min(128, cw - j * 128)
                    ps = C.ps_mm[io % 2]
                    ot = C.ot[io % 3]
                    for c in range(8):
                        P.mm(ps[0:m, 0:n], wt[:, c, j * 128:j * 128 + m], hT[:, c, 0:n], c == 0, c == 7, R=[wt, hT], W=[ps])
                    P.copy(ot[0:m, 0:n], ps[0:m, 0:n], R=[ps], W=[ot], en="act" if io % 2 else "dve")
                    r0 = c0 + j * 128
                    P.dma(C.proj[b, r0:r0 + m, t0:t0 + n], ot[0:m, 0:n], R=[ot], W=[], q="act")
                    io += 1
            for i in range(n // 128):
                for (q0, qw) in ((0, 512), (512, NTM - 512)):
                    ps = C.ps_mm[io % 2]
                    ot = C.ot[io % 3]
                    for c in range(8):
                        P.mm(ps[:, 0:qw], hT[:, c, i * 128:(i + 1) * 128], C.wtm[:, c, q0:q0 + qw], c == 0, c == 7, R=[C.wtm, hT], W=[ps])
                    P.copy(ot[:, 0:qw], ps[:, 0:qw], R=[ps], W=[ot], en="act" if io % 2 else "dve")
                    P.dma(C.projT[b, t0 + i * 128:t0 + (i + 1) * 128, q0:q0 + qw], ot[:, 0:qw], R=[ot], W=[], q="act")
                    io += 1


DN_STOP = 0
def bc_mid(ap, n):
    return ap.unsqueeze(1).broadcast_to([ap.shape[0], n, ap.shape[1]])


def bc_last(ap, n):
    return ap.unsqueeze(2).broadcast_to([ap.shape[0], ap.shape[1], n])


def declare_dn(P, C):
    nc = P.nc
    def inp(name, shape):
        return T(nc.dram_tensor(name, list(shape), F32, kind="ExternalInput"))
    C.cw = inp("cw", [DEPTH, 128, 12, 5])
    C.dn_a_log = inp("dn_a_log", [DEPTH, 16])
    C.dn_dt_bias = inp("dn_dt_bias", [DEPTH, 16])
    C.dn_norm_g = inp("dn_norm_g", [DEPTH, 64])
    C.masks = inp("masks", [64, 6, 64])
    C.bd = inp("bd", [128, 128])
    C.dnf = P.dram("dnf", [NB, 1024, TA])
    C.dnt = P.dram("dnt", [NB, TA, 1536])
    C.gates = P.dram("gates", [NB, TA, 32])
    C.dno = P.dram("dno", [NB, 2, TA, 512])
    C.br = P.dram("br", [NB, 3, 512, TA])


def host_masks():
    a = np.arange(64)[:, None]
    b = np.arange(64)[None, :]
    m = np.stack([a <= b, a >= b, a > b, a < b, a == b, np.ones((64, 64), bool)], 1).astype(np.float32)
    bd = np.kron(np.eye(2), np.ones((64, 64))).astype(np.float32)
    return np.ascontiguousarray(m), bd


def stage_dn_prep(P, C, l, b, parts=(1, 2, 3, 4)):
    with P.scope():
        _stage_dn_prep(P, C, l, b, parts)


def _stage_dn_prep(P, C, l, b, parts=(1, 2, 3, 4)):
    WV = TA + 4
    ub = [P.sb("ub%d" % i, [128, TA + 8]) for i in range(2)]
    acc = [P.sb("acc%d" % i, [128, WV]) for i in range(2)]
    sqb = P.sb("sqb", [128, WV])
    rs = [P.sb("rs%d" % i, [128, 512]) for i in range(2)]
    cws = P.sb("cws", [128, 12, 5])
    bds = P.sb("bds", [128, 128])
    tmb = [P.sb("tmb%d" % i, [128, 4, 128]) for i in range(2)]
    ps_n = [P.ps("ps_n%d" % i, [128, 512]) for i in range(2)]
    ps_t = [P.ps("ps_t%d" % i, [128, 512]) for i in range(2)]
    P.dma(cws[:], C.cw[l], R=[C.cw], W=[cws])
    P.dma(bds[:], C.bd[:, :], R=[C.bd], W=[bds])
    for u in ub:
        P.op("dve", lambda e: e.memset(u[:], 0.0), W=[u])
    bg = P.sb("bg", [128, 18, 32])
    go = P.sb("go", [128, 18, 32])
    dtb = P.sb("dtb", [128, 16])
    nA = P.sb("nA", [128, 16])
    if 1 in parts:
      P.dma(bg[:], C.projT[b, :, 640:672].rearrange("(t p) f -> p t f", p=128), R=[C.projT], W=[bg])
      P.dma(dtb[:], C.dn_dt_bias[l, :].partition_broadcast(128), R=[C.dn_dt_bias], W=[dtb])
      P.dma(nA[:], C.dn_a_log[l, :].partition_broadcast(128), R=[C.dn_a_log], W=[nA])
      P.act(nA[:], nA[:], AF.Exp, R=[nA], W=[nA])
      P.ts(nA[:], nA[:], -1.0, None, ALU.mult, ALU.bypass, R=[nA], W=[nA])
      P.act(go[:, :, 0:16], bg[:, :, 0:16], AF.Sigmoid, R=[bg], W=[go])
      P.tt(bg[:, :, 16:32], bg[:, :, 16:32], bc_mid(dtb[:], 18), ALU.add, R=[bg, dtb], W=[bg])
      P.act(bg[:, :, 16:32], bg[:, :, 16:32], AF.Exp, R=[bg], W=[bg])
      P.act(bg[:, :, 16:32], bg[:, :, 16:32], AF.Ln, R=[bg], W=[bg], bias=1.0)
      P.tt(go[:, :, 16:32], bg[:, :, 16:32], bc_mid(nA[:], 18), ALU.mult, R=[bg, nA], W=[go])
      P.dma(C.gates[b].rearrange("(t p) f -> p t f", p=128), go[:], R=[go], W=[], q="act")
    r0 = FM_OFF["dqkv"]
    it = 0
    for c in (range(12) if 2 in parts else []):
        u = ub[c % 2]
        a = acc[c % 2]
        P.dma(u[:, 2:2 + TC], C.proj[b, r0 + c * 128:r0 + (c + 1) * 128, 0:TC], R=[C.proj], W=[u])
        P.dma(u[:, 6 + TC:6 + TA], C.proj[b, r0 + c * 128:r0 + (c + 1) * 128, TC:TA], R=[C.proj], W=[u])
        P.ts(a[:], u[:, 0:WV], cws[:, c, 0:1], None, ALU.mult, ALU.bypass, R=[u, cws], W=[a])
        for j in range(1, 5):
            P.stt(a[:], u[:, j:j + WV], cws[:, c, j:j + 1], a[:], ALU.mult, ALU.add, R=[u, cws, a], W=[a])
        P.act(a[:], a[:], AF.Silu, R=[a], W=[a])
        if c < 8 and 3 in parts:
            P.act(sqb[:], a[:], AF.Square, R=[a], W=[sqb])
            for k in range((WV + 511) // 512):
                n = min(512, WV - k * 512)
                ps = ps_n[k % 2]
                r = rs[k % 2]
                P.mm(ps[:, 0:n], bds[:], sqb[:, k * 512:k * 512 + n], True, True, R=[bds, sqb], W=[ps])
                P.ts(r[:, 0:n], ps[:, 0:n], 1.0, EPS, ALU.mult, ALU.add, R=[ps], W=[r])
                P.act(r[:, 0:n], r[:, 0:n], AF.Sqrt, R=[r], W=[r])
                P.op("dve", lambda e: e.reciprocal(out=r[:, 0:n], in_=r[:, 0:n]), R=[r], W=[r])
                P.stt(a[:, k * 512:k * 512 + n], a[:, k * 512:k * 512 + n], 0.125 if c < 4 else 1.0, r[:, 0:n], ALU.mult, ALU.mult, R=[a, r], W=[a])
            P.dma(C.dnf[b, c * 128:(c + 1) * 128, 0:TC], a[:, 0:TC], R=[a], W=[], q="act")
            P.dma(C.dnf[b, c * 128:(c + 1) * 128, TC:TA], a[:, TC + 4:WV], R=[a], W=[], q="act")
        for t0 in (range(0, 18, 4) if 4 in parts else []):
            nt = min(4, 18 - t0)
            ps = ps_t[it % 2]
            tb = tmb[it % 2]
            it += 1
            for k in range(nt):
                tile = t0 + k
                col = tile * 128 if tile < 2 else 4 + tile * 128
                P.tr(ps[:, k * 128:(k + 1) * 128], a[:, col:col + 128], C.ident_sb[:], R=[a, C.ident_sb], W=[ps])
            P.copy(tb[:, 0:nt, :], ps[:, 0:nt * 128].rearrange("p (t f) -> p t f", f=128), R=[ps], W=[tb], en="act" if it % 2 else "dve")
            P.dma(C.dnt[b, t0 * 128:(t0 + nt) * 128, c * 128:(c + 1) * 128].rearrange("(t p) f -> p t f", p=128), tb[:, 0:nt, :], R=[tb], W=[], q="act")


def stage_dn_scan(P, C, l, b, with_ctx_out, only_chunks=None):
    with P.scope():
        _stage_dn_scan(P, C, l, b, with_ctx_out, only_chunks)


def _stage_dn_scan(P, C, l, b, with_ctx_out, only_chunks):
    mk = P.sb("mk", [64, 6, 64])
    P.dma(mk[:], C.masks[:, :, :], R=[C.masks], W=[mk])
    LE, GE, GT, LT, I64, ONE = [mk[:, i, :] for i in range(6)]
    NBUF = 2
    def sbl(name, shape):
        return [P.sb(name + str(i), shape) for i in range(NBUF)]
    ktm = sbl("ktm", [64, 8, 64]); vtm = sbl("vtm", [64, 8, 64]); kT = sbl("kT", [64, 8, 64]); qT = sbl("qT", [64, 8, 64])
    gt = sbl("gt", [64, 32])
    sm = sbl("sm", [64, 8, 8])
    rd = sbl("rd", [64, 8, 64]); rdT = sbl("rdT", [64, 8, 64])
    decS = sbl("decS", [64, 8, 64]); decCT = sbl("decCT", [64, 8, 64])
    Ma = sbl("Ma", [64, 8, 64]); Mb = sbl("Mb", [64, 8, 64]); MTa = sbl("MTa", [64, 8, 64]); MTb = sbl("MTb", [64, 8, 64])
    Pa = sbl("Pa", [64, 8, 64]); Pb = sbl("Pb", [64, 8, 64])
    vb = sbl("vb", [64, 8, 64]); rw = sbl("rw", [64, 8, 64]); kdec = sbl("kdec", [64, 8, 64])
    u_ = sbl("u_", [64, 8, 64]); wT = sbl("wT", [64, 8, 64]); aT = sbl("aT", [64, 8, 64])
    vnew = sbl("vnew", [64, 8, 64]); ot = sbl("dno_t", [64, 8, 64]); tmp = sbl("dtmp", [64, 8, 64])
    S = P.sb("Sst", [64, 8, 64])
    psb = [P.ps("dps%d" % i, [64, 512]) for i in range(6)]
    pss = P.ps("dpss", [64, 16])
    pc = [0]

    def nps():
        pc[0] += 1
        return psb[pc[0] % 6]

    def mmh(ps, lhs, rhs, Rl):
        for h in range(8):
            P.mm(ps[:, h * 64:(h + 1) * 64], lhs[:, h, :], rhs[:, h, :], True, True, R=Rl, W=[ps])

    def v3(t):
        return t[:].rearrange("p (h f) -> p h f", h=8)

    ci = 0
    for d in range(2):
        Minc, Mstr = (LE, GT) if d == 0 else (GE, LT)
        Sm = Mstr
        CT = Minc
        P.op("dve", lambda e: e.memset(S[:], 0.0), W=[S])
        order = list(range(4)) + list(range(4, 36)) if d == 0 else list(range(3, -1, -1)) + list(range(35, 3, -1))
        for cidx in order:
            if only_chunks is not None and cidx not in only_chunks:
                continue
            is_ctx = cidx < 4
            want_out = (not is_ctx) or with_ctx_out
            tok0 = cidx * 64
            k = ci % NBUF
            ci += 1
            P.dma(ktm[k][:], C.dnt[b, tok0:tok0 + 64, 512:1024].rearrange("t (h f) -> t h f", h=8), R=[C.dnt], W=[ktm[k]])
            P.dma(vtm[k][:], C.dnt[b, tok0:tok0 + 64, 1024:1536].rearrange("t (h f) -> t h f", h=8), R=[C.dnt], W=[vtm[k]])
            P.dma(kT[k][:], C.dnf[b, 512:1024, tok0:tok0 + 64].rearrange("(h f) t -> f h t", h=8), R=[C.dnf], W=[kT[k]])
            P.dma(qT[k][:], C.dnf[b, 0:512, tok0:tok0 + 64].rearrange("(h f) t -> f h t", h=8), R=[C.dnf], W=[qT[k]])
            P.dma(gt[k][:], C.gates[b, tok0:tok0 + 64, :], R=[C.gates], W=[gt[k]])
            beta = gt[k][:, d * 8:(d + 1) * 8]
            g = gt[k][:, 16 + d * 8:16 + (d + 1) * 8]
            s = sm[k]
            P.mm(pss[:, 0:8], Minc, g, True, True, R=[mk, gt[k]], W=[pss])
            P.mm(pss[:, 8:16], ONE, g, True, True, R=[mk, gt[k]], W=[pss])
            P.copy(s[:, 0:2, :], pss[:, 0:16].rearrange("p (a h) -> p a h", a=2), R=[pss], W=[s])
            P.act(s[:, 2:4, :], s[:, 0:2, :], AF.Exp, R=[s], W=[s])
            P.tt(s[:, 7, :], s[:, 1, :], s[:, 0, :], ALU.subtract, R=[s], W=[s])
            P.act(s[:, 4, :], s[:, 7, :], AF.Exp, R=[s], W=[s])
            P.tt(s[:, 5, :], beta, s[:, 2, :], ALU.mult, R=[s, gt[k]], W=[s])
            P.ts(s[:, 6, :], beta, -1.0, None, ALU.mult, ALU.bypass, R=[gt[k]], W=[s])
            if DN_STOP == 1:
                continue
            P.tt(rd[k][:], bc_mid(Mstr, 8), bc_last(g, 64), ALU.mult, R=[mk, gt[k]], W=[rd[k]])
            P.tt(rdT[k][:], bc_mid(Minc, 8), bc_last(g, 64), ALU.mult, R=[mk, gt[k]], W=[rdT[k]])
            p1 = nps()
            P.mm(p1[:, :], Minc, rd[k][:].rearrange("p h f -> p (h f)"), True, True, R=[mk, rd[k]], W=[p1])
            P.act(decS[k][:].rearrange("p h f -> p (h f)"), p1[:, :], AF.Exp, R=[p1], W=[decS[k]])
            P.tt(decS[k][:], decS[k][:], bc_mid(Sm, 8), ALU.mult, R=[decS[k], mk], W=[decS[k]])
            if DN_STOP == 2:
                continue
            p2 = nps()
            P.mm(p2[:, :], Mstr, rdT[k][:].rearrange("p h f -> p (h f)"), True, True, R=[mk, rdT[k]], W=[p2])
            P.act(decCT[k][:].rearrange("p h f -> p (h f)"), p2[:, :], AF.Exp, R=[p2], W=[decCT[k]])
            P.tt(decCT[k][:], decCT[k][:], bc_mid(CT, 8), ALU.mult, R=[decCT[k], mk], W=[decCT[k]])
            if DN_STOP == 3:
                continue
            p3 = nps()
            mmh(p3, kT[k], kT[k], [kT[k]])
            MT, M, MT2, M2 = MTa[k], Ma[k], MTb[k], Mb[k]
            P.tt(MT[:], v3(p3), decS[k][:], ALU.mult, R=[p3, decS[k]], W=[MT])
            P.tt(MT[:], MT[:], bc_last(s[:, 6, :], 64), ALU.mult, R=[MT, s], W=[MT])
            if DN_STOP == 4:
                continue
            p4 = nps()
            for h in range(8):
                P.tr(p4[:, h * 64:(h + 1) * 64], MT[:, h, :], I64, R=[MT, mk], W=[p4])
            P.copy(M[:], v3(p4), R=[p4], W=[M], en="act")
            if DN_STOP == 5:
                continue
            Pc, Pn = Pa[k], Pb[k]
            P.tt(Pc[:], v3(p4), bc_mid(I64, 8), ALU.add, R=[p4, mk], W=[Pc])
            for lev in range(5):
                pa = nps()
                mmh(pa, M, MT, [M, MT])
                P.copy(MT2[:], v3(pa), R=[pa], W=[MT2], en="act")
                if lev < 4:
                    pb = nps()
                    mmh(pb, MT, M, [M, MT])
                    P.copy(M2[:], v3(pb), R=[pb], W=[M2], en="dve")
                pcx = nps()
                mmh(pcx, MT2, Pc, [MT2, Pc])
                P.tt(Pn[:], v3(pcx), Pc[:], ALU.add, R=[pcx, Pc], W=[Pn])
                Pc, Pn = Pn, Pc
                M, M2 = M2, M
                MT, MT2 = MT2, MT
            if DN_STOP == 6:
                continue
            P.tt(vb[k][:], vtm[k][:], bc_last(beta, 64), ALU.mult, R=[vtm[k], gt[k]], W=[vb[k]])
            P.tt(rw[k][:], ktm[k][:], bc_last(s[:, 5, :], 64), ALU.mult, R=[ktm[k], s], W=[rw[k]])
            P.tt(kdec[k][:], ktm[k][:], bc_last(s[:, 4, :], 64), ALU.mult, R=[ktm[k], s], W=[kdec[k]])
            pu = nps()
            mmh(pu, Pc, vb[k], [Pc, vb[k]])
            P.copy(u_[k][:], v3(pu), R=[pu], W=[u_[k]], en="act")
            pw = nps()
            mmh(pw, rw[k], Pc, [Pc, rw[k]])
            P.copy(wT[k][:], v3(pw), R=[pw], W=[wT[k]], en="act")
            if want_out:
                pa2 = nps()
                mmh(pa2, kT[k], qT[k], [kT[k], qT[k]])
                P.tt(aT[k][:], v3(pa2), decCT[k][:], ALU.mult, R=[pa2, decCT[k]], W=[aT[k]])
            if DN_STOP == 7:
                continue
            pws = nps()
            mmh(pws, wT[k], S, [wT[k], S])
            P.tt(vnew[k][:], u_[k][:], v3(pws), ALU.subtract, R=[u_[k], pws], W=[vnew[k]])
            if want_out:
                pq = nps()
                mmh(pq, qT[k], S, [qT[k], S])
                pv = nps()
                mmh(pv, aT[k], vnew[k], [aT[k], vnew[k]])
                P.tt(tmp[k][:], v3(pq), bc_last(s[:, 2, :], 64), ALU.mult, R=[pq, s], W=[tmp[k]])
                P.tt(ot[k][:], tmp[k][:], v3(pv), ALU.add, R=[tmp[k], pv], W=[ot[k]])
                P.dma(C.dno[b, d, tok0:tok0 + 64, :], ot[k][:].rearrange("p h f -> p (h f)"), R=[ot[k]], W=[], q="act")
            pk = nps()
            mmh(pk, kdec[k], vnew[k], [kdec[k], vnew[k]])
            P.tt(S[:], S[:], bc_last(s[:, 3, :], 64), ALU.mult, R=[S, s], W=[S])
            P.tt(S[:], S[:], v3(pk), ALU.add, R=[S, pk], W=[S])


def stage_dn_out(P, C, l, b, with_ctx_out):
    with P.scope():
        _stage_dn_out(P, C, l, b, with_ctx_out)


def _stage_dn_out(P, C, l, b, with_ctx_out):
    o0 = [P.sb("o0_%d" % i, [128, 8, 64]) for i in range(2)]
    o1 = [P.sb("o1_%d" % i, [128, 8, 64]) for i in range(2)]
    zt = [P.sb("zt_%d" % i, [128, 8, 64]) for i in range(2)]
    sq = [P.sb("osq_%d" % i, [128, 8, 64]) for i in range(2)]
    ms = [P.sb("oms_%d" % i, [128, 8]) for i in range(2)]
    ng = P.sb("ong", [128, 64])
    ps = [P.ps("ops%d" % i, [128, 512]) for i in range(2)]
    oT = [P.sb("ooT%d" % i, [128, 4, 128]) for i in range(2)]
    P.dma(ng[:], C.dn_norm_g[l, :].partition_broadcast(128), R=[C.dn_norm_g], W=[ng])
    for t in range(18):
        if t < 2 and not with_ctx_out:
            continue
        k = t % 2
        rows = slice(t * 128, (t + 1) * 128)
        P.dma(o0[k][:].rearrange("p h f -> p (h f)"), C.dno[b, 0, rows, :], R=[C.dno], W=[o0[k]])
        P.dma(o1[k][:].rearrange("p h f -> p (h f)"), C.dno[b, 1, rows, :], R=[C.dno], W=[o1[k]])
        P.dma(zt[k][:].rearrange("p h f -> p (h f)"), C.projT[b, rows, 128:640], R=[C.projT], W=[zt[k]])
        P.tt(o0[k][:], o0[k][:], o1[k][:], ALU.add, R=[o0[k], o1[k]], W=[o0[k]])
        P.act(sq[k][:], o0[k][:], AF.Square, R=[o0[k]], W=[sq[k]])
        P.op("dve", lambda e: e.tensor_reduce(out=ms[k][:], in_=sq[k][:], axis=AX.X, op=ALU.add), R=[sq[k]], W=[ms[k]])
        P.ts(ms[k][:], ms[k][:], 1.0 / 64, EPS, ALU.mult, ALU.add, R=[ms[k]], W=[ms[k]])
        P.act(ms[k][:], ms[k][:], AF.Sqrt, R=[ms[k]], W=[ms[k]])
        P.op("dve", lambda e: e.reciprocal(out=ms[k][:], in_=ms[k][:]), R=[ms[k]], W=[ms[k]])
        P.tt(o0[k][:], o0[k][:], bc_last(ms[k][:], 64), ALU.mult, R=[o0[k], ms[k]], W=[o0[k]])
        P.tt(o0[k][:], o0[k][:], bc_mid(ng[:], 8), ALU.mult, R=[o0[k], ng], W=[o0[k]])
        P.act(zt[k][:], zt[k][:], AF.Silu, R=[zt[k]], W=[zt[k]])
        P.tt(o0[k][:], o0[k][:], zt[k][:], ALU.mult, R=[o0[k], zt[k]], W=[o0[k]])
        of = o0[k][:].rearrange("p h f -> p (h f)")
        for c in range(4):
            P.tr(ps[k][:, c * 128:(c + 1) * 128], of[:, c * 128:(c + 1) * 128], C.ident_sb[:], R=[o0[k], C.ident_sb], W=[ps[k]])
        P.copy(oT[k][:], ps[k][:].rearrange("p (c t) -> p c t", c=4), R=[ps[k]], W=[oT[k]], en="act")
        P.dma(C.br[b, 2, :, rows].rearrange("(c p) t -> p c t", p=128), oT[k][:], R=[oT[k]], W=[], q="act")


def host_shared(I):
    f = lambda a: np.ascontiguousarray(np.asarray(a, dtype=np.float32))
    w_fm, w_tm = host_w_in_layout(np.asarray(I["w_in"]))
    masks, bd = host_masks()
    L = DEPTH
    sh = dict(
        w_mod=f(I["w_mod"]), b_mod=f(I["b_mod"]), norm1_g=f(I["norm1_g"]), norm2_g=f(I["norm2_g"]),
        w_fm=f(w_fm), w_tm=f(w_tm), ident=np.eye(128, dtype=np.float32),
        cw=f(np.asarray(I["dn_conv_w"]).reshape(L, 5, 12, 128).transpose(0, 3, 2, 1)),
        dn_a_log=f(np.asarray(I["dn_a_log"]).reshape(L, 16)), dn_dt_bias=f(np.asarray(I["dn_dt_bias"]).reshape(L, 16)),
        dn_norm_g=f(I["dn_norm_g"]), masks=masks, bd=bd,
    )
    sh.update(host_attn(I))
    return sh


def host_core(I, core):
    b0 = core * NB
    cs = np.stack([np.asarray(I["c"])[b0], np.asarray(I["c"])[b0 + 1], np.asarray(I["c_ctx"])], 0)
    cT = np.ascontiguousarray(cs.T.reshape(8, 128, 3).transpose(1, 0, 2)).astype(np.float32)
    return dict(x=np.ascontiguousarray(np.asarray(I["x"])[b0:b0 + NB]), ctx=np.ascontiguousarray(np.asarray(I["ctx"])[b0:b0 + NB]), cT=cT)


def rope_tables_np(n_tok, rot_dim):
    rows = n_tok // 64
    row = np.broadcast_to(np.arange(rows)[:, None], (rows, 64)).reshape(-1).astype(np.float32)
    col = np.broadcast_to(np.arange(64)[None, :], (rows, 64)).reshape(-1).astype(np.float32)
    n_freq = rot_dim // 4
    inv_freq = (np.float32(10000.0) ** (-np.arange(n_freq, dtype=np.float32) / np.float32(n_freq))).astype(np.float32)
    ang = np.concatenate([row[:, None] * inv_freq, col[:, None] * inv_freq], axis=-1).astype(np.float32)
    cos, sin = np.cos(ang).astype(np.float32), np.sin(ang).astype(np.float32)
    CC = np.concatenate([cos.T, cos.T], 0)
    SS = np.concatenate([-sin.T, sin.T], 0)
    return np.ascontiguousarray(np.stack([CC, SS], 0))


def declare_attn(P, C):
    nc = P.nc
    def inp(name, shape):
        return T(nc.dram_tensor(name, list(shape), F32, kind="ExternalInput"))
    C.qg = inp("qg", [DEPTH, 128, 3])
    C.kvg = inp("kvg", [DEPTH, 128, 2])
    C.w_qn = inp("w_qn", [DEPTH, 384, 512])
    C.w_qrA = inp("w_qrA", [DEPTH, 384, 256])
    C.w_qrB = inp("w_qrB", [DEPTH, 384, 256])
    C.w_kn = inp("w_kn", [DEPTH, 256, 512])
    C.w_v = inp("w_v", [DEPTH, 256, 512])
    C.ropeM = inp("ropeM", [2, 32, TL])
    C.ropeS = inp("ropeS", [2, 64, TL])
    C.swam = inp("swam", [128, 2, 128])
    C.swa_sink = inp("swa_sink", [DEPTH, 8])


def host_attn(I):
    f = lambda a: np.ascontiguousarray(np.asarray(a, dtype=np.float32))
    L = DEPTH
    wq = np.asarray(I["mla_w_q_up"]).reshape(L, 384, 8, 96)
    wkv = np.asarray(I["mla_w_kv_up"]).reshape(L, 256, 8, 128)
    kk = np.arange(128)[:, None]
    qq = np.arange(128)[None, :]
    swam = np.stack([kk >= qq, kk <= qq], 1).astype(np.float32)
    return dict(
        qg=f(np.asarray(I["mla_q_norm_g"]).reshape(L, 3, 128).transpose(0, 2, 1)),
        kvg=f(np.asarray(I["mla_kv_norm_g"]).reshape(L, 2, 128).transpose(0, 2, 1)),
        w_qn=f(wq[..., :64].reshape(L, 384, 512)),
        w_qrA=f(wq[..., 64:96].reshape(L, 384, 256)),
        w_qrB=f(np.concatenate([wq[..., 80:96], wq[..., 64:80]], -1).reshape(L, 384, 256)),
        w_kn=f(wkv[..., :64].reshape(L, 256, 512)),
        w_v=f(wkv[..., 64:].reshape(L, 256, 512)),
        ropeM=rope_tables_np(TL, 32), ropeS=rope_tables_np(TL, 64), swam=f(swam), swa_sink=f(I["swa_sink"]),
    )


def col_blocks():
    return [(i * 512, min(512, TA - i * 512)) for i in range((TA + 511) // 512)]


def stage_mla(P, C, l, b, ctx_out):
    with P.scope():
        _stage_mla(P, C, l, b, ctx_out)


def _stage_mla(P, C, l, b, ctx_out):
    SCALE = float(96 ** -0.5)
    cqn = P.sb("cqn", [128, 3, TA]); ckvn = P.sb("ckvn", [128, 2, TA])
    sqt = P.sb("sqt", [128, 3, 512]); rs = P.sb("mrs", [128, 512])
    qg = P.sb("qg", [128, 3]); kvg = P.sb("kvg", [128, 2])
    wqn = P.sb("wqn", [128, 3, 512]); wqa = P.sb("wqa", [128, 3, 256]); wqb = P.sb("wqb", [128, 3, 256])
    wkn = P.sb("wkn", [128, 2, 512]); wv = P.sb("wv", [128, 2, 512])
    rope = P.sb("ropem", [32, 2, TL])
    krr = P.sb("krr", [32, TA]); krb = P.sb("krb", [32, TA])
    qn = P.sb("qn", [64, TA]); qr = P.sb("qr", [32, TA]); qrb = P.sb("qrb", [32, TA]); kn = P.sb("kn", [64, TA])
    vh = P.sb("vh", [128, 18, 64]); oT = P.sb("oT", [64, TA])
    pt = [P.sb("pt%d" % i, [128, 512]) for i in range(3)]
    rd = [P.sb("rdm%d" % i, [64, 512]) for i in range(2)]
    pA = [P.ps("pA%d" % i, [128, 512]) for i in range(2)]
    pO = [P.ps("pO%d" % i, [64, 512]) for i in range(2)]
    pD = [P.ps("pD%d" % i, [64, 512]) for i in range(2)]
    pX = [P.ps("pX%d" % i, [128, 512]) for i in range(2)]
    ix = [0]

    def npx():
        ix[0] += 1
        return pX[ix[0] % 2]

    P.dma(qg[:], C.qg[l], R=[C.qg], W=[qg]); P.dma(kvg[:], C.kvg[l], R=[C.kvg], W=[kvg])
    for (wt, src) in ((wqn, C.w_qn), (wqa, C.w_qrA), (wqb, C.w_qrB), (wkn, C.w_kn), (wv, C.w_v)):
        P.dma(wt[:], src.t[l].rearrange("(c p) n -> p c n", p=128), R=[src], W=[wt])
    P.dma(rope[:], C.ropeM[:, :, :].rearrange("a p t -> p a t"), R=[C.ropeM], W=[rope])
    CC, SS = rope[:, 0, :], rope[:, 1, :]
    for c in range(3):
        P.dma(cqn[:, c, :], C.proj[b, c * 128:(c + 1) * 128, :], R=[], W=[cqn])
    for c in range(2):
        P.dma(ckvn[:, c, :], C.proj[b, 384 + c * 128:384 + (c + 1) * 128, :], R=[], W=[ckvn])
    P.dma(krr[:], C.proj[b, FM_OFF["krA"]:FM_OFF["krA"] + 32, :], R=[], W=[krr])
    P.dma(krb[:], C.proj[b, FM_OFF["krB"]:FM_OFF["krB"] + 32, :], R=[], W=[krb])
    for (xt, nchunk, g, dim) in ((cqn, 3, qg, 384), (ckvn, 2, kvg, 256)):
        for (c0, n) in col_blocks():
            P.act(sqt[:, 0:nchunk, 0:n], xt[:, :, c0:c0 + n], AF.Square, R=[xt], W=[sqt])
            ps = npx()
            for c in range(nchunk):
                P.mm(ps[:, 0:n], C.ones_sb[:], sqt[:, c, 0:n], c == 0, c == nchunk - 1, R=[C.ones_sb, sqt], W=[ps])
            P.ts(rs[:, 0:n], ps[:, 0:n], 1.0 / dim, EPS, ALU.mult, ALU.add, R=[ps], W=[rs])
            P.act(rs[:, 0:n], rs[:, 0:n], AF.Sqrt, R=[rs], W=[rs])
            P.op("dve", lambda e: e.reciprocal(out=rs[:, 0:n], in_=rs[:, 0:n]), R=[rs], W=[rs])
            for c in range(nchunk):
                P.stt(xt[:, c, c0:c0 + n], xt[:, c, c0:c0 + n], g[:, c:c + 1], rs[:, 0:n], ALU.mult, ALU.mult, R=[xt, g, rs], W=[xt])
    P.tt(krr[:, TC:TA], krr[:, TC:TA], CC, ALU.mult, R=[krr, rope], W=[krr])
    P.tt(krb[:, TC:TA], krb[:, TC:TA], SS, ALU.mult, R=[krb, rope], W=[krb])
    P.tt(krr[:, TC:TA], krr[:, TC:TA], krb[:, TC:TA], ALU.add, R=[krr, krb], W=[krr])
    ia = 0
    for h in range(8):
        for (c0, n) in col_blocks():
            ps = npx()
            for c in range(3):
                P.mm(ps[0:64, 0:n], wqn[:, c, h * 64:(h + 1) * 64], cqn[:, c, c0:c0 + n], c == 0, c == 2, R=[wqn, cqn], W=[ps])
            P.act(qn[:, c0:c0 + n], ps[0:64, 0:n], AF.Copy, R=[ps], W=[qn], scale=SCALE)
            ps = npx()
            for c in range(3):
                P.mm(ps[0:32, 0:n], wqa[:, c, h * 32:(h + 1) * 32], cqn[:, c, c0:c0 + n], c == 0, c == 2, R=[wqa, cqn], W=[ps])
            P.act(qr[:, c0:c0 + n], ps[0:32, 0:n], AF.Copy, R=[ps], W=[qr], scale=SCALE)
            ps = npx()
            for c in range(3):
                P.mm(ps[0:32, 0:n], wqb[:, c, h * 32:(h + 1) * 32], cqn[:, c, c0:c0 + n], c == 0, c == 2, R=[wqb, cqn], W=[ps])
            P.act(qrb[:, c0:c0 + n], ps[0:32, 0:n], AF.Copy, R=[ps], W=[qrb], scale=SCALE)
            ps = npx()
            for c in range(2):
                P.mm(ps[0:64, 0:n], wkn[:, c, h * 64:(h + 1) * 64], ckvn[:, c, c0:c0 + n], c == 0, c == 1, R=[wkn, ckvn], W=[ps])
            P.copy(kn[:, c0:c0 + n], ps[0:64, 0:n], R=[ps], W=[kn])
        P.tt(qr[:, TC:TA], qr[:, TC:TA], CC, ALU.mult, R=[qr, rope], W=[qr])
        P.tt(qrb[:, TC:TA], qrb[:, TC:TA], SS, ALU.mult, R=[qrb, rope], W=[qrb])
        P.tt(qr[:, TC:TA], qr[:, TC:TA], qrb[:, TC:TA], ALU.add, R=[qr, qrb], W=[qr])
        for t0 in range(0, 18, 8):
            nt = min(8, 18 - t0)
            ps = npx()
            for j in range(nt):
                for c in range(2):
                    P.mm(ps[:, j * 64:(j + 1) * 64], ckvn[:, c, (t0 + j) * 128:(t0 + j + 1) * 128], wv[:, c, h * 64:(h + 1) * 64], c == 0, c == 1, R=[wv, ckvn], W=[ps])
            P.copy(vh[:, t0:t0 + nt, :], ps[:, 0:nt * 64].rearrange("p (t f) -> p t f", f=64), R=[ps], W=[vh])
        qblocks = [(TC + i * 512, 512, 18) for i in range(4)]
        if ctx_out:
            qblocks.append((0, TC, 2))
        for (q0, n, nkt) in qblocks:
            po = pO[ia % 2]; pd = pD[ia % 2]; r = rd[ia % 2]
            ia += 1
            for kt in range(nkt):
                pa = pA[kt % 2]
                p = pt[kt % 3]
                P.mm(pa[:, 0:n], kn[:, kt * 128:(kt + 1) * 128], qn[:, q0:q0 + n], True, False, R=[kn, qn], W=[pa])
                P.mm(pa[:, 0:n], krr[:, kt * 128:(kt + 1) * 128], qr[:, q0:q0 + n], False, True, R=[krr, qr], W=[pa])
                P.act(p[:, 0:n], pa[:, 0:n], AF.Exp, R=[pa], W=[p])
                P.mm(po[:, 0:n], vh[:, kt, :], p[:, 0:n], kt == 0, kt == nkt - 1, R=[vh, p], W=[po])
                P.mm(pd[:, 0:n], C.ones_sb[:, 0:64], p[:, 0:n], kt == 0, kt == nkt - 1, R=[C.ones_sb, p], W=[pd])
            P.op("dve", lambda e: e.reciprocal(out=r[:, 0:n], in_=pd[:, 0:n]), R=[pd], W=[r])
            P.tt(oT[:, q0:q0 + n], po[:, 0:n], r[:, 0:n], ALU.mult, R=[po, r], W=[oT])
        c_lo = 0 if ctx_out else TC
        P.dma(C.br[b, 0, h * 64:(h + 1) * 64, c_lo:TA], oT[:, c_lo:TA], R=[oT], W=[], q="act")


def stage_swa(P, C, l, b, ctx_out):
    with P.scope():
        _stage_swa(P, C, l, b, ctx_out)


def _stage_swa(P, C, l, b, ctx_out):
    qA = P.sb("sqA", [64, 4, TA]); qB = P.sb("sqB", [64, 4, TA])
    kA = P.sb("skA", [64, TA]); kB = P.sb("skB", [64, TA])
    vn = P.sb("svn", [128, 18, 64])
    rope = P.sb("ropes", [64, 2, TL])
    msk = P.sb("swam", [128, 2, 128])
    snk = P.sb("snk", [64, 8])
    pt = [P.sb("spt%d" % i, [128, 4, 128]) for i in range(3)]
    rd = [P.sb("srd%d" % i, [64, 4, 128]) for i in range(2)]
    ot = [P.sb("sot%d" % i, [64, 4, 128]) for i in range(2)]
    pA = [P.ps("spA%d" % i, [128, 512]) for i in range(2)]
    pO = [P.ps("spO%d" % i, [64, 512]) for i in range(2)]
    pD = [P.ps("spD%d" % i, [64, 512]) for i in range(2)]
    P.dma(rope[:], C.ropeS[:, :, :].rearrange("a p t -> p a t"), R=[C.ropeS], W=[rope])
    P.dma(msk[:], C.swam[:, :, :], R=[C.swam], W=[msk])
    P.dma(snk[:], C.swa_sink[l, :].partition_broadcast(64), R=[C.swa_sink], W=[snk])
    P.act(snk[:], snk[:], AF.Exp, R=[snk], W=[snk])
    CC, SS = rope[:, 0, :], rope[:, 1, :]
    ib = 0
    ip = 0
    for n in range(2):
        for hh in range(4):
            r0 = FM_OFF["sqA"] + (4 * n + hh) * 64
            P.dma(qA[:, hh, :], C.proj[b, r0:r0 + 64, :], R=[], W=[qA])
            r0 = FM_OFF["sqB"] + (4 * n + hh) * 64
            P.dma(qB[:, hh, :], C.proj[b, r0:r0 + 64, :], R=[], W=[qB])
        P.dma(kA[:], C.proj[b, FM_OFF["skA"] + n * 64:FM_OFF["skA"] + (n + 1) * 64, :], R=[], W=[kA])
        P.dma(kB[:], C.proj[b, FM_OFF["skB"] + n * 64:FM_OFF["skB"] + (n + 1) * 64, :], R=[], W=[kB])
        P.dma(vn[:], C.projT[b, :, n * 64:(n + 1) * 64].rearrange("(t p) f -> p t f", p=128), R=[], W=[vn])
        P.tt(qA[:, :, TC:TA], qA[:, :, TC:TA], bc_mid(CC, 4), ALU.mult, R=[qA, rope], W=[qA])
        P.tt(qB[:, :, TC:TA], qB[:, :, TC:TA], bc_mid(SS, 4), ALU.mult, R=[qB, rope], W=[qB])
        P.tt(qA[:, :, TC:TA], qA[:, :, TC:TA], qB[:, :, TC:TA], ALU.add, R=[qA, qB], W=[qA])
        P.act(qA[:], qA[:], AF.Copy, R=[qA], W=[qA], scale=0.125)
        P.tt(kA[:, TC:TA], kA[:, TC:TA], CC, ALU.mult, R=[kA, rope], W=[kA])
        P.tt(kB[:, TC:TA], kB[:, TC:TA], SS, ALU.mult, R=[kB, rope], W=[kB])
        P.tt(kA[:, TC:TA], kA[:, TC:TA], kB[:, TC:TA], ALU.add, R=[kA, kB], W=[kA])
        qblocks = []
        if ctx_out:
            qblocks += [(0, -1), (128, -1)]
        qblocks += [(TC + i * 128, i) for i in range(16)]
        for (q0, i) in qblocks:
            tiles = [(0, None), (1, None)]
            if i >= 0:
                if i - 1 >= 0:
                    tiles.append((2 + i - 1, 0))
                tiles.append((2 + i, None))
                if i + 1 <= 15:
                    tiles.append((2 + i + 1, 1))
            po = pO[ib % 2]; pd = pD[ib % 2]; r = rd[ib % 2]; o = ot[ib % 2]
            ib += 1
            for ti, (kt, mi) in enumerate(tiles):
                pa = pA[ip % 2]; p = pt[ip % 3]
                ip += 1
                P.mm(pa[:].rearrange("p (h q) -> p h q", h=4), kA[:, kt * 128:(kt + 1) * 128], qA[:, :, q0:q0 + 128], True, True, R=[kA, qA], W=[pa])
                P.act(p[:], pa[:].rearrange("p (h q) -> p h q", h=4), AF.Exp, R=[pa], W=[p])
                if mi is not None:
                    P.tt(p[:], p[:], bc_mid(msk[:, mi, :], 4), ALU.mult, R=[p, msk], W=[p])
                pf = p[:].rearrange("p h q -> p (h q)")
                P.mm(po[:, :], vn[:, kt, :], pf, ti == 0, ti == len(tiles) - 1, R=[vn, p], W=[po])
                P.mm(pd[:, :], C.ones_sb[:, 0:64], pf, ti == 0, ti == len(tiles) - 1, R=[C.ones_sb, p], W=[pd])
            P.tt(r[:], pd[:].rearrange("p (h q) -> p h q", h=4), bc_last(snk[:, 4 * n:4 * n + 4], 128), ALU.add, R=[pd, snk], W=[r])
            P.op("dve", lambda e: e.reciprocal(out=r[:], in_=r[:]), R=[r], W=[r])
            P.tt(o[:], po[:].rearrange("p (h q) -> p h q", h=4), r[:], ALU.mult, R=[po, r], W=[o])
            P.dma(C.br[b, 1, n * 256:(n + 1) * 256, q0:q0 + 128].rearrange("(h f) t -> f h t", h=4), o[:], R=[o], W=[], q="act")


def declare_rest(P, C):
    nc = P.nc
    def inp(name, shape):
        return T(nc.dram_tensor(name, list(shape), F32, kind="ExternalInput"))
    C.w_branch = inp("w_branch", [DEPTH, 3, 512, D])
    C.w_out = inp("w_out", [DEPTH, D, D])
    C.router_w = inp("router_w", [DEPTH, D, 32])
    C.router_b = inp("router_b", [DEPTH, 32])
    C.exp_w_gu = inp("exp_w_gu", [DEPTH, 32, D, 2048])
    C.bgu = inp("bgu", [DEPTH, 128, 32, 2, 8])
    C.exp_w_dn = inp("exp_w_dn", [DEPTH, 32, D, D])
    C.exp_b_dn = inp("exp_b_dn", [DEPTH, 32, D])
    C.final_norm_g = inp("final_norm_g", [D])
    C.xres = P.dram("xres", [NB, TA, D])
    C.out = T(nc.dram_tensor("out", [NB, TL, D], F32, kind="ExternalOutput"))


def host_rest(I):
    f = lambda a: np.ascontiguousarray(np.asarray(a, dtype=np.float32))
    L = DEPTH
    bgu = np.asarray(I["exp_b_gu"]).reshape(L, 32, 8, 128, 2).transpose(0, 3, 1, 4, 2)
    return dict(w_branch=f(I["w_branch"]), w_out=f(I["w_out"]), router_w=f(I["router_w"]), router_b=f(I["router_b"]),
                exp_w_gu=f(I["exp_w_gu"]), bgu=f(bgu), exp_w_dn=f(I["exp_w_dn"]), exp_b_dn=f(I["exp_b_dn"]),
                final_norm_g=f(I["final_norm_g"]))


def stage_merge(P, C, l, b, ctx_out):
    with P.scope():
        _stage_merge(P, C, l, b, ctx_out)


def _stage_merge(P, C, l, b, ctx_out):
    wbr = P.sb("wbr", [128, 3, 4, D]); wout = P.sb("wout", [128, 8, D])
    brt = P.sb("brt", [128, 3, 4, 512])
    gch = [P.sb("gch%d" % i, [128, 512]) for i in range(3)]
    tmp = [P.sb("mtmp%d" % i, [128, 512]) for i in range(2)]
    yT = P.sb("yT", [128, 8, 512])
    g1 = [P.sb("g1bc%d" % i, [128, D]) for i in range(2)]
    xt = [P.sb("mxt%d" % i, [128, D]) for i in range(2)]
    pA = [P.ps("mpA%d" % i, [128, 512]) for i in range(3)]
    pB = [P.ps("mpB%d" % i, [128, 512]) for i in range(2)]
    P.dma(wbr[:].rearrange("p i c n -> p (i c) n"), C.w_branch.t[l].rearrange("i (c p) n -> p (i c) n", p=128), R=[C.w_branch], W=[wbr])
    P.dma(wout[:], C.w_out.t[l].rearrange("(c p) n -> p c n", p=128), R=[C.w_out], W=[wout])
    load_mod_bc(P, C, l, b, (2,), (g1[0],))
    load_mod_bc(P, C, l, 2, (2,), (g1[1],))
    ig = 0
    ix = 0
    for (t0, n) in blocks():
        if t0 < TC and not ctx_out:
            continue
        gbc = g1[1] if t0 < TC else g1[0]
        for i in range(3):
            P.dma(brt[:, i, :, 0:n], C.br[b, i, :, t0:t0 + n].rearrange("(c p) t -> p c t", p=128), R=[], W=[brt])
        for m in range(8):
            for i in range(3):
                gc = gch[ig % 3]; pa = pA[ig % 3]; tm = tmp[ig % 2]
                ig += 1
                r0 = FM_OFF["gate"] + i * D + m * 128
                P.dma(gc[:, 0:n], C.proj[b, r0:r0 + 128, t0:t0 + n], R=[], W=[gc])
                for c in range(4):
                    P.mm(pa[:, 0:n], wbr[:, i, c, m * 128:(m + 1) * 128], brt[:, i, c, 0:n], c == 0, c == 3, R=[wbr, brt], W=[pa])
                P.act(gc[:, 0:n], gc[:, 0:n], AF.Sigmoid, R=[gc], W=[gc])
                if i == 0:
                    P.tt(yT[:, m, 0:n], pa[:, 0:n], gc[:, 0:n], ALU.mult, R=[pa, gc], W=[yT])
                else:
                    P.tt(tm[:, 0:n], pa[:, 0:n], gc[:, 0:n], ALU.mult, R=[pa, gc], W=[tm])
                    P.tt(yT[:, m, 0:n], yT[:, m, 0:n], tm[:, 0:n], ALU.add, R=[yT, tm], W=[yT])
        for i in range(n // 128):
            x = xt[ix % 2]
            src_t, src_ap = token_src(C, l, b, t0 + i * 128, 128)
            P.dma(x[:], src_ap, R=[], W=[x])
            for half in range(2):
                pb = pB[(2 * ix + half) % 2]
                for m in range(8):
                    P.mm(pb[:, :], yT[:, m, i * 128:(i + 1) * 128], wout[:, m, half * 512:(half + 1) * 512], m == 0, m == 7, R=[yT, wout], W=[pb])
                tm = tmp[half]
                P.tt(tm[:], pb[:, :], gbc[:, half * 512:(half + 1) * 512], ALU.mult, R=[pb, gbc], W=[tm])
                P.tt(x[:, half * 512:(half + 1) * 512], x[:, half * 512:(half + 1) * 512], tm[:], ALU.add, R=[x, tm], W=[x])
            P.dma(C.xres[b, t0 + i * 128:t0 + (i + 1) * 128, :], x[:], R=[x], W=[], q="act")
            ix += 1


def stage_moe(P, C, l, b, ctx_out, n_exp=32):
    with P.scope():
        _stage_moe(P, C, l, b, ctx_out, n_exp)


def _stage_moe(P, C, l, b, ctx_out, n_exp):
    C.xt = [P.sb("xt%d" % i, [128, D]) for i in range(2)]
    C.ht = [P.sb("ht%d" % i, [128, D]) for i in range(2)]
    C.st = [P.sb("st%d" % i, [128, 4]) for i in range(2)]
    C.ps_tr = [P.ps("ps_tr%d" % i, [128, 1024]) for i in range(1)] * 2
    hT = P.sb("mhT", [128, 8, 512])
    A2 = [P.sb("A2bc%d" % i, [128, D]) for i in range(2)]
    sh2 = [P.sb("sh2bc%d" % i, [128, D]) for i in range(2)]
    g2 = [P.sb("g2bc%d" % i, [128, D]) for i in range(2)]
    rw = P.sb("rw", [128, 8, 32]); rb = P.sb("rb", [128, 32])
    bgu = P.sb("bgu", [128, 32, 2, 8]); bdn = P.sb("bdn", [32, D])
    lg = P.sb("lg", [128, 32]); ex = P.sb("ex", [128, 32]); mk = P.sb("rmk", [128, 32]); t8 = P.sb("t8", [128, 8]); sc = P.sb("rsc", [128, 4])
    G = P.sb("G", [128, 4, 32]); GT = P.sb("GT", [32, 512])
    wq = [P.sb("wq%d" % i, [128, 8, 512]) for i in range(3)]
    wd = [P.sb("wd%d" % i, [128, 8, 512]) for i in range(2)]
    actT = P.sb("actT", [128, 8, 512])
    acc = P.sb("acc", [128, 4, D])
    gs = [P.sb("gs%d" % i, [128, 512]) for i in range(2)]
    us = [P.sb("us%d" % i, [128, 512]) for i in range(2)]
    sg = [P.sb("sg%d" % i, [128, 512]) for i in range(2)]
    pR = P.ps("pR", [128, 512])
    pG = [P.ps("pG%d" % i, [128, 512]) for i in range(2)]
    pU = [P.ps("pU%d" % i, [128, 512]) for i in range(2)]
    pY = [P.ps("pY%d" % i, [128, 512]) for i in range(1)]
    load_mod_bc(P, C, l, b, (4, 3, 5), (A2[0], sh2[0], g2[0]))
    load_mod_bc(P, C, l, 2, (4, 3, 5), (A2[1], sh2[1], g2[1]))
    P.dma(rw[:], C.router_w.t[l].rearrange("(c p) n -> p c n", p=128), R=[C.router_w], W=[rw])
    P.dma(rb[:], C.router_b[l, :].partition_broadcast(128), R=[C.router_b], W=[rb])
    P.dma(bgu[:], C.bgu[l], R=[C.bgu], W=[bgu])
    P.dma(bdn[:], C.exp_b_dn[l], R=[C.exp_b_dn], W=[bdn])
    it = 0
    iq = 0
    idn = 0
    ie = 0
    for (t0, n) in blocks():
        if t0 < TC and not ctx_out:
            continue
        r = 1 if t0 < TC else 0
        nt = n // 128
        for i in range(nt):
            norm_mod_T(P, C, C.xres, C.xres[b, t0 + i * 128:t0 + (i + 1) * 128, :], A2[r], sh2[r], hT, i * 128, it)
            it += 1
        for i in range(nt):
            for c in range(8):
                P.mm(pR[:, 0:32], hT[:, c, i * 128:(i + 1) * 128], rw[:, c, :], c == 0, c == 7, R=[hT, rw], W=[pR])
            P.tt(lg[:], pR[:, 0:32], rb[:], ALU.add, R=[pR, rb], W=[lg])
            P.op("dve", lambda e: e.max(out=t8[:], in_=lg[:]), R=[lg], W=[t8])
            P.ts(mk[:], lg[:], t8[:, 3:4], None, ALU.is_ge, ALU.bypass, R=[lg, t8], W=[mk])
            P.ts(sc[:, 0:1], t8[:, 0:1], -1.0, None, ALU.mult, ALU.bypass, R=[t8], W=[sc])
            P.act(ex[:], lg[:], AF.Exp, R=[lg, sc], W=[ex], bias=sc[:, 0:1], scale=1.0)
            P.tt(ex[:], ex[:], mk[:], ALU.mult, R=[ex, mk], W=[ex])
            P.op("dve", lambda e: e.tensor_reduce(out=sc[:, 1:2], in_=ex[:], axis=AX.X, op=ALU.add), R=[ex], W=[sc])
            P.op("dve", lambda e: e.reciprocal(out=sc[:, 2:3], in_=sc[:, 1:2]), R=[sc], W=[sc])
            P.ts(G[:, i, :], ex[:], sc[:, 2:3], None, ALU.mult, ALU.bypass, R=[ex, sc], W=[G])
            P.tr(pR[0:32, 128:256], G[:, i, :], C.ident_sb[:], R=[G, C.ident_sb], W=[pR])
            P.copy(GT[:, i * 128:(i + 1) * 128], pR[0:32, 128:256], R=[pR], W=[GT])
        for i in range(nt):
            for half in range(2):
                P.mm(pR[:, :], GT[:, i * 128:(i + 1) * 128], bdn[:, half * 512:(half + 1) * 512], True, True, R=[GT, bdn], W=[pR])
                P.copy(acc[:, i, half * 512:(half + 1) * 512], pR[:, :], R=[pR], W=[acc])
        for e in range(n_exp):
            wgv = C.exp_w_gu.t[l, e].rearrange("(c p) n -> p c n", p=128)
            for qd in range(4):
                w = wq[iq % 3]
                iq += 1
                P.dma(w[:], wgv[:, :, qd * 512:(qd + 1) * 512], R=[C.exp_w_gu], W=[w])
                for s in range(2):
                    fc = qd * 2 + s
                    pg = pG[ie % 2]; pu = pU[ie % 2]; g_ = gs[ie % 2]; u_ = us[ie % 2]; s_ = sg[ie % 2]
                    ie += 1
                    for c in range(8):
                        P.mm(pg[:, 0:n], w[:, c, s * 256:(s + 1) * 256:2], hT[:, c, 0:n], c == 0, c == 7, R=[w, hT], W=[pg])
                    for c in range(8):
                        P.mm(pu[:, 0:n], w[:, c, s * 256 + 1:(s + 1) * 256:2], hT[:, c, 0:n], c == 0, c == 7, R=[w, hT], W=[pu])
                    P.ts(g_[:, 0:n], pg[:, 0:n], bgu[:, e, 0, fc:fc + 1], 7.0, ALU.add, ALU.min, R=[pg, bgu], W=[g_])
                    P.ts(u_[:, 0:n], pu[:, 0:n], bgu[:, e, 1, fc:fc + 1], 7.0, ALU.add, ALU.min, R=[pu, bgu], W=[u_])
                    P.ts(u_[:, 0:n], u_[:, 0:n], -7.0, 1.0, ALU.max, ALU.add, R=[u_], W=[u_])
                    P.act(s_[:, 0:n], g_[:, 0:n], AF.Sigmoid, R=[g_], W=[s_], scale=1.702)
                    P.tt(g_[:, 0:n], g_[:, 0:n], s_[:, 0:n], ALU.mult, R=[g_, s_], W=[g_], en="pool")
                    P.tt(actT[:, fc, 0:n], g_[:, 0:n], u_[:, 0:n], ALU.mult, R=[g_, u_], W=[actT], en="pool")
            wdv = C.exp_w_dn.t[l, e].rearrange("(c p) n -> p c n", p=128)
            for half in range(2):
                w = wd[idn % 2]
                idn += 1
                P.dma(w[:], wdv[:, :, half * 512:(half + 1) * 512], R=[C.exp_w_dn], W=[w])
                for i in range(nt):
                    py = pY[0]
                    for fc in range(8):
                        P.mm(py[:, :], actT[:, fc, i * 128:(i + 1) * 128], w[:, fc, :], fc == 0, fc == 7, R=[actT, w], W=[py])
                    a = acc[:, i, half * 512:(half + 1) * 512]
                    P.stt(a, py[:, :], G[:, i, e:e + 1], a, ALU.mult, ALU.add, R=[py, G, acc], W=[acc])
        for i in range(nt):
            x = C.xt[i % 2]
            rows = slice(t0 + i * 128, t0 + (i + 1) * 128)
            P.dma(x[:], C.xres[b, rows, :], R=[], W=[x])
            P.tt(acc[:, i, :], acc[:, i, :], g2[r][:], ALU.mult, R=[acc, g2[r]], W=[acc])
            P.tt(x[:], x[:], acc[:, i, :], ALU.add, R=[x, acc], W=[x])
            P.dma(C.xres[b, rows, :], x[:], R=[x], W=[], q="act")


def stage_final(P, C, b):
    with P.scope():
        xt = [P.sb("fxt%d" % i, [128, D]) for i in range(2)]
        ht = [P.sb("fht%d" % i, [128, D]) for i in range(2)]
        st = [P.sb("fst%d" % i, [128, 4]) for i in range(2)]
        g = P.sb("fg", [128, D])
        P.dma(g[:], C.final_norm_g[:].partition_broadcast(128), R=[C.final_norm_g], W=[g])
        for i in range(TL // 128):
            x = xt[i % 2]; h = ht[i % 2]; s = st[i % 2]
            P.dma(x[:], C.xres[b, TC + i * 128:TC + (i + 1) * 128, :], R=[], W=[x])
            P.act(h[:], x[:], AF.Square, R=[x], W=[h, s], accum_out=s[:, 0:1])
            P.ts(s[:, 1:2], s[:, 0:1], 1.0 / D, EPS, ALU.mult, ALU.add, R=[s], W=[s])
            P.act(s[:, 2:3], s[:, 1:2], AF.Sqrt, R=[s], W=[s])
            P.op("dve", lambda e: e.reciprocal(out=s[:, 3:4], in_=s[:, 2:3]), R=[s], W=[s])
            P.stt(h[:], x[:], s[:, 3:4], g[:], ALU.mult, ALU.mult, R=[x, s, g], W=[h])
            P.dma(C.out[b, i * 128:(i + 1) * 128, :], h[:], R=[h], W=[], q="act", is_output=True)


def build_program(debug=False):
    P = Prog(debug=debug)
    C = Ctx()
    declare_io(P, C); declare_dn(P, C); declare_attn(P, C); declare_rest(P, C)
    stage_consts(P, C)
    for l in range(DEPTH):
        ctx_out = l < DEPTH - 1
        stage_mod(P, C, l)
        stage_proj(P, C, l)
        for b in range(NB):
            stage_mla(P, C, l, b, ctx_out)
            stage_swa(P, C, l, b, ctx_out)
            stage_dn_prep(P, C, l, b)
            stage_dn_scan(P, C, l, b, ctx_out)
            stage_dn_out(P, C, l, b, ctx_out)
            stage_merge(P, C, l, b, ctx_out)
            stage_moe(P, C, l, b, ctx_out)
    for b in range(NB):
        stage_final(P, C, b)
    P.finish()
    return P, C


_CACHE = {}


def kernel(**inputs):
    I = {k: np.asarray(v) for k, v in inputs.items()}
    if "prog" not in _CACHE:
        _CACHE["prog"] = build_program()
    P, C = _CACHE["prog"]
    sh = host_shared(I)
    sh.update(host_rest(I))
    in_maps = []
    for core in range(NCORES):
        m = dict(sh)
        m.update(host_core(I, core))
        in_maps.append(m)
    res = run_bass_kernel_spmd(P.nc, in_maps, core_ids=list(range(NCORES)))
    out = np.concatenate([np.asarray(r["out"]) for r in res.results], axis=0)
    return out.astype(np.float32)
```

```python
import numpy as np
import concourse.bass as bass
import concourse.mybir as mybir
from concourse.bass_utils import run_bass_kernel_spmd
from concourse.alu_op_type import AluOpType as ALU

F32 = mybir.dt.float32
BF16 = mybir.dt.bfloat16
AF = mybir.ActivationFunctionType
AX = mybir.AxisListType

D = 1024
DEPTH = 2
NB = 2
TC = 256
TL = 2048
TA = TC + TL
EPS = 1e-6
NCORES = 8


class Buf:
    __slots__ = ("w", "r")

    def __init__(self):
        self.w = None
        self.r = []


class T:
    def __init__(self, t, psum=False):
        self.t = t
        self.b = Buf()
        self.psum = psum

    def __getitem__(self, k):
        return self.t[k]


class Prog:
    def __init__(self, debug=False, as_input=()):
        self.as_input = set(as_input)
        self.nc = bass.Bass("TRN2", target_bir_lowering=False)
        nc = self.nc
        self.debug = debug
        self.eng = {"pe": nc.tensor, "act": nc.scalar, "dve": nc.vector, "pool": nc.gpsimd, "sp": nc.sync}
        self.esem = {k: nc.alloc_semaphore("sem_" + k) for k in ("pe", "act", "dve", "pool")}
        self.ecnt = {k: 0 for k in self.esem}
        self.waited = {k: {} for k in self.eng}
        self.dsems = [nc.alloc_semaphore("dsem%d" % i) for i in range(48)]
        self.dcnt = [0] * len(self.dsems)
        self.dnext = 0
        self.semid = {}
        self.n_inst = 0
        self.out_events = []
        self.scopes = []
        self.uid = 0

    def sb(self, name, shape, dt=F32):
        self.uid += 1
        name = "%s_%d" % (name, self.uid)
        if self.scopes:
            return T(self.scopes[-1].enter_context(self.nc.sbuf_tensor(name, list(shape), dt)))
        return T(self.nc.alloc_sbuf_tensor(name, list(shape), dt))

    def ps(self, name, shape, dt=F32):
        self.uid += 1
        name = "%s_%d" % (name, self.uid)
        if self.scopes:
            return T(self.scopes[-1].enter_context(self.nc.psum_tensor(name, list(shape), dt)), psum=True)
        return T(self.nc.alloc_psum_tensor(name, list(shape), dt), psum=True)

    def scope(self):
        return _Scope(self)

    def barrier(self):
        evs = [(self.dsems[i], self.dcnt[i]) for i in range(len(self.dsems)) if self.dcnt[i] > 0]
        evs += [(self.esem[k], self.ecnt[k]) for k in self.esem if self.ecnt[k] > 0]
        for en in self.eng:
            self._wait(en, evs)

    def dram(self, name, shape, dt=F32, kind="Internal"):
        if self.debug and kind == "Internal":
            kind = "ExternalInput" if name in self.as_input else "ExternalOutput"
        return T(self.nc.dram_tensor(name, list(shape), dt, kind=kind))

    def _wait(self, en, evs):
        e = self.eng[en]
        w = self.waited[en]
        best = {}
        for ev in evs:
            if ev is None:
                continue
            sem, val = ev
            k = id(sem)
            if w.get(k, 0) >= val:
                continue
            if k not in best or best[k][1] < val:
                best[k] = (sem, val)
        for k, (sem, val) in best.items():
            e.wait_ge(sem, val)
            w[k] = val

    def _deps(self, en, R, W):
        evs = []
        for b in R:
            evs.append(b.b.w)
            if b.psum:
                own = self.esem.get(en)
                evs.extend(ev for ev in b.b.r if ev[0] is not own)
        for b in W:
            evs.append(b.b.w)
            evs.extend(b.b.r)
        if en == "pe":
            s = self.esem["pe"]
            evs = [ev for ev in evs if ev is not None and ev[0] is not s]
        return evs

    def _commit(self, ev, R, W):
        for b in R:
            b.b.r.append(ev)
            if len(b.b.r) > 6:
                d = {}
                for s, v in b.b.r:
                    if id(s) not in d or d[id(s)][1] < v:
                        d[id(s)] = (s, v)
                b.b.r = list(d.values())
        for b in W:
            b.b.w = ev
            b.b.r = []

    def op(self, en, fn, R=(), W=()):
        self._wait(en, self._deps(en, R, W))
        inst = fn(self.eng[en])
        self.ecnt[en] += 1
        inst.then_inc(self.esem[en], 1)
        self._commit((self.esem[en], self.ecnt[en]), R, W)
        self.n_inst += 1
        return inst

    def dma(self, out, in_, R=(), W=(), q="sp", is_output=False, **kw):
        i = self.dnext
        self.dnext = (self.dnext + 1) % len(self.dsems)
        sem = self.dsems[i]
        evs = self._deps(q, R, W)
        if self.dcnt[i] > 0:
            evs.append((sem, self.dcnt[i]))
        self._wait(q, evs)
        inst = self.eng[q].dma_start(out=out, in_=in_, **kw)
        self.dcnt[i] += 16
        inst.then_inc(sem, 16)
        ev = (sem, self.dcnt[i])
        self._commit(ev, R, W)
        if is_output:
            self.out_events.append(ev)
        self.n_inst += 1
        return inst

    def finish(self):
        evs = [(self.dsems[i], self.dcnt[i]) for i in range(len(self.dsems)) if self.dcnt[i] > 0]
        evs += [(self.esem[k], self.ecnt[k]) for k in self.esem if self.ecnt[k] > 0]
        self._wait("sp", evs)

    def mm(self, out, lhsT, rhs, start, stop, R, W):
        return self.op("pe", lambda e: e.matmul(out, lhsT, rhs, start=start, stop=stop), R, W)

    def tr(self, out, in_, ident, R, W):
        return self.op("pe", lambda e: e.transpose(out, in_, ident), R, W)

    def act(self, out, in_, func, R, W, en="act", **kw):
        return self.op(en, lambda e: e.activation(out=out, in_=in_, func=func, **kw), R, W)

    def tt(self, out, in0, in1, op, R, W, en="dve"):
        return self.op(en, lambda e: e.tensor_tensor(out=out, in0=in0, in1=in1, op=op), R, W)

    def ts(self, out, in0, s1, s2, op0, op1, R, W, en="dve"):
        return self.op(en, lambda e: e.tensor_scalar(out=out, in0=in0, scalar1=s1, scalar2=s2, op0=op0, op1=op1), R, W)

    def stt(self, out, in0, scalar, in1, op0, op1, R, W):
        return self.op("dve", lambda e: e.scalar_tensor_tensor(out=out, in0=in0, scalar=scalar, in1=in1, op0=op0, op1=op1), R, W)

    def copy(self, out, in_, R, W, en="dve"):
        if en == "act":
            return self.op("act", lambda e: e.copy(out=out, in_=in_), R, W)
        return self.op(en, lambda e: e.tensor_copy(out=out, in_=in_), R, W)


class _Scope:
    def __init__(self, P):
        self.P = P

    def __enter__(self):
        import contextlib
        self.es = contextlib.ExitStack()
        self.P.scopes.append(self.es)
        return self

    def __exit__(self, *a):
        self.P.barrier()
        self.P.scopes.pop()
        self.es.close()
        return False


NFM = 6592
NTM = 672
FM_OFF = dict(cq=0, ckv=384, sqA=640, sqB=1152, skA=1664, skB=1792, dqkv=1920, gate=3456, krA=6528, krB=6560)
TM_OFF = dict(sv=0, dz=128, dbeta=640, da=656)
IN_OFF = dict(cq=0, ckv=384, kr=640, sq=672, sk=1184, sv=1312, dqkv=1440, dz=2976, dbeta=3488, da=3504, gate=3520)


def host_w_in_layout(w_in):
    o = IN_OFF
    idx = []
    idx += list(range(o["cq"], o["cq"] + 384))
    idx += list(range(o["ckv"], o["ckv"] + 256))
    idx += list(range(o["sq"], o["sq"] + 512))
    for h in range(8):
        b = o["sq"] + 64 * h
        idx += list(range(b + 32, b + 64)) + list(range(b, b + 32))
    idx += list(range(o["sk"], o["sk"] + 128))
    for h in range(2):
        b = o["sk"] + 64 * h
        idx += list(range(b + 32, b + 64)) + list(range(b, b + 32))
    idx += list(range(o["dqkv"], o["dqkv"] + 1536))
    idx += list(range(o["gate"], o["gate"] + 3072))
    idx += list(range(o["kr"], o["kr"] + 32))
    idx += list(range(o["kr"] + 16, o["kr"] + 32)) + list(range(o["kr"], o["kr"] + 16))
    assert len(idx) == NFM
    w_fm = np.ascontiguousarray(w_in[:, :, idx])
    idt = list(range(o["sv"], o["sv"] + 128)) + list(range(o["dz"], o["dz"] + 512)) + list(range(o["dbeta"], o["dbeta"] + 32))
    w_tm = np.ascontiguousarray(w_in[:, :, idt])
    return w_fm, w_tm


class Ctx:
    pass


def declare_io(P, C):
    nc = P.nc
    def inp(name, shape):
        return T(nc.dram_tensor(name, list(shape), F32, kind="ExternalInput"))
    C.x = inp("x", [NB, TL, D])
    C.ctx = inp("ctx", [NB, TC, D])
    C.cT = inp("cT", [128, 8, 3])
    C.w_mod = inp("w_mod", [DEPTH, D, 6 * D])
    C.b_mod = inp("b_mod", [DEPTH, 6 * D])
    C.norm1_g = inp("norm1_g", [DEPTH, D])
    C.norm2_g = inp("norm2_g", [DEPTH, D])
    C.w_fm = inp("w_fm", [DEPTH, D, NFM])
    C.w_tm = inp("w_tm", [DEPTH, D, NTM])
    C.ident = inp("ident", [128, 128])
    C.mod = P.dram("mod", [DEPTH, 3, 6 * D])
    C.proj = P.dram("proj", [NB, NFM, TA])
    C.projT = P.dram("projT", [NB, TA, NTM])


def stage_consts(P, C):
    C.ident_sb = P.sb("ident_sb", [128, 128])
    P.dma(C.ident_sb[:], C.ident[:, :], R=[C.ident], W=[C.ident_sb])
    C.ones_sb = P.sb("ones_sb", [128, 128])
    P.op("dve", lambda e: e.memset(C.ones_sb[:], 1.0), W=[C.ones_sb])


def stage_mod(P, C, l):
    with P.scope():
        _stage_mod(P, C, l)


def _stage_mod(P, C, l):
    C.scT = P.sb("scT", [128, 8, 3])
    C.modrow = P.sb("modrow", [3, 6 * D])
    C.wm = [P.sb("wm%d" % i, [128, 8, 512]) for i in range(2)]
    C.bm = P.sb("bm", [3, 6 * D])
    C.gbc = P.sb("gbc", [3, 2, D])
    C.ps_mod = [P.ps("ps_mod%d" % i, [128, 512]) for i in range(2)]
    P.dma(C.scT[:], C.cT[:, :, :], R=[C.cT], W=[C.scT])
    P.act(C.scT[:], C.scT[:], AF.Silu, R=[C.scT], W=[C.scT])
    P.dma(C.bm[:], C.b_mod[l, :].partition_broadcast(3), R=[C.b_mod], W=[C.bm])
    P.dma(C.gbc[:, 0, :], C.norm1_g[l, :].partition_broadcast(3), R=[C.norm1_g], W=[C.gbc])
    P.dma(C.gbc[:, 1, :], C.norm2_g[l, :].partition_broadcast(3), R=[C.norm2_g], W=[C.gbc])
    wv = C.w_mod.t[l].rearrange("(c p) n -> p c n", p=128)
    for j in range(12):
        wt = C.wm[j % 2]
        ps = C.ps_mod[j % 2]
        P.dma(wt[:], wv[:, :, j * 512:(j + 1) * 512], R=[C.w_mod], W=[wt])
        for c in range(8):
            P.mm(ps[0:3, :], C.scT[:, c, :], wt[:, c, :], c == 0, c == 7, R=[C.scT, wt], W=[ps])
        P.tt(C.modrow[:, j * 512:(j + 1) * 512], ps[0:3, :], C.bm[:, j * 512:(j + 1) * 512], ALU.add, R=[ps, C.bm], W=[C.modrow])
    for slot, gi in ((1, 0), (4, 1)):
        sl = C.modrow[:, slot * D:(slot + 1) * D]
        P.stt(sl, sl, 1.0, C.gbc[:, gi, :], ALU.add, ALU.mult, R=[C.modrow, C.gbc], W=[C.modrow])
    P.dma(C.mod[l], C.modrow[:], R=[C.modrow], W=[C.mod])


def token_src(C, l, b, t0, n):
    if l == 0:
        if t0 < TC:
            return C.ctx, C.ctx[b, t0:t0 + n, :]
        return C.x, C.x[b, t0 - TC:t0 - TC + n, :]
    return C.xres, C.xres[b, t0:t0 + n, :]


def blocks():
    out = [(0, TC)]
    for i in range(TL // 512):
        out.append((TC + i * 512, 512))
    return out


def load_mod_bc(P, C, l, row, slots, dst):
    for s, d in zip(slots, dst):
        P.dma(d[:], C.mod[l, row, s * D:(s + 1) * D].partition_broadcast(128), R=[C.mod], W=[d])


def norm_mod_T(P, C, src_t, src_ap, A_bc, sh_bc, hT, col0, it):
    xt = C.xt[it % 2]
    ht = C.ht[it % 2]
    st = C.st[it % 2]
    ps = C.ps_tr[it % 2]
    P.dma(xt[:], src_ap, R=[src_t], W=[xt])
    P.act(ht[:], xt[:], AF.Square, R=[xt], W=[ht, st], accum_out=st[:, 0:1])
    P.ts(st[:, 1:2], st[:, 0:1], 1.0 / D, EPS, ALU.mult, ALU.add, R=[st], W=[st])
    P.act(st[:, 2:3], st[:, 1:2], AF.Sqrt, R=[st], W=[st])
    P.op("dve", lambda e: e.reciprocal(out=st[:, 3:4], in_=st[:, 2:3]), R=[st], W=[st])
    P.stt(ht[:], xt[:], st[:, 3:4], A_bc[:], ALU.mult, ALU.mult, R=[xt, st, A_bc], W=[ht])
    P.tt(ht[:], ht[:], sh_bc[:], ALU.add, R=[ht, sh_bc], W=[ht])
    for c in range(8):
        P.tr(ps[:, c * 128:(c + 1) * 128], ht[:, c * 128:(c + 1) * 128], C.ident_sb[:], R=[ht, C.ident_sb], W=[ps])
    P.copy(hT[:, :, col0:col0 + 128], ps[:].rearrange("p (c t) -> p c t", c=8), R=[ps], W=[hT], en="act" if it % 2 else "dve")


def stage_proj(P, C, l, do_blocks=None):
    with P.scope():
        _stage_proj(P, C, l, do_blocks)


def _stage_proj(P, C, l, do_blocks=None):
    C.xt = [P.sb("xt%d" % i, [128, D]) for i in range(2)]
    C.ht = [P.sb("ht%d" % i, [128, D]) for i in range(2)]
    C.st = [P.sb("st%d" % i, [128, 4]) for i in range(2)]
    C.ps_tr = [P.ps("ps_tr%d" % i, [128, 1024]) for i in range(2)]
    C.hT = [P.sb("hT%d" % i, [128, 8, 512]) for i in range(2)]
    C.A_bc = [P.sb("A_bc%d" % i, [128, D]) for i in range(3)]
    C.sh_bc = [P.sb("sh_bc%d" % i, [128, D]) for i in range(3)]
    C.wfm = [P.sb("wfm%d" % i, [128, 8, 512]) for i in range(2)]
    C.wtm = P.sb("wtm", [128, 8, NTM])
    C.ps_mm = [P.ps("ps_mm%d" % i, [128, 512]) for i in range(2)]
    C.ot = [P.sb("ot%d" % i, [128, 512]) for i in range(3)]
    for row in range(3):
        load_mod_bc(P, C, l, row, (1, 0), (C.A_bc[row], C.sh_bc[row]))
    P.dma(C.wtm[:], C.w_tm.t[l].rearrange("(c p) n -> p c n", p=128), R=[C.w_tm], W=[C.wtm])
    wv = C.w_fm.t[l].rearrange("(c p) n -> p c n", p=128)
    it = 0
    ib = 0
    ig = 0
    io = 0
    for b in range(NB):
        for (t0, n) in blocks():
            if do_blocks is not None and (b, t0) not in do_blocks:
                continue
            row = 2 if t0 < TC else b
            hT = C.hT[ib % 2]
            ib += 1
            for i in range(n // 128):
                src_t, src_ap = token_src(C, l, b, t0 + i * 128, 128)
                norm_mod_T(P, C, src_t, src_ap, C.A_bc[row], C.sh_bc[row], hT, i * 128, it)
                it += 1
            for g in range((NFM + 511) // 512):
                c0 = g * 512
                cw = min(512, NFM - c0)
                wt = C.wfm[ig % 2]
                ig += 1
                P.dma(wt[:, :, 0:cw], wv[:, :, c0:c0 + cw], R=[C.w_fm], W=[wt])
                for j in range((cw + 127) // 128):
                    m = min(128, cw - j * 128)
                    ps = C.ps_mm[io % 2]
                    ot = C.ot[io % 3]
                    for c in range(8):
                        P.mm(ps[0:m, 0:n], wt[:, c, j * 128:j * 128 + m], hT[:, c, 0:n], c == 0, c == 7, R=[wt, hT], W=[ps])
                    P.copy(ot[0:m, 0:n], ps[0:m, 0:n], R=[ps], W=[ot], en="act" if io % 2 else "dve")
                    r0 = c0 + j * 128
                    P.dma(C.proj[b, r0:r0 + m, t0:t0 + n], ot[0:m, 0:n], R=[ot], W=[], q="act")
                    io += 1
            for i in range(n // 128):
                for (q0, qw) in ((0, 512), (512, NTM - 512)):
                    ps = C.ps_mm[io % 2]
                    ot = C.ot[io % 3]
                    for c in range(8):
                        P.mm(ps[:, 0:qw], hT[:, c, i * 128:(i + 1) * 128], C.wtm[:, c, q0:q0 + qw], c == 0, c == 7, R=[C.wtm, hT], W=[ps])
                    P.copy(ot[:, 0:qw], ps[:, 0:qw], R=[ps], W=[ot], en="act" if io % 2 else "dve")
                    P.dma(C.projT[b, t0 + i * 128:t0 + (i + 1) * 128, q0:q0 + qw], ot[:, 0:qw], R=[ot], W=[], q="act")
                    io += 1


DN_STOP = 0
def bc_mid(ap, n):
    return ap.unsqueeze(1).broadcast_to([ap.shape[0], n, ap.shape[1]])


def bc_last(ap, n):
    return ap.unsqueeze(2).broadcast_to([ap.shape[0], ap.shape[1], n])


def declare_dn(P, C):
    nc = P.nc
    def inp(name, shape):
        return T(nc.dram_tensor(name, list(shape), F32, kind="ExternalInput"))
    C.cw = inp("cw", [DEPTH, 128, 12, 5])
    C.dn_a_log = inp("dn_a_log", [DEPTH, 16])
    C.dn_dt_bias = inp("dn_dt_bias", [DEPTH, 16])
    C.dn_norm_g = inp("dn_norm_g", [DEPTH, 64])
    C.masks = inp("masks", [64, 6, 64])
    C.bd = inp("bd", [128, 128])
    C.dnf = P.dram("dnf", [NB, 1024, TA])
    C.dnt = P.dram("dnt", [NB, TA, 1536])
    C.gates = P.dram("gates", [NB, TA, 32])
    C.dno = P.dram("dno", [NB, 2, TA, 512])
    C.br = P.dram("br", [NB, 3, 512, TA])


def host_masks():
    a = np.arange(64)[:, None]
    b = np.arange(64)[None, :]
    m = np.stack([a <= b, a >= b, a > b, a < b, a == b, np.ones((64, 64), bool)], 1).astype(np.float32)
    bd = np.kron(np.eye(2), np.ones((64, 64))).astype(np.float32)
    return np.ascontiguousarray(m), bd


def stage_dn_prep(P, C, l, b, parts=(1, 2, 3, 4)):
    with P.scope():
        _stage_dn_prep(P, C, l, b, parts)


def _stage_dn_prep(P, C, l, b, parts=(1, 2, 3, 4)):
    WV = TA + 4
    ub = [P.sb("ub%d" % i, [128, TA + 8]) for i in range(2)]
    acc = [P.sb("acc%d" % i, [128, WV]) for i in range(2)]
    sqb = P.sb("sqb", [128, WV])
    rs = [P.sb("rs%d" % i, [128, 512]) for i in range(2)]
    cws = P.sb("cws", [128, 12, 5])
    bds = P.sb("bds", [128, 128])
    tmb = [P.sb("tmb%d" % i, [128, 4, 128]) for i in range(2)]
    ps_n = [P.ps("ps_n%d" % i, [128, 512]) for i in range(2)]
    ps_t = [P.ps("ps_t%d" % i, [128, 512]) for i in range(2)]
    P.dma(cws[:], C.cw[l], R=[C.cw], W=[cws])
    P.dma(bds[:], C.bd[:, :], R=[C.bd], W=[bds])
    for u in ub:
        P.op("dve", lambda e: e.memset(u[:], 0.0), W=[u])
    bg = P.sb("bg", [128, 18, 32])
    go = P.sb("go", [128, 18, 32])
    dtb = P.sb("dtb", [128, 16])
    nA = P.sb("nA", [128, 16])
    if 1 in parts:
      P.dma(bg[:], C.projT[b, :, 640:672].rearrange("(t p) f -> p t f", p=128), R=[C.projT], W=[bg])
      P.dma(dtb[:], C.dn_dt_bias[l, :].partition_broadcast(128), R=[C.dn_dt_bias], W=[dtb])
      P.dma(nA[:], C.dn_a_log[l, :].partition_broadcast(128), R=[C.dn_a_log], W=[nA])
      P.act(nA[:], nA[:], AF.Exp, R=[nA], W=[nA])
      P.ts(nA[:], nA[:], -1.0, None, ALU.mult, ALU.bypass, R=[nA], W=[nA])
      P.act(go[:, :, 0:16], bg[:, :, 0:16], AF.Sigmoid, R=[bg], W=[go])
      P.tt(bg[:, :, 16:32], bg[:, :, 16:32], bc_mid(dtb[:], 18), ALU.add, R=[bg, dtb], W=[bg])
      P.act(bg[:, :, 16:32], bg[:, :, 16:32], AF.Exp, R=[bg], W=[bg])
      P.act(bg[:, :, 16:32], bg[:, :, 16:32], AF.Ln, R=[bg], W=[bg], bias=1.0)
      P.tt(go[:, :, 16:32], bg[:, :, 16:32], bc_mid(nA[:], 18), ALU.mult, R=[bg, nA], W=[go])
      P.dma(C.gates[b].rearrange("(t p) f -> p t f", p=128), go[:], R=[go], W=[], q="act")
    r0 = FM_OFF["dqkv"]
    it = 0
    for c in (range(12) if 2 in parts else []):
        u = ub[c % 2]
        a = acc[c % 2]
        P.dma(u[:, 2:2 + TC], C.proj[b, r0 + c * 128:r0 + (c + 1) * 128, 0:TC], R=[C.proj], W=[u])
        P.dma(u[:, 6 + TC:6 + TA], C.proj[b, r0 + c * 128:r0 + (c + 1) * 128, TC:TA], R=[C.proj], W=[u])
        P.ts(a[:], u[:, 0:WV], cws[:, c, 0:1], None, ALU.mult, ALU.bypass, R=[u, cws], W=[a])
        for j in range(1, 5):
            P.stt(a[:], u[:, j:j + WV], cws[:, c, j:j + 1], a[:], ALU.mult, ALU.add, R=[u, cws, a], W=[a])
        P.act(a[:], a[:], AF.Silu, R=[a], W=[a])
        if c < 8 and 3 in parts:
            P.act(sqb[:], a[:], AF.Square, R=[a], W=[sqb])
            for k in range((WV + 511) // 512):
                n = min(512, WV - k * 512)
                ps = ps_n[k % 2]
                r = rs[k % 2]
                P.mm(ps[:, 0:n], bds[:], sqb[:, k * 512:k * 512 + n], True, True, R=[bds, sqb], W=[ps])
                P.ts(r[:, 0:n], ps[:, 0:n], 1.0, EPS, ALU.mult, ALU.add, R=[ps], W=[r])
                P.act(r[:, 0:n], r[:, 0:n], AF.Sqrt, R=[r], W=[r])
                P.op("dve", lambda e: e.reciprocal(out=r[:, 0:n], in_=r[:, 0:n]), R=[r], W=[r])
                P.stt(a[:, k * 512:k * 512 + n], a[:, k * 512:k * 512 + n], 0.125 if c < 4 else 1.0, r[:, 0:n], ALU.mult, ALU.mult, R=[a, r], W=[a])
            P.dma(C.dnf[b, c * 128:(c + 1) * 128, 0:TC], a[:, 0:TC], R=[a], W=[], q="act")
            P.dma(C.dnf[b, c * 128:(c + 1) * 128, TC:TA], a[:, TC + 4:WV], R=[a], W=[], q="act")
        for t0 in (range(0, 18, 4) if 4 in parts else []):
            nt = min(4, 18 - t0)
            ps = ps_t[it % 2]
            tb = tmb[it % 2]
            it += 1
            for k in range(nt):
                tile = t0 + k
                col = tile * 128 if tile < 2 else 4 + tile * 128
                P.tr(ps[:, k * 128:(k + 1) * 128], a[:, col:col + 128], C.ident_sb[:], R=[a, C.ident_sb], W=[ps])
            P.copy(tb[:, 0:nt, :], ps[:, 0:nt * 128].rearrange("p (t f) -> p t f", f=128), R=[ps], W=[tb], en="act" if it % 2 else "dve")
            P.dma(C.dnt[b, t0 * 128:(t0 + nt) * 128, c * 128:(c + 1) * 128].rearrange("(t p) f -> p t f", p=128), tb[:, 0:nt, :], R=[tb], W=[], q="act")


def stage_dn_scan(P, C, l, b, with_ctx_out, only_chunks=None):
    with P.scope():
        _stage_dn_scan(P, C, l, b, with_ctx_out, only_chunks)


def _stage_dn_scan(P, C, l, b, with_ctx_out, only_chunks):
    mk = P.sb("mk", [64, 6, 64])
    P.dma(mk[:], C.masks[:, :, :], R=[C.masks], W=[mk])
    LE, GE, GT, LT, I64, ONE = [mk[:, i, :] for i in range(6)]
    NBUF = 2
    def sbl(name, shape):
        return [P.sb(name + str(i), shape) for i in range(NBUF)]
    ktm = sbl("ktm", [64, 8, 64]); vtm = sbl("vtm", [64, 8, 64]); kT = sbl("kT", [64, 8, 64]); qT = sbl("qT", [64, 8, 64])
    gt = sbl("gt", [64, 32])
    sm = sbl("sm", [64, 8, 8])
    rd = sbl("rd", [64, 8, 64]); rdT = sbl("rdT", [64, 8, 64])
    decS = sbl("decS", [64, 8, 64]); decCT = sbl("decCT", [64, 8, 64])
    Ma = sbl("Ma", [64, 8, 64]); Mb = sbl("Mb", [64, 8, 64]); MTa = sbl("MTa", [64, 8, 64]); MTb = sbl("MTb", [64, 8, 64])
    Pa = sbl("Pa", [64, 8, 64]); Pb = sbl("Pb", [64, 8, 64])
    vb = sbl("vb", [64, 8, 64]); rw = sbl("rw", [64, 8, 64]); kdec = sbl("kdec", [64, 8, 64])
    u_ = sbl("u_", [64, 8, 64]); wT = sbl("wT", [64, 8, 64]); aT = sbl("aT", [64, 8, 64])
    vnew = sbl("vnew", [64, 8, 64]); ot = sbl("dno_t", [64, 8, 64]); tmp = sbl("dtmp", [64, 8, 64])
    S = P.sb("Sst", [64, 8, 64])
    psb = [P.ps("dps%d" % i, [64, 512]) for i in range(6)]
    pss = P.ps("dpss", [64, 16])
    pc = [0]

    def nps():
        pc[0] += 1
        return psb[pc[0] % 6]

    def mmh(ps, lhs, rhs, Rl):
        for h in range(8):
            P.mm(ps[:, h * 64:(h + 1) * 64], lhs[:, h, :], rhs[:, h, :], True, True, R=Rl, W=[ps])

    def v3(t):
        return t[:].rearrange("p (h f) -> p h f", h=8)

    ci = 0
    for d in range(2):
        Minc, Mstr = (LE, GT) if d == 0 else (GE, LT)
        Sm = Mstr
        CT = Minc
        P.op("dve", lambda e: e.memset(S[:], 0.0), W=[S])
        order = list(range(4)) + list(range(4, 36)) if d == 0 else list(range(3, -1, -1)) + list(range(35, 3, -1))
        for cidx in order:
            if only_chunks is not None and cidx not in only_chunks:
                continue
            is_ctx = cidx < 4
            want_out = (not is_ctx) or with_ctx_out
            tok0 = cidx * 64
            k = ci % NBUF
            ci += 1
            P.dma(ktm[k][:], C.dnt[b, tok0:tok0 + 64, 512:1024].rearrange("t (h f) -> t h f", h=8), R=[C.dnt], W=[ktm[k]])
            P.dma(vtm[k][:], C.dnt[b, tok0:tok0 + 64, 1024:1536].rearrange("t (h f) -> t h f", h=8), R=[C.dnt], W=[vtm[k]])
            P.dma(kT[k][:], C.dnf[b, 512:1024, tok0:tok0 + 64].rearrange("(h f) t -> f h t", h=8), R=[C.dnf], W=[kT[k]])
            P.dma(qT[k][:], C.dnf[b, 0:512, tok0:tok0 + 64].rearrange("(h f) t -> f h t", h=8), R=[C.dnf], W=[qT[k]])
            P.dma(gt[k][:], C.gates[b, tok0:tok0 + 64, :], R=[C.gates], W=[gt[k]])
            beta = gt[k][:, d * 8:(d + 1) * 8]
            g = gt[k][:, 16 + d * 8:16 + (d + 1) * 8]
            s = sm[k]
            P.mm(pss[:, 0:8], Minc, g, True, True, R=[mk, gt[k]], W=[pss])
            P.mm(pss[:, 8:16], ONE, g, True, True, R=[mk, gt[k]], W=[pss])
            P.copy(s[:, 0:2, :], pss[:, 0:16].rearrange("p (a h) -> p a h", a=2), R=[pss], W=[s])
            P.act(s[:, 2:4, :], s[:, 0:2, :], AF.Exp, R=[s], W=[s])
            P.tt(s[:, 7, :], s[:, 1, :], s[:, 0, :], ALU.subtract, R=[s], W=[s])
            P.act(s[:, 4, :], s[:, 7, :], AF.Exp, R=[s], W=[s])
            P.tt(s[:, 5, :], beta, s[:, 2, :], ALU.mult, R=[s, gt[k]], W=[s])
            P.ts(s[:, 6, :], beta, -1.0, None, ALU.mult, ALU.bypass, R=[gt[k]], W=[s])
            if DN_STOP == 1:
                continue
            P.tt(rd[k][:], bc_mid(Mstr, 8), bc_last(g, 64), ALU.mult, R=[mk, gt[k]], W=[rd[k]])
            P.tt(rdT[k][:], bc_mid(Minc, 8), bc_last(g, 64), ALU.mult, R=[mk, gt[k]], W=[rdT[k]])
            p1 = nps()
            P.mm(p1[:, :], Minc, rd[k][:].rearrange("p h f -> p (h f)"), True, True, R=[mk, rd[k]], W=[p1])
            P.act(decS[k][:].rearrange("p h f -> p (h f)"), p1[:, :], AF.Exp, R=[p1], W=[decS[k]])
            P.tt(decS[k][:], decS[k][:], bc_mid(Sm, 8), ALU.mult, R=[decS[k], mk], W=[decS[k]])
            if DN_STOP == 2:
                continue
            p2 = nps()
            P.mm(p2[:, :], Mstr, rdT[k][:].rearrange("p h f -> p (h f)"), True, True, R=[mk, rdT[k]], W=[p2])
            P.act(decCT[k][:].rearrange("p h f -> p (h f)"), p2[:, :], AF.Exp, R=[p2], W=[decCT[k]])
            P.tt(decCT[k][:], decCT[k][:], bc_mid(CT, 8), ALU.mult, R=[decCT[k], mk], W=[decCT[k]])
            if DN_STOP == 3:
                continue
            p3 = nps()
            mmh(p3, kT[k], kT[k], [kT[k]])
            MT, M, MT2, M2 = MTa[k], Ma[k], MTb[k], Mb[k]
            P.tt(MT[:], v3(p3), decS[k][:], ALU.mult, R=[p3, decS[k]], W=[MT])
            P.tt(MT[:], MT[:], bc_last(s[:, 6, :], 64), ALU.mult, R=[MT, s], W=[MT])
            if DN_STOP == 4:
                continue
            p4 = nps()
            for h in range(8):
                P.tr(p4[:, h * 64:(h + 1) * 64], MT[:, h, :], I64, R=[MT, mk], W=[p4])
            P.copy(M[:], v3(p4), R=[p4], W=[M], en="act")
            if DN_STOP == 5:
                continue
            Pc, Pn = Pa[k], Pb[k]
            P.tt(Pc[:], v3(p4), bc_mid(I64, 8), ALU.add, R=[p4, mk], W=[Pc])
            for lev in range(5):
                pa = nps()
                mmh(pa, M, MT, [M, MT])
                P.copy(MT2[:], v3(pa), R=[pa], W=[MT2], en="act")
                if lev < 4:
                    pb = nps()
                    mmh(pb, MT, M, [M, MT])
                    P.copy(M2[:], v3(pb), R=[pb], W=[M2], en="dve")
                pcx = nps()
                mmh(pcx, MT2, Pc, [MT2, Pc])
                P.tt(Pn[:], v3(pcx), Pc[:], ALU.add, R=[pcx, Pc], W=[Pn])
                Pc, Pn = Pn, Pc
                M, M2 = M2, M
                MT, MT2 = MT2, MT
            if DN_STOP == 6:
                continue
            P.tt(vb[k][:], vtm[k][:], bc_last(beta, 64), ALU.mult, R=[vtm[k], gt[k]], W=[vb[k]])
            P.tt(rw[k][:], ktm[k][:], bc_last(s[:, 5, :], 64), ALU.mult, R=[ktm[k], s], W=[rw[k]])
            P.tt(kdec[k][:], ktm[k][:], bc_last(s[:, 4, :], 64), ALU.mult, R=[ktm[k], s], W=[kdec[k]])
            pu = nps()
            mmh(pu, Pc, vb[k], [Pc, vb[k]])
            P.copy(u_[k][:], v3(pu), R=[pu], W=[u_[k]], en="act")
            pw = nps()
            mmh(pw, rw[k], Pc, [Pc, rw[k]])
            P.copy(wT[k][:], v3(pw), R=[pw], W=[wT[k]], en="act")
            if want_out:
                pa2 = nps()
                mmh(pa2, kT[k], qT[k], [kT[k], qT[k]])
                P.tt(aT[k][:], v3(pa2), decCT[k][:], ALU.mult, R=[pa2, decCT[k]], W=[aT[k]])
            if DN_STOP == 7:
                continue
            pws = nps()
            mmh(pws, wT[k], S, [wT[k], S])
            P.tt(vnew[k][:], u_[k][:], v3(pws), ALU.subtract, R=[u_[k], pws], W=[vnew[k]])
            if want_out:
                pq = nps()
                mmh(pq, qT[k], S, [qT[k], S])
                pv = nps()
                mmh(pv, aT[k], vnew[k], [aT[k], vnew[k]])
                P.tt(tmp[k][:], v3(pq), bc_last(s[:, 2, :], 64), ALU.mult, R=[pq, s], W=[tmp[k]])
                P.tt(ot[k][:], tmp[k][:], v3(pv), ALU.add, R=[tmp[k], pv], W=[ot[k]])
                P.dma(C.dno[b, d, tok0:tok0 + 64, :], ot[k][:].rearrange("p h f -> p (h f)"), R=[ot[k]], W=[], q="act")
            pk = nps()
            mmh(pk, kdec[k], vnew[k], [kdec[k], vnew[k]])
            P.tt(S[:], S[:], bc_last(s[:, 3, :], 64), ALU.mult, R=[S, s], W=[S])
            P.tt(S[:], S[:], v3(pk), ALU.add, R=[S, pk], W=[S])


def stage_dn_out(P, C, l, b, with_ctx_out):
    with P.scope():
        _stage_dn_out(P, C, l, b, with_ctx_out)


def _stage_dn_out(P, C, l, b, with_ctx_out):
    o0 = [P.sb("o0_%d" % i, [128, 8, 64]) for i in range(2)]
    o1 = [P.sb("o1_%d" % i, [128, 8, 64]) for i in range(2)]
    zt = [P.sb("zt_%d" % i, [128, 8, 64]) for i in range(2)]
    sq = [P.sb("osq_%d" % i, [128, 8, 64]) for i in range(2)]
    ms = [P.sb("oms_%d" % i, [128, 8]) for i in range(2)]
    ng = P.sb("ong", [128, 64])
    ps = [P.ps("ops%d" % i, [128, 512]) for i in range(2)]
    oT = [P.sb("ooT%d" % i, [128, 4, 128]) for i in range(2)]
    P.dma(ng[:], C.dn_norm_g[l, :].partition_broadcast(128), R=[C.dn_norm_g], W=[ng])
    for t in range(18):
        if t < 2 and not with_ctx_out:
            continue
        k = t % 2
        rows = slice(t * 128, (t + 1) * 128)
        P.dma(o0[k][:].rearrange("p h f -> p (h f)"), C.dno[b, 0, rows, :], R=[C.dno], W=[o0[k]])
        P.dma(o1[k][:].rearrange("p h f -> p (h f)"), C.dno[b, 1, rows, :], R=[C.dno], W=[o1[k]])
        P.dma(zt[k][:].rearrange("p h f -> p (h f)"), C.projT[b, rows, 128:640], R=[C.projT], W=[zt[k]])
        P.tt(o0[k][:], o0[k][:], o1[k][:], ALU.add, R=[o0[k], o1[k]], W=[o0[k]])
        P.act(sq[k][:], o0[k][:], AF.Square, R=[o0[k]], W=[sq[k]])
        P.op("dve", lambda e: e.tensor_reduce(out=ms[k][:], in_=sq[k][:], axis=AX.X, op=ALU.add), R=[sq[k]], W=[ms[k]])
        P.ts(ms[k][:], ms[k][:], 1.0 / 64, EPS, ALU.mult, ALU.add, R=[ms[k]], W=[ms[k]])
        P.act(ms[k][:], ms[k][:], AF.Sqrt, R=[ms[k]], W=[ms[k]])
        P.op("dve", lambda e: e.reciprocal(out=ms[k][:], in_=ms[k][:]), R=[ms[k]], W=[ms[k]])
        P.tt(o0[k][:], o0[k][:], bc_last(ms[k][:], 64), ALU.mult, R=[o0[k], ms[k]], W=[o0[k]])
        P.tt(o0[k][:], o0[k][:], bc_mid(ng[:], 8), ALU.mult, R=[o0[k], ng], W=[o0[k]])
        P.act(zt[k][:], zt[k][:], AF.Silu, R=[zt[k]], W=[zt[k]])
        P.tt(o0[k][:], o0[k][:], zt[k][:], ALU.mult, R=[o0[k], zt[k]], W=[o0[k]])
        of = o0[k][:].rearrange("p h f -> p (h f)")
        for c in range(4):
            P.tr(ps[k][:, c * 128:(c + 1) * 128], of[:, c * 128:(c + 1) * 128], C.ident_sb[:], R=[o0[k], C.ident_sb], W=[ps[k]])
        P.copy(oT[k][:], ps[k][:].rearrange("p (c t) -> p c t", c=4), R=[ps[k]], W=[oT[k]], en="act")
        P.dma(C.br[b, 2, :, rows].rearrange("(c p) t -> p c t", p=128), oT[k][:], R=[oT[k]], W=[], q="act")


def host_shared(I):
    f = lambda a: np.ascontiguousarray(np.asarray(a, dtype=np.float32))
    w_fm, w_tm = host_w_in_layout(np.asarray(I["w_in"]))
    masks, bd = host_masks()
    L = DEPTH
    sh = dict(
        w_mod=f(I["w_mod"]), b_mod=f(I["b_mod"]), norm1_g=f(I["norm1_g"]), norm2_g=f(I["norm2_g"]),
        w_fm=f(w_fm), w_tm=f(w_tm), ident=np.eye(128, dtype=np.float32),
        cw=f(np.asarray(I["dn_conv_w"]).reshape(L, 5, 12, 128).transpose(0, 3, 2, 1)),
        dn_a_log=f(np.asarray(I["dn_a_log"]).reshape(L, 16)), dn_dt_bias=f(np.asarray(I["dn_dt_bias"]).reshape(L, 16)),
        dn_norm_g=f(I["dn_norm_g"]), masks=masks, bd=bd,
    )
    sh.update(host_attn(I))
    return sh


def host_core(I, core):
    b0 = core * NB
    cs = np.stack([np.asarray(I["c"])[b0], np.asarray(I["c"])[b0 + 1], np.asarray(I["c_ctx"])], 0)
    cT = np.ascontiguousarray(cs.T.reshape(8, 128, 3).transpose(1, 0, 2)).astype(np.float32)
    return dict(x=np.ascontiguousarray(np.asarray(I["x"])[b0:b0 + NB]), ctx=np.ascontiguousarray(np.asarray(I["ctx"])[b0:b0 + NB]), cT=cT)


def rope_tables_np(n_tok, rot_dim):
    rows = n_tok // 64
    row = np.broadcast_to(np.arange(rows)[:, None], (rows, 64)).reshape(-1).astype(np.float32)
    col = np.broadcast_to(np.arange(64)[None, :], (rows, 64)).reshape(-1).astype(np.float32)
    n_freq = rot_dim // 4
    inv_freq = (np.float32(10000.0) ** (-np.arange(n_freq, dtype=np.float32) / np.float32(n_freq))).astype(np.float32)
    ang = np.concatenate([row[:, None] * inv_freq, col[:, None] * inv_freq], axis=-1).astype(np.float32)
    cos, sin = np.cos(ang).astype(np.float32), np.sin(ang).astype(np.float32)
    CC = np.concatenate([cos.T, cos.T], 0)
    SS = np.concatenate([-sin.T, sin.T], 0)
    return np.ascontiguousarray(np.stack([CC, SS], 0))


def declare_attn(P, C):
    nc = P.nc
    def inp(name, shape):
        return T(nc.dram_tensor(name, list(shape), F32, kind="ExternalInput"))
    C.qg = inp("qg", [DEPTH, 128, 3])
    C.kvg = inp("kvg", [DEPTH, 128, 2])
    C.w_qn = inp("w_qn", [DEPTH, 384, 512])
    C.w_qrA = inp("w_qrA", [DEPTH, 384, 256])
    C.w_qrB = inp("w_qrB", [DEPTH, 384, 256])
    C.w_kn = inp("w_kn", [DEPTH, 256, 512])
    C.w_v = inp("w_v", [DEPTH, 256, 512])
    C.ropeM = inp("ropeM", [2, 32, TL])
    C.ropeS = inp("ropeS", [2, 64, TL])
    C.swam = inp("swam", [128, 2, 128])
    C.swa_sink = inp("swa_sink", [DEPTH, 8])


def host_attn(I):
    f = lambda a: np.ascontiguousarray(np.asarray(a, dtype=np.float32))
    L = DEPTH
    wq = np.asarray(I["mla_w_q_up"]).reshape(L, 384, 8, 96)
    wkv = np.asarray(I["mla_w_kv_up"]).reshape(L, 256, 8, 128)
    kk = np.arange(128)[:, None]
    qq = np.arange(128)[None, :]
    swam = np.stack([kk >= qq, kk <= qq], 1).astype(np.float32)
    return dict(
        qg=f(np.asarray(I["mla_q_norm_g"]).reshape(L, 3, 128).transpose(0, 2, 1)),
        kvg=f(np.asarray(I["mla_kv_norm_g"]).reshape(L, 2, 128).transpose(0, 2, 1)),
        w_qn=f(wq[..., :64].reshape(L, 384, 512)),
        w_qrA=f(wq[..., 64:96].reshape(L, 384, 256)),
        w_qrB=f(np.concatenate([wq[..., 80:96], wq[..., 64:80]], -1).reshape(L, 384, 256)),
        w_kn=f(wkv[..., :64].reshape(L, 256, 512)),
        w_v=f(wkv[..., 64:].reshape(L, 256, 512)),
        ropeM=rope_tables_np(TL, 32), ropeS=rope_tables_np(TL, 64), swam=f(swam), swa_sink=f(I["swa_sink"]),
    )


def col_blocks():
    return [(i * 512, min(512, TA - i * 512)) for i in range((TA + 511) // 512)]


def stage_mla(P, C, l, b, ctx_out):
    with P.scope():
        _stage_mla(P, C, l, b, ctx_out)


def _stage_mla(P, C, l, b, ctx_out):
    SCALE = float(96 ** -0.5)
    cqn = P.sb("cqn", [128, 3, TA]); ckvn = P.sb("ckvn", [128, 2, TA])
    sqt = P.sb("sqt", [128, 3, 512]); rs = P.sb("mrs", [128, 512])
    qg = P.sb("qg", [128, 3]); kvg = P.sb("kvg", [128, 2])
    wqn = P.sb("wqn", [128, 3, 512]); wqa = P.sb("wqa", [128, 3, 256]); wqb = P.sb("wqb", [128, 3, 256])
    wkn = P.sb("wkn", [128, 2, 512]); wv = P.sb("wv", [128, 2, 512])
    rope = P.sb("ropem", [32, 2, TL])
    krr = P.sb("krr", [32, TA]); krb = P.sb("krb", [32, TA])
    qn = P.sb("qn", [64, TA]); qr = P.sb("qr", [32, TA]); qrb = P.sb("qrb", [32, TA]); kn = P.sb("kn", [64, TA])
    vh = P.sb("vh", [128, 18, 64]); oT = P.sb("oT", [64, TA])
    pt = [P.sb("pt%d" % i, [128, 512]) for i in range(3)]
    rd = [P.sb("rdm%d" % i, [64, 512]) for i in range(2)]
    pA = [P.ps("pA%d" % i, [128, 512]) for i in range(2)]
    pO = [P.ps("pO%d" % i, [64, 512]) for i in range(2)]
    pD = [P.ps("pD%d" % i, [64, 512]) for i in range(2)]
    pX = [P.ps("pX%d" % i, [128, 512]) for i in range(2)]
    ix = [0]

    def npx():
        ix[0] += 1
        return pX[ix[0] % 2]

    P.dma(qg[:], C.qg[l], R=[C.qg], W=[qg]); P.dma(kvg[:], C.kvg[l], R=[C.kvg], W=[kvg])
    for (wt, src) in ((wqn, C.w_qn), (wqa, C.w_qrA), (wqb, C.w_qrB), (wkn, C.w_kn), (wv, C.w_v)):
        P.dma(wt[:], src.t[l].rearrange("(c p) n -> p c n", p=128), R=[src], W=[wt])
    P.dma(rope[:], C.ropeM[:, :, :].rearrange("a p t -> p a t"), R=[C.ropeM], W=[rope])
    CC, SS = rope[:, 0, :], rope[:, 1, :]
    for c in range(3):
        P.dma(cqn[:, c, :], C.proj[b, c * 128:(c + 1) * 128, :], R=[], W=[cqn])
    for c in range(2):
        P.dma(ckvn[:, c, :], C.proj[b, 384 + c * 128:384 + (c + 1) * 128, :], R=[], W=[ckvn])
    P.dma(krr[:], C.proj[b, FM_OFF["krA"]:FM_OFF["krA"] + 32, :], R=[], W=[krr])
    P.dma(krb[:], C.proj[b, FM_OFF["krB"]:FM_OFF["krB"] + 32, :], R=[], W=[krb])
    for (xt, nchunk, g, dim) in ((cqn, 3, qg, 384), (ckvn, 2, kvg, 256)):
        for (c0, n) in col_blocks():
            P.act(sqt[:, 0:nchunk, 0:n], xt[:, :, c0:c0 + n], AF.Square, R=[xt], W=[sqt])
            ps = npx()
            for c in range(nchunk):
                P.mm(ps[:, 0:n], C.ones_sb[:], sqt[:, c, 0:n], c == 0, c == nchunk - 1, R=[C.ones_sb, sqt], W=[ps])
            P.ts(rs[:, 0:n], ps[:, 0:n], 1.0 / dim, EPS, ALU.mult, ALU.add, R=[ps], W=[rs])
            P.act(rs[:, 0:n], rs[:, 0:n], AF.Sqrt, R=[rs], W=[rs])
            P.op("dve", lambda e: e.reciprocal(out=rs[:, 0:n], in_=rs[:, 0:n]), R=[rs], W=[rs])
            for c in range(nchunk):
                P.stt(xt[:, c, c0:c0 + n], xt[:, c, c0:c0 + n], g[:, c:c + 1], rs[:, 0:n], ALU.mult, ALU.mult, R=[xt, g, rs], W=[xt])
    P.tt(krr[:, TC:TA], krr[:, TC:TA], CC, ALU.mult, R=[krr, rope], W=[krr])
    P.tt(krb[:, TC:TA], krb[:, TC:TA], SS, ALU.mult, R=[krb, rope], W=[krb])
    P.tt(krr[:, TC:TA], krr[:, TC:TA], krb[:, TC:TA], ALU.add, R=[krr, krb], W=[krr])
    ia = 0
    for h in range(8):
        for (c0, n) in col_blocks():
            ps = npx()
            for c in range(3):
                P.mm(ps[0:64, 0:n], wqn[:, c, h * 64:(h + 1) * 64], cqn[:, c, c0:c0 + n], c == 0, c == 2, R=[wqn, cqn], W=[ps])
            P.act(qn[:, c0:c0 + n], ps[0:64, 0:n], AF.Copy, R=[ps], W=[qn], scale=SCALE)
            ps = npx()
            for c in range(3):
                P.mm(ps[0:32, 0:n], wqa[:, c, h * 32:(h + 1) * 32], cqn[:, c, c0:c0 + n], c == 0, c == 2, R=[wqa, cqn], W=[ps])
            P.act(qr[:, c0:c0 + n], ps[0:32, 0:n], AF.Copy, R=[ps], W=[qr], scale=SCALE)
            ps = npx()
            for c in range(3):
                P.mm(ps[0:32, 0:n], wqb[:, c, h * 32:(h + 1) * 32], cqn[:, c, c0:c0 + n], c == 0, c == 2, R=[wqb, cqn], W=[ps])
            P.act(qrb[:, c0:c0 + n], ps[0:32, 0:n], AF.Copy, R=[ps], W=[qrb], scale=SCALE)
            ps = npx()
            for c in range(2):
                P.mm(ps[0:64, 0:n], wkn[:, c, h * 64:(h + 1) * 64], ckvn[:, c, c0:c0 + n], c == 0, c == 1, R=[wkn, ckvn], W=[ps])
            P.copy(kn[:, c0:c0 + n], ps[0:64, 0:n], R=[ps], W=[kn])
        P.tt(qr[:, TC:TA], qr[:, TC:TA], CC, ALU.mult, R=[qr, rope], W=[qr])
        P.tt(qrb[:, TC:TA], qrb[:, TC:TA], SS, ALU.mult, R=[qrb, rope], W=[qrb])
        P.tt(qr[:, TC:TA], qr[:, TC:TA], qrb[:, TC:TA], ALU.add, R=[qr, qrb], W=[qr])
        for t0 in range(0, 18, 8):
            nt = min(8, 18 - t0)
            ps = npx()
            for j in range(nt):
                for c in range(2):
                    P.mm(ps[:, j * 64:(j + 1) * 64], ckvn[:, c, (t0 + j) * 128:(t0 + j + 1) * 128], wv[:, c, h * 64:(h + 1) * 64], c == 0, c == 1, R=[wv, ckvn], W=[ps])
            P.copy(vh[:, t0:t0 + nt, :], ps[:, 0:nt * 64].rearrange("p (t f) -> p t f", f=64), R=[ps], W=[vh])
        qblocks = [(TC + i * 512, 512, 18) for i in range(4)]
        if ctx_out:
            qblocks.append((0, TC, 2))
        for (q0, n, nkt) in qblocks:
            po = pO[ia % 2]; pd = pD[ia % 2]; r = rd[ia % 2]
            ia += 1
            for kt in range(nkt):
                pa = pA[kt % 2]
                p = pt[kt % 3]
                P.mm(pa[:, 0:n], kn[:, kt * 128:(kt + 1) * 128], qn[:, q0:q0 + n], True, False, R=[kn, qn], W=[pa])
                P.mm(pa[:, 0:n], krr[:, kt * 128:(kt + 1) * 128], qr[:, q0:q0 + n], False, True, R=[krr, qr], W=[pa])
                P.act(p[:, 0:n], pa[:, 0:n], AF.Exp, R=[pa], W=[p])
                P.mm(po[:, 0:n], vh[:, kt, :], p[:, 0:n], kt == 0, kt == nkt - 1, R=[vh, p], W=[po])
                P.mm(pd[:, 0:n], C.ones_sb[:, 0:64], p[:, 0:n], kt == 0, kt == nkt - 1, R=[C.ones_sb, p], W=[pd])
            P.op("dve", lambda e: e.reciprocal(out=r[:, 0:n], in_=pd[:, 0:n]), R=[pd], W=[r])
            P.tt(oT[:, q0:q0 + n], po[:, 0:n], r[:, 0:n], ALU.mult, R=[po, r], W=[oT])
        c_lo = 0 if ctx_out else TC
        P.dma(C.br[b, 0, h * 64:(h + 1) * 64, c_lo:TA], oT[:, c_lo:TA], R=[oT], W=[], q="act")


def stage_swa(P, C, l, b, ctx_out):
    with P.scope():
        _stage_swa(P, C, l, b, ctx_out)


def _stage_swa(P, C, l, b, ctx_out):
    qA = P.sb("sqA", [64, 4, TA]); qB = P.sb("sqB", [64, 4, TA])
    kA = P.sb("skA", [64, TA]); kB = P.sb("skB", [64, TA])
    vn = P.sb("svn", [128, 18, 64])
    rope = P.sb("ropes", [64, 2, TL])
    msk = P.sb("swam", [128, 2, 128])
    snk = P.sb("snk", [64, 8])
    pt = [P.sb("spt%d" % i, [128, 4, 128]) for i in range(3)]
    rd = [P.sb("srd%d" % i, [64, 4, 128]) for i in range(2)]
    ot = [P.sb("sot%d" % i, [64, 4, 128]) for i in range(2)]
    pA = [P.ps("spA%d" % i, [128, 512]) for i in range(2)]
    pO = [P.ps("spO%d" % i, [64, 512]) for i in range(2)]
    pD = [P.ps("spD%d" % i, [64, 512]) for i in range(2)]
    P.dma(rope[:], C.ropeS[:, :, :].rearrange("a p t -> p a t"), R=[C.ropeS], W=[rope])
    P.dma(msk[:], C.swam[:, :, :], R=[C.swam], W=[msk])
    P.dma(snk[:], C.swa_sink[l, :].partition_broadcast(64), R=[C.swa_sink], W=[snk])
    P.act(snk[:], snk[:], AF.Exp, R=[snk], W=[snk])
    CC, SS = rope[:, 0, :], rope[:, 1, :]
    ib = 0
    ip = 0
    for n in range(2):
        for hh in range(4):
            r0 = FM_OFF["sqA"] + (4 * n + hh) * 64
            P.dma(qA[:, hh, :], C.proj[b, r0:r0 + 64, :], R=[], W=[qA])
            r0 = FM_OFF["sqB"] + (4 * n + hh) * 64
            P.dma(qB[:, hh, :], C.proj[b, r0:r0 + 64, :], R=[], W=[qB])
        P.dma(kA[:], C.proj[b, FM_OFF["skA"] + n * 64:FM_OFF["skA"] + (n + 1) * 64, :], R=[], W=[kA])
        P.dma(kB[:], C.proj[b, FM_OFF["skB"] + n * 64:FM_OFF["skB"] + (n + 1) * 64, :], R=[], W=[kB])
        P.dma(vn[:], C.projT[b, :, n * 64:(n + 1) * 64].rearrange("(t p) f -> p t f", p=128), R=[], W=[vn])
        P.tt(qA[:, :, TC:TA], qA[:, :, TC:TA], bc_mid(CC, 4), ALU.mult, R=[qA, rope], W=[qA])
        P.tt(qB[:, :, TC:TA], qB[:, :, TC:TA], bc_mid(SS, 4), ALU.mult, R=[qB, rope], W=[qB])
        P.tt(qA[:, :, TC:TA], qA[:, :, TC:TA], qB[:, :, TC:TA], ALU.add, R=[qA, qB], W=[qA])
        P.act(qA[:], qA[:], AF.Copy, R=[qA], W=[qA], scale=0.125)
        P.tt(kA[:, TC:TA], kA[:, TC:TA], CC, ALU.mult, R=[kA, rope], W=[kA])
        P.tt(kB[:, TC:TA], kB[:, TC:TA], SS, ALU.mult, R=[kB, rope], W=[kB])
        P.tt(kA[:, TC:TA], kA[:, TC:TA], kB[:, TC:TA], ALU.add, R=[kA, kB], W=[kA])
        qblocks = []
        if ctx_out:
            qblocks += [(0, -1), (128, -1)]
        qblocks += [(TC + i * 128, i) for i in range(16)]
        for (q0, i) in qblocks:
            tiles = [(0, None), (1, None)]
            if i >= 0:
                if i - 1 >= 0:
                    tiles.append((2 + i - 1, 0))
                tiles.append((2 + i, None))
                if i + 1 <= 15:
                    tiles.append((2 + i + 1, 1))
            po = pO[ib % 2]; pd = pD[ib % 2]; r = rd[ib % 2]; o = ot[ib % 2]
            ib += 1
            for ti, (kt, mi) in enumerate(tiles):
                pa = pA[ip % 2]; p = pt[ip % 3]
                ip += 1
                P.mm(pa[:].rearrange("p (h q) -> p h q", h=4), kA[:, kt * 128:(kt + 1) * 128], qA[:, :, q0:q0 + 128], True, True, R=[kA, qA], W=[pa])
                P.act(p[:], pa[:].rearrange("p (h q) -> p h q", h=4), AF.Exp, R=[pa], W=[p])
                if mi is not None:
                    P.tt(p[:], p[:], bc_mid(msk[:, mi, :], 4), ALU.mult, R=[p, msk], W=[p])
                pf = p[:].rearrange("p h q -> p (h q)")
                P.mm(po[:, :], vn[:, kt, :], pf, ti == 0, ti == len(tiles) - 1, R=[vn, p], W=[po])
                P.mm(pd[:, :], C.ones_sb[:, 0:64], pf, ti == 0, ti == len(tiles) - 1, R=[C.ones_sb, p], W=[pd])
            P.tt(r[:], pd[:].rearrange("p (h q) -> p h q", h=4), bc_last(snk[:, 4 * n:4 * n + 4], 128), ALU.add, R=[pd, snk], W=[r])
            P.op("dve", lambda e: e.reciprocal(out=r[:], in_=r[:]), R=[r], W=[r])
            P.tt(o[:], po[:].rearrange("p (h q) -> p h q", h=4), r[:], ALU.mult, R=[po, r], W=[o])
            P.dma(C.br[b, 1, n * 256:(n + 1) * 256, q0:q0 + 128].rearrange("(h f) t -> f h t", h=4), o[:], R=[o], W=[], q="act")


def declare_rest(P, C):
    nc = P.nc
    def inp(name, shape):
        return T(nc.dram_tensor(name, list(shape), F32, kind="ExternalInput"))
    C.w_branch = inp("w_branch", [DEPTH, 3, 512, D])
    C.w_out = inp("w_out", [DEPTH, D, D])
    C.router_w = inp("router_w", [DEPTH, D, 32])
    C.router_b = inp("router_b", [DEPTH, 32])
    C.exp_w_gu = inp("exp_w_gu", [DEPTH, 32, D, 2048])
    C.bgu = inp("bgu", [DEPTH, 128, 32, 2, 8])
    C.exp_w_dn = inp("exp_w_dn", [DEPTH, 32, D, D])
    C.exp_b_dn = inp("exp_b_dn", [DEPTH, 32, D])
    C.final_norm_g = inp("final_norm_g", [D])
    C.xres = P.dram("xres", [NB, TA, D])
    C.out = T(nc.dram_tensor("out", [NB, TL, D], F32, kind="ExternalOutput"))


def host_rest(I):
    f = lambda a: np.ascontiguousarray(np.asarray(a, dtype=np.float32))
    L = DEPTH
    bgu = np.asarray(I["exp_b_gu"]).reshape(L, 32, 8, 128, 2).transpose(0, 3, 1, 4, 2)
    return dict(w_branch=f(I["w_branch"]), w_out=f(I["w_out"]), router_w=f(I["router_w"]), router_b=f(I["router_b"]),
                exp_w_gu=f(I["exp_w_gu"]), bgu=f(bgu), exp_w_dn=f(I["exp_w_dn"]), exp_b_dn=f(I["exp_b_dn"]),
                final_norm_g=f(I["final_norm_g"]))


def stage_merge(P, C, l, b, ctx_out):
    with P.scope():
        _stage_merge(P, C, l, b, ctx_out)


def _stage_merge(P, C, l, b, ctx_out):
    wbr = P.sb("wbr", [128, 3, 4, D]); wout = P.sb("wout", [128, 8, D])
    brt = P.sb("brt", [128, 3, 4, 512])
    gch = [P.sb("gch%d" % i, [128, 512]) for i in range(3)]
    tmp = [P.sb("mtmp%d" % i, [128, 512]) for i in range(2)]
    yT = P.sb("yT", [128, 8, 512])
    g1 = [P.sb("g1bc%d" % i, [128, D]) for i in range(2)]
    xt = [P.sb("mxt%d" % i, [128, D]) for i in range(2)]
    pA = [P.ps("mpA%d" % i, [128, 512]) for i in range(3)]
    pB = [P.ps("mpB%d" % i, [128, 512]) for i in range(2)]
    P.dma(wbr[:].rearrange("p i c n -> p (i c) n"), C.w_branch.t[l].rearrange("i (c p) n -> p (i c) n", p=128), R=[C.w_branch], W=[wbr])
    P.dma(wout[:], C.w_out.t[l].rearrange("(c p) n -> p c n", p=128), R=[C.w_out], W=[wout])
    load_mod_bc(P, C, l, b, (2,), (g1[0],))
    load_mod_bc(P, C, l, 2, (2,), (g1[1],))
    ig = 0
    ix = 0
    for (t0, n) in blocks():
        if t0 < TC and not ctx_out:
            continue
        gbc = g1[1] if t0 < TC else g1[0]
        for i in range(3):
            P.dma(brt[:, i, :, 0:n], C.br[b, i, :, t0:t0 + n].rearrange("(c p) t -> p c t", p=128), R=[], W=[brt])
        for m in range(8):
            for i in range(3):
                gc = gch[ig % 3]; pa = pA[ig % 3]; tm = tmp[ig % 2]
                ig += 1
                r0 = FM_OFF["gate"] + i * D + m * 128
                P.dma(gc[:, 0:n], C.proj[b, r0:r0 + 128, t0:t0 + n], R=[], W=[gc])
                for c in range(4):
                    P.mm(pa[:, 0:n], wbr[:, i, c, m * 128:(m + 1) * 128], brt[:, i, c, 0:n], c == 0, c == 3, R=[wbr, brt], W=[pa])
                P.act(gc[:, 0:n], gc[:, 0:n], AF.Sigmoid, R=[gc], W=[gc])
                if i == 0:
                    P.tt(yT[:, m, 0:n], pa[:, 0:n], gc[:, 0:n], ALU.mult, R=[pa, gc], W=[yT])
                else:
                    P.tt(tm[:, 0:n], pa[:, 0:n], gc[:, 0:n], ALU.mult, R=[pa, gc], W=[tm])
                    P.tt(yT[:, m, 0:n], yT[:, m, 0:n], tm[:, 0:n], ALU.add, R=[yT, tm], W=[yT])
        for i in range(n // 128):
            x = xt[ix % 2]
            src_t, src_ap = token_src(C, l, b, t0 + i * 128, 128)
            P.dma(x[:], src_ap, R=[], W=[x])
            for half in range(2):
                pb = pB[(2 * ix + half) % 2]
                for m in range(8):
                    P.mm(pb[:, :], yT[:, m, i * 128:(i + 1) * 128], wout[:, m, half * 512:(half + 1) * 512], m == 0, m == 7, R=[yT, wout], W=[pb])
                tm = tmp[half]
                P.tt(tm[:], pb[:, :], gbc[:, half * 512:(half + 1) * 512], ALU.mult, R=[pb, gbc], W=[tm])
                P.tt(x[:, half * 512:(half + 1) * 512], x[:, half * 512:(half + 1) * 512], tm[:], ALU.add, R=[x, tm], W=[x])
            P.dma(C.xres[b, t0 + i * 128:t0 + (i + 1) * 128, :], x[:], R=[x], W=[], q="act")
            ix += 1


def stage_moe(P, C, l, b, ctx_out, n_exp=32):
    with P.scope():
        _stage_moe(P, C, l, b, ctx_out, n_exp)


def _stage_moe(P, C, l, b, ctx_out, n_exp):
    C.xt = [P.sb("xt%d" % i, [128, D]) for i in range(2)]
    C.ht = [P.sb("ht%d" % i, [128, D]) for i in range(2)]
    C.st = [P.sb("st%d" % i, [128, 4]) for i in range(2)]
    C.ps_tr = [P.ps("ps_tr%d" % i, [128, 1024]) for i in range(1)] * 2
    hT = P.sb("mhT", [128, 8, 512])
    hT16 = P.sb("mhT16", [128, 8, 512], BF16)
    modbc = [P.sb("modbc%d" % i, [128, D]) for i in range(3)]
    rw = P.sb("rw", [128, 8, 32]); rb = P.sb("rb", [128, 32])
    bgu = P.sb("bgu", [128, 32, 2, 8]); bdn = P.sb("bdn", [32, D])
    lg = P.sb("lg", [128, 32]); ex = P.sb("ex", [128, 32]); mk = P.sb("rmk", [128, 32]); t8 = P.sb("t8", [128, 8]); sc = P.sb("rsc", [128, 4])
    G = P.sb("G", [128, 4, 32]); GT = P.sb("GT", [32, 512])
    stg = [P.sb("stg%d" % i, [128, 8, 512]) for i in range(3)]
    wq = [P.sb("wq%d" % i, [128, 8, 512], BF16) for i in range(3)]
    wd = [P.sb("wd%d" % i, [128, 8, 512], BF16) for i in range(2)]
    actT = P.sb("actT", [128, 8, 512], BF16)
    acc = P.sb("acc", [128, 4, D])
    gs = [P.sb("gs%d" % i, [128, 512]) for i in range(2)]
    us = [P.sb("us%d" % i, [128, 512]) for i in range(2)]
    sg = [P.sb("sg%d" % i, [128, 512]) for i in range(2)]
    pR = P.ps("pR", [128, 512])
    pG = [P.ps("pG%d" % i, [128, 512]) for i in range(2)]
    pU = [P.ps("pU%d" % i, [128, 512]) for i in range(2)]
    pY = [P.ps("pY%d" % i, [128, 512]) for i in range(1)]
    P.dma(rw[:], C.router_w.t[l].rearrange("(c p) n -> p c n", p=128), R=[C.router_w], W=[rw])
    P.dma(rb[:], C.router_b[l, :].partition_broadcast(128), R=[C.router_b], W=[rb])
    P.dma(bgu[:], C.bgu[l], R=[C.bgu], W=[bgu])
    P.dma(bdn[:], C.exp_b_dn[l], R=[C.exp_b_dn], W=[bdn])
    it = 0
    ist = 0
    iq = 0
    idn = 0
    ie = 0
    last_r = None
    for (t0, n) in blocks():
        if t0 < TC and not ctx_out:
            continue
        r = 1 if t0 < TC else 0
        if r != last_r:
            load_mod_bc(P, C, l, 2 if r else b, (4, 3, 5), modbc)
            last_r = r
        A2, sh2, g2 = modbc
        nt = n // 128
        for i in range(nt):
            norm_mod_T(P, C, C.xres, C.xres[b, t0 + i * 128:t0 + (i + 1) * 128, :], A2, sh2, hT, i * 128, it)
            it += 1
        P.copy(hT16[:, :, 0:n], hT[:, :, 0:n], R=[hT], W=[hT16], en="pool")
        for i in range(nt):
            for c in range(8):
                P.mm(pR[:, 0:32], hT[:, c, i * 128:(i + 1) * 128], rw[:, c, :], c == 0, c == 7, R=[hT, rw], W=[pR])
            P.tt(lg[:], pR[:, 0:32], rb[:], ALU.add, R=[pR, rb], W=[lg])
            P.op("dve", lambda e: e.max(out=t8[:], in_=lg[:]), R=[lg], W=[t8])
            P.ts(mk[:], lg[:], t8[:, 3:4], None, ALU.is_ge, ALU.bypass, R=[lg, t8], W=[mk])
            P.ts(sc[:, 0:1], t8[:, 0:1], -1.0, None, ALU.mult, ALU.bypass, R=[t8], W=[sc])
            P.act(ex[:], lg[:], AF.Exp, R=[lg, sc], W=[ex], bias=sc[:, 0:1], scale=1.0)
            P.tt(ex[:], ex[:], mk[:], ALU.mult, R=[ex, mk], W=[ex])
            P.op("dve", lambda e: e.tensor_reduce(out=sc[:, 1:2], in_=ex[:], axis=AX.X, op=ALU.add), R=[ex], W=[sc])
            P.op("dve", lambda e: e.reciprocal(out=sc[:, 2:3], in_=sc[:, 1:2]), R=[sc], W=[sc])
            P.ts(G[:, i, :], ex[:], sc[:, 2:3], None, ALU.mult, ALU.bypass, R=[ex, sc], W=[G])
            P.tr(pR[0:32, 128:256], G[:, i, :], C.ident_sb[:], R=[G, C.ident_sb], W=[pR])
            P.copy(GT[:, i * 128:(i + 1) * 128], pR[0:32, 128:256], R=[pR], W=[GT])
        for i in range(nt):
            for half in range(2):
                P.mm(pR[:, :], GT[:, i * 128:(i + 1) * 128], bdn[:, half * 512:(half + 1) * 512], True, True, R=[GT, bdn], W=[pR])
                P.copy(acc[:, i, half * 512:(half + 1) * 512], pR[:, :], R=[pR], W=[acc])
        for e in range(n_exp):
            wgv = C.exp_w_gu.t[l, e].rearrange("(c p) n -> p c n", p=128)
            for qd in range(4):
                sgt = stg[ist % 3]
                ist += 1
                w = wq[iq % 3]
                iq += 1
                P.dma(sgt[:], wgv[:, :, qd * 512:(qd + 1) * 512], R=[C.exp_w_gu], W=[sgt])
                P.copy(w[:], sgt[:], R=[sgt], W=[w], en="act")
                for s in range(2):
                    fc = qd * 2 + s
                    pg = pG[ie % 2]; pu = pU[ie % 2]; g_ = gs[ie % 2]; u_ = us[ie % 2]; s_ = sg[ie % 2]
                    ie += 1
                    for c in range(8):
                        P.mm(pg[:, 0:n], w[:, c, s * 256:(s + 1) * 256:2], hT16[:, c, 0:n], c == 0, c == 7, R=[w, hT16], W=[pg])
                    for c in range(8):
                        P.mm(pu[:, 0:n], w[:, c, s * 256 + 1:(s + 1) * 256:2], hT16[:, c, 0:n], c == 0, c == 7, R=[w, hT16], W=[pu])
                    P.ts(g_[:, 0:n], pg[:, 0:n], bgu[:, e, 0, fc:fc + 1], 7.0, ALU.add, ALU.min, R=[pg, bgu], W=[g_])
                    P.ts(u_[:, 0:n], pu[:, 0:n], bgu[:, e, 1, fc:fc + 1], 7.0, ALU.add, ALU.min, R=[pu, bgu], W=[u_])
                    P.ts(u_[:, 0:n], u_[:, 0:n], -7.0, 1.0, ALU.max, ALU.add, R=[u_], W=[u_])
                    P.act(s_[:, 0:n], g_[:, 0:n], AF.Sigmoid, R=[g_], W=[s_], scale=1.702)
                    P.tt(g_[:, 0:n], g_[:, 0:n], s_[:, 0:n], ALU.mult, R=[g_, s_], W=[g_], en="pool")
                    P.tt(actT[:, fc, 0:n], g_[:, 0:n], u_[:, 0:n], ALU.mult, R=[g_, u_], W=[actT], en="pool")
            wdv = C.exp_w_dn.t[l, e].rearrange("(c p) n -> p c n", p=128)
            for half in range(2):
                sgt = stg[ist % 3]
                ist += 1
                w = wd[idn % 2]
                idn += 1
                P.dma(sgt[:], wdv[:, :, half * 512:(half + 1) * 512], R=[C.exp_w_dn], W=[sgt])
                P.copy(w[:], sgt[:], R=[sgt], W=[w], en="act")
                for i in range(nt):
                    py = pY[0]
                    for fc in range(8):
                        P.mm(py[:, :], actT[:, fc, i * 128:(i + 1) * 128], w[:, fc, :], fc == 0, fc == 7, R=[actT, w], W=[py])
                    a = acc[:, i, half * 512:(half + 1) * 512]
                    P.stt(a, py[:, :], G[:, i, e:e + 1], a, ALU.mult, ALU.add, R=[py, G, acc], W=[acc])
        for i in range(nt):
            x = C.xt[i % 2]
            rows = slice(t0 + i * 128, t0 + (i + 1) * 128)
            P.dma(x[:], C.xres[b, rows, :], R=[], W=[x])
            P.tt(acc[:, i, :], acc[:, i, :], g2[:], ALU.mult, R=[acc, g2], W=[acc])
            P.tt(x[:], x[:], acc[:, i, :], ALU.add, R=[x, acc], W=[x])
            P.dma(C.xres[b, rows, :], x[:], R=[x], W=[], q="act")


def stage_final(P, C, b):
    with P.scope():
        xt = [P.sb("fxt%d" % i, [128, D]) for i in range(2)]
        ht = [P.sb("fht%d" % i, [128, D]) for i in range(2)]
        st = [P.sb("fst%d" % i, [128, 4]) for i in range(2)]
        g = P.sb("fg", [128, D])
        P.dma(g[:], C.final_norm_g[:].partition_broadcast(128), R=[C.final_norm_g], W=[g])
        for i in range(TL // 128):
            x = xt[i % 2]; h = ht[i % 2]; s = st[i % 2]
            P.dma(x[:], C.xres[b, TC + i * 128:TC + (i + 1) * 128, :], R=[], W=[x])
            P.act(h[:], x[:], AF.Square, R=[x], W=[h, s], accum_out=s[:, 0:1])
            P.ts(s[:, 1:2], s[:, 0:1], 1.0 / D, EPS, ALU.mult, ALU.add, R=[s], W=[s])
            P.act(s[:, 2:3], s[:, 1:2], AF.Sqrt, R=[s], W=[s])
            P.op("dve", lambda e: e.reciprocal(out=s[:, 3:4], in_=s[:, 2:3]), R=[s], W=[s])
            P.stt(h[:], x[:], s[:, 3:4], g[:], ALU.mult, ALU.mult, R=[x, s, g], W=[h])
            P.dma(C.out[b, i * 128:(i + 1) * 128, :], h[:], R=[h], W=[], q="act", is_output=True)


def build_program(debug=False):
    P = Prog(debug=debug)
    C = Ctx()
    declare_io(P, C); declare_dn(P, C); declare_attn(P, C); declare_rest(P, C)
    stage_consts(P, C)
    for l in range(DEPTH):
        ctx_out = l < DEPTH - 1
        stage_mod(P, C, l)
        stage_proj(P, C, l)
        for b in range(NB):
            stage_mla(P, C, l, b, ctx_out)
            stage_swa(P, C, l, b, ctx_out)
            stage_dn_prep(P, C, l, b)
            stage_dn_scan(P, C, l, b, ctx_out)
            stage_dn_out(P, C, l, b, ctx_out)
            stage_merge(P, C, l, b, ctx_out)
            stage_moe(P, C, l, b, ctx_out)
    for b in range(NB):
        stage_final(P, C, b)
    P.finish()
    return P, C


_CACHE = {}


def kernel(**inputs):
    I = {k: np.asarray(v) for k, v in inputs.items()}
    if "prog" not in _CACHE:
        _CACHE["prog"] = build_program()
    P, C = _CACHE["prog"]
    sh = host_shared(I)
    sh.update(host_rest(I))
    in_maps = []
    for core in range(NCORES):
        m = dict(sh)
        m.update(host_core(I, core))
        in_maps.append(m)
    res = run_bass_kernel_spmd(P.nc, in_maps, core_ids=list(range(NCORES)))
    out = np.concatenate([np.asarray(r["out"]) for r in res.results], axis=0)
    return out.astype(np.float32)
```

```python
import numpy as np
import concourse.bass as bass
import concourse.mybir as mybir
from concourse.bass_utils import run_bass_kernel_spmd
from concourse.alu_op_type import AluOpType as ALU

F32 = mybir.dt.float32
BF16 = mybir.dt.bfloat16
AF = mybir.ActivationFunctionType
AX = mybir.AxisListType

D = 1024
DEPTH = 2
NB = 2
TC = 256
TL = 2048
TA = TC + TL
EPS = 1e-6
NCORES = 8


class Buf:
    __slots__ = ("w", "r")

    def __init__(self):
        self.w = None
        self.r = []


class T:
    def __init__(self, t, psum=False):
        self.t = t
        self.b = Buf()
        self.psum = psum

    def __getitem__(self, k):
        return self.t[k]


class Prog:
    def __init__(self, debug=False, as_input=()):
        self.as_input = set(as_input)
        self.nc = bass.Bass("TRN2", target_bir_lowering=False)
        nc = self.nc
        self.debug = debug
        self.eng = {"pe": nc.tensor, "act": nc.scalar, "dve": nc.vector, "pool": nc.gpsimd, "sp": nc.sync}
        self.esem = {k: nc.alloc_semaphore("sem_" + k) for k in ("pe", "act", "dve", "pool")}
        self.ecnt = {k: 0 for k in self.esem}
        self.waited = {k: {} for k in self.eng}
        self.dsems = [nc.alloc_semaphore("dsem%d" % i) for i in range(48)]
        self.dcnt = [0] * len(self.dsems)
        self.dnext = 0
        self.semid = {}
        self.n_inst = 0
        self.out_events = []
        self.scopes = []
        self.uid = 0

    def sb(self, name, shape, dt=F32):
        self.uid += 1
        name = "%s_%d" % (name, self.uid)
        if self.scopes:
            return T(self.scopes[-1].enter_context(self.nc.sbuf_tensor(name, list(shape), dt)))
        return T(self.nc.alloc_sbuf_tensor(name, list(shape), dt))

    def ps(self, name, shape, dt=F32):
        self.uid += 1
        name = "%s_%d" % (name, self.uid)
        if self.scopes:
            return T(self.scopes[-1].enter_context(self.nc.psum_tensor(name, list(shape), dt)), psum=True)
        return T(self.nc.alloc_psum_tensor(name, list(shape), dt), psum=True)

    def scope(self):
        return _Scope(self)

    def barrier(self):
        evs = [(self.dsems[i], self.dcnt[i]) for i in range(len(self.dsems)) if self.dcnt[i] > 0]
        evs += [(self.esem[k], self.ecnt[k]) for k in self.esem if self.ecnt[k] > 0]
        for en in self.eng:
            self._wait(en, evs)

    def dram(self, name, shape, dt=F32, kind="Internal"):
        if self.debug and kind == "Internal":
            kind = "ExternalInput" if name in self.as_input else "ExternalOutput"
        return T(self.nc.dram_tensor(name, list(shape), dt, kind=kind))

    def _wait(self, en, evs):
        e = self.eng[en]
        w = self.waited[en]
        best = {}
        for ev in evs:
            if ev is None:
                continue
            sem, val = ev
            k = id(sem)
            if w.get(k, 0) >= val:
                continue
            if k not in best or best[k][1] < val:
                best[k] = (sem, val)
        for k, (sem, val) in best.items():
            e.wait_ge(sem, val)
            w[k] = val

    def _deps(self, en, R, W):
        evs = []
        for b in R:
            evs.append(b.b.w)
            if b.psum:
                own = self.esem.get(en)
                evs.extend(ev for ev in b.b.r if ev[0] is not own)
        for b in W:
            evs.append(b.b.w)
            evs.extend(b.b.r)
        if en == "pe":
            s = self.esem["pe"]
            evs = [ev for ev in evs if ev is not None and ev[0] is not s]
        return evs

    def _commit(self, ev, R, W):
        for b in R:
            b.b.r.append(ev)
            if len(b.b.r) > 6:
                d = {}
                for s, v in b.b.r:
                    if id(s) not in d or d[id(s)][1] < v:
                        d[id(s)] = (s, v)
                b.b.r = list(d.values())
        for b in W:
            b.b.w = ev
            b.b.r = []

    def op(self, en, fn, R=(), W=()):
        self._wait(en, self._deps(en, R, W))
        inst = fn(self.eng[en])
        self.ecnt[en] += 1
        inst.then_inc(self.esem[en], 1)
        self._commit((self.esem[en], self.ecnt[en]), R, W)
        self.n_inst += 1
        return inst

    def dma(self, out, in_, R=(), W=(), q="sp", is_output=False, **kw):
        i = self.dnext
        self.dnext = (self.dnext + 1) % len(self.dsems)
        sem = self.dsems[i]
        evs = self._deps(q, R, W)
        if self.dcnt[i] > 0:
            evs.append((sem, self.dcnt[i]))
        self._wait(q, evs)
        inst = self.eng[q].dma_start(out=out, in_=in_, **kw)
        self.dcnt[i] += 16
        inst.then_inc(sem, 16)
        ev = (sem, self.dcnt[i])
        self._commit(ev, R, W)
        if is_output:
            self.out_events.append(ev)
        self.n_inst += 1
        return inst

    def finish(self):
        evs = [(self.dsems[i], self.dcnt[i]) for i in range(len(self.dsems)) if self.dcnt[i] > 0]
        evs += [(self.esem[k], self.ecnt[k]) for k in self.esem if self.ecnt[k] > 0]
        self._wait("sp", evs)

    def mm(self, out, lhsT, rhs, start, stop, R, W):
        return self.op("pe", lambda e: e.matmul(out, lhsT, rhs, start=start, stop=stop), R, W)

    def tr(self, out, in_, ident, R, W):
        return self.op("pe", lambda e: e.transpose(out, in_, ident), R, W)

    def act(self, out, in_, func, R, W, en="act", **kw):
        return self.op(en, lambda e: e.activation(out=out, in_=in_, func=func, **kw), R, W)

    def tt(self, out, in0, in1, op, R, W, en="dve"):
        return self.op(en, lambda e: e.tensor_tensor(out=out, in0=in0, in1=in1, op=op), R, W)

    def ts(self, out, in0, s1, s2, op0, op1, R, W, en="dve"):
        return self.op(en, lambda e: e.tensor_scalar(out=out, in0=in0, scalar1=s1, scalar2=s2, op0=op0, op1=op1), R, W)

    def stt(self, out, in0, scalar, in1, op0, op1, R, W):
        return self.op("dve", lambda e: e.scalar_tensor_tensor(out=out, in0=in0, scalar=scalar, in1=in1, op0=op0, op1=op1), R, W)

    def copy(self, out, in_, R, W, en="dve"):
        if en == "act":
            return self.op("act", lambda e: e.copy(out=out, in_=in_), R, W)
        return self.op(en, lambda e: e.tensor_copy(out=out, in_=in_), R, W)


class _Scope:
    def __init__(self, P):
        self.P = P

    def __enter__(self):
        import contextlib
        self.es = contextlib.ExitStack()
        self.P.scopes.append(self.es)
        return self

    def __exit__(self, *a):
        self.P.barrier()
        self.P.scopes.pop()
        self.es.close()
        return False


NFM = 6592
NTM = 672
FM_OFF = dict(cq=0, ckv=384, sqA=640, sqB=1152, skA=1664, skB=1792, dqkv=1920, gate=3456, krA=6528, krB=6560)
TM_OFF = dict(sv=0, dz=128, dbeta=640, da=656)
IN_OFF = dict(cq=0, ckv=384, kr=640, sq=672, sk=1184, sv=1312, dqkv=1440, dz=2976, dbeta=3488, da=3504, gate=3520)


def host_w_in_layout(w_in):
    o = IN_OFF
    idx = []
    idx += list(range(o["cq"], o["cq"] + 384))
    idx += list(range(o["ckv"], o["ckv"] + 256))
    idx += list(range(o["sq"], o["sq"] + 512))
    for h in range(8):
        b = o["sq"] + 64 * h
        idx += list(range(b + 32, b + 64)) + list(range(b, b + 32))
    idx += list(range(o["sk"], o["sk"] + 128))
    for h in range(2):
        b = o["sk"] + 64 * h
        idx += list(range(b + 32, b + 64)) + list(range(b, b + 32))
    idx += list(range(o["dqkv"], o["dqkv"] + 1536))
    idx += list(range(o["gate"], o["gate"] + 3072))
    idx += list(range(o["kr"], o["kr"] + 32))
    idx += list(range(o["kr"] + 16, o["kr"] + 32)) + list(range(o["kr"], o["kr"] + 16))
    assert len(idx) == NFM
    w_fm = np.ascontiguousarray(w_in[:, :, idx])
    idt = list(range(o["sv"], o["sv"] + 128)) + list(range(o["dz"], o["dz"] + 512)) + list(range(o["dbeta"], o["dbeta"] + 32))
    w_tm = np.ascontiguousarray(w_in[:, :, idt])
    return w_fm, w_tm


class Ctx:
    pass


def declare_io(P, C):
    nc = P.nc
    def inp(name, shape):
        return T(nc.dram_tensor(name, list(shape), F32, kind="ExternalInput"))
    C.x = inp("x", [NB, TL, D])
    C.ctx = inp("ctx", [NB, TC, D])
    C.cT = inp("cT", [128, 8, 3])
    C.w_mod = inp("w_mod", [DEPTH, D, 6 * D])
    C.b_mod = inp("b_mod", [DEPTH, 6 * D])
    C.norm1_g = inp("norm1_g", [DEPTH, D])
    C.norm2_g = inp("norm2_g", [DEPTH, D])
    C.w_fm = inp("w_fm", [DEPTH, D, NFM])
    C.w_tm = inp("w_tm", [DEPTH, D, NTM])
    C.ident = inp("ident", [128, 128])
    C.mod = P.dram("mod", [DEPTH, 3, 6 * D])
    C.proj = P.dram("proj", [NB, NFM, TA])
    C.projT = P.dram("projT", [NB, TA, NTM])


def stage_consts(P, C):
    C.ident_sb = P.sb("ident_sb", [128, 128])
    P.dma(C.ident_sb[:], C.ident[:, :], R=[C.ident], W=[C.ident_sb])
    C.ones_sb = P.sb("ones_sb", [128, 128])
    P.op("dve", lambda e: e.memset(C.ones_sb[:], 1.0), W=[C.ones_sb])


def stage_mod(P, C, l):
    with P.scope():
        _stage_mod(P, C, l)


def _stage_mod(P, C, l):
    C.scT = P.sb("scT", [128, 8, 3])
    C.modrow = P.sb("modrow", [3, 6 * D])
    C.wm = [P.sb("wm%d" % i, [128, 8, 512]) for i in range(2)]
    C.bm = P.sb("bm", [3, 6 * D])
    C.gbc = P.sb("gbc", [3, 2, D])
    C.ps_mod = [P.ps("ps_mod%d" % i, [128, 512]) for i in range(2)]
    P.dma(C.scT[:], C.cT[:, :, :], R=[C.cT], W=[C.scT])
    P.act(C.scT[:], C.scT[:], AF.Silu, R=[C.scT], W=[C.scT])
    P.dma(C.bm[:], C.b_mod[l, :].partition_broadcast(3), R=[C.b_mod], W=[C.bm])
    P.dma(C.gbc[:, 0, :], C.norm1_g[l, :].partition_broadcast(3), R=[C.norm1_g], W=[C.gbc])
    P.dma(C.gbc[:, 1, :], C.norm2_g[l, :].partition_broadcast(3), R=[C.norm2_g], W=[C.gbc])
    wv = C.w_mod.t[l].rearrange("(c p) n -> p c n", p=128)
    for j in range(12):
        wt = C.wm[j % 2]
        ps = C.ps_mod[j % 2]
        P.dma(wt[:], wv[:, :, j * 512:(j + 1) * 512], R=[C.w_mod], W=[wt])
        for c in range(8):
            P.mm(ps[0:3, :], C.scT[:, c, :], wt[:, c, :], c == 0, c == 7, R=[C.scT, wt], W=[ps])
        P.tt(C.modrow[:, j * 512:(j + 1) * 512], ps[0:3, :], C.bm[:, j * 512:(j + 1) * 512], ALU.add, R=[ps, C.bm], W=[C.modrow])
    for slot, gi in ((1, 0), (4, 1)):
        sl = C.modrow[:, slot * D:(slot + 1) * D]
        P.stt(sl, sl, 1.0, C.gbc[:, gi, :], ALU.add, ALU.mult, R=[C.modrow, C.gbc], W=[C.modrow])
    P.dma(C.mod[l], C.modrow[:], R=[C.modrow], W=[C.mod])


def token_src(C, l, b, t0, n):
    if l == 0:
        if t0 < TC:
            return C.ctx, C.ctx[b, t0:t0 + n, :]
        return C.x, C.x[b, t0 - TC:t0 - TC + n, :]
    return C.xres, C.xres[b, t0:t0 + n, :]


def blocks():
    out = [(0, TC)]
    for i in range(TL // 512):
        out.append((TC + i * 512, 512))
    return out


def load_mod_bc(P, C, l, row, slots, dst):
    for s, d in zip(slots, dst):
        P.dma(d[:], C.mod[l, row, s * D:(s + 1) * D].partition_broadcast(128), R=[C.mod], W=[d])


def norm_mod_T(P, C, src_t, src_ap, A_bc, sh_bc, hT, col0, it):
    xt = C.xt[it % 2]
    ht = C.ht[it % 2]
    st = C.st[it % 2]
    ps = C.ps_tr[it % 2]
    P.dma(xt[:], src_ap, R=[src_t], W=[xt])
    P.act(ht[:], xt[:], AF.Square, R=[xt], W=[ht, st], accum_out=st[:, 0:1])
    P.ts(st[:, 1:2], st[:, 0:1], 1.0 / D, EPS, ALU.mult, ALU.add, R=[st], W=[st])
    P.act(st[:, 2:3], st[:, 1:2], AF.Sqrt, R=[st], W=[st])
    P.op("dve", lambda e: e.reciprocal(out=st[:, 3:4], in_=st[:, 2:3]), R=[st], W=[st])
    P.stt(ht[:], xt[:], st[:, 3:4], A_bc[:], ALU.mult, ALU.mult, R=[xt, st, A_bc], W=[ht])
    P.tt(ht[:], ht[:], sh_bc[:], ALU.add, R=[ht, sh_bc], W=[ht])
    for c in range(8):
        P.tr(ps[:, c * 128:(c + 1) * 128], ht[:, c * 128:(c + 1) * 128], C.ident_sb[:], R=[ht, C.ident_sb], W=[ps])
    P.copy(hT[:, :, col0:col0 + 128], ps[:].rearrange("p (c t) -> p c t", c=8), R=[ps], W=[hT], en="act" if it % 2 else "dve")


def stage_proj(P, C, l, do_blocks=None):
    with P.scope():
        _stage_proj(P, C, l, do_blocks)


def _stage_proj(P, C, l, do_blocks=None):
    C.xt = [P.sb("xt%d" % i, [128, D]) for i in range(2)]
    C.ht = [P.sb("ht%d" % i, [128, D]) for i in range(2)]
    C.st = [P.sb("st%d" % i, [128, 4]) for i in range(2)]
    C.ps_tr = [P.ps("ps_tr%d" % i, [128, 1024]) for i in range(2)]
    C.hT = [P.sb("hT%d" % i, [128, 8, 512]) for i in range(2)]
    C.A_bc = [P.sb("A_bc%d" % i, [128, D]) for i in range(3)]
    C.sh_bc = [P.sb("sh_bc%d" % i, [128, D]) for i in range(3)]
    C.wfm = [P.sb("wfm%d" % i, [128, 8, 512]) for i in range(2)]
    C.wtm = P.sb("wtm", [128, 8, NTM])
    C.ps_mm = [P.ps("ps_mm%d" % i, [128, 512]) for i in range(2)]
    C.ot = [P.sb("ot%d" % i, [128, 512]) for i in range(3)]
    for row in range(3):
        load_mod_bc(P, C, l, row, (1, 0), (C.A_bc[row], C.sh_bc[row]))
    P.dma(C.wtm[:], C.w_tm.t[l].rearrange("(c p) n -> p c n", p=128), R=[C.w_tm], W=[C.wtm])
    wv = C.w_fm.t[l].rearrange("(c p) n -> p c n", p=128)
    it = 0
    ib = 0
    ig = 0
    io = 0
    for b in range(NB):
        for (t0, n) in blocks():
            if do_blocks is not None and (b, t0) not in do_blocks:
                continue
            row = 2 if t0 < TC else b
            hT = C.hT[ib % 2]
            ib += 1
            for i in range(n // 128):
                src_t, src_ap = token_src(C, l, b, t0 + i * 128, 128)
                norm_mod_T(P, C, src_t, src_ap, C.A_bc[row], C.sh_bc[row], hT, i * 128, it)
                it += 1
            for g in range((NFM + 511) // 512):
                c0 = g * 512
                cw = min(512, NFM - c0)
                wt = C.wfm[ig % 2]
                ig += 1
                P.dma(wt[:, :, 0:cw], wv[:, :, c0:c0 + cw], R=[C.w_fm], W=[wt])
                for j in range((cw + 127) // 128):
                    m = min(128, cw - j * 128)
                    ps = C.ps_mm[io % 2]
                    ot = C.ot[io % 3]
                    for c in range(8):
                        P.mm(ps[0:m, 0:n], wt[:, c, j * 128:j * 128 + m], hT[:, c, 0:n], c == 0, c == 7, R=[wt, hT], W=[ps])
                    P.copy(ot[0:m, 0:n], ps[0:m, 0:n], R=[ps], W=[ot], en="act" if io % 2 else "dve")
                    r0 = c0 + j * 128
                    P.dma(C.proj[b, r0:r0 + m, t0:t0 + n], ot[0:m, 0:n], R=[ot], W=[], q="act")
                    io += 1
            for i in range(n // 128):
                for (q0, qw) in ((0, 512), (512, NTM - 512)):
                    ps = C.ps_mm[io % 2]
                    ot = C.ot[io % 3]
                    for c in range(8):
                        P.mm(ps[:, 0:qw], hT[:, c, i * 128:(i + 1) * 128], C.wtm[:, c, q0:q0 + qw], c == 0, c == 7, R=[C.wtm, hT], W=[ps])
                    P.copy(ot[:, 0:qw], ps[:, 0:qw], R=[ps], W=[ot], en="act" if io % 2 else "dve")
                    P.dma(C.projT[b, t0 + i * 128:t0 + (i + 1) * 128, q0:q0 + qw], ot[:, 0:qw], R=[ot], W=[], q="act")
                    io += 1


DN_STOP = 0
def bc_mid(ap, n):
    return ap.unsqueeze(1).broadcast_to([ap.shape[0], n, ap.shape[1]])


def bc_last(ap, n):
    return ap.unsqueeze(2).broadcast_to([ap.shape[0], ap.shape[1], n])


def declare_dn(P, C):
    nc = P.nc
    def inp(name, shape):
        return T(nc.dram_tensor(name, list(shape), F32, kind="ExternalInput"))
    C.cw = inp("cw", [DEPTH, 128, 12, 5])
    C.dn_a_log = inp("dn_a_log", [DEPTH, 16])
    C.dn_dt_bias = inp("dn_dt_bias", [DEPTH, 16])
    C.dn_norm_g = inp("dn_norm_g", [DEPTH, 64])
    C.masks = inp("masks", [64, 6, 64])
    C.bd = inp("bd", [128, 128])
    C.dnf = P.dram("dnf", [NB, 1024, TA])
    C.dnt = P.dram("dnt", [NB, TA, 1536])
    C.gates = P.dram("gates", [NB, TA, 32])
    C.dno = P.dram("dno", [NB, 2, TA, 512])
    C.br = P.dram("br", [NB, 3, 512, TA])


def host_masks():
    a = np.arange(64)[:, None]
    b = np.arange(64)[None, :]
    m = np.stack([a <= b, a >= b, a > b, a < b, a == b, np.ones((64, 64), bool)], 1).astype(np.float32)
    bd = np.kron(np.eye(2), np.ones((64, 64))).astype(np.float32)
    return np.ascontiguousarray(m), bd


def stage_dn_prep(P, C, l, b, parts=(1, 2, 3, 4)):
    with P.scope():
        _stage_dn_prep(P, C, l, b, parts)


def _stage_dn_prep(P, C, l, b, parts=(1, 2, 3, 4)):
    WV = TA + 4
    ub = [P.sb("ub%d" % i, [128, TA + 8]) for i in range(2)]
    acc = [P.sb("acc%d" % i, [128, WV]) for i in range(2)]
    sqb = P.sb("sqb", [128, WV])
    rs = [P.sb("rs%d" % i, [128, 512]) for i in range(2)]
    cws = P.sb("cws", [128, 12, 5])
    bds = P.sb("bds", [128, 128])
    tmb = [P.sb("tmb%d" % i, [128, 4, 128]) for i in range(2)]
    ps_n = [P.ps("ps_n%d" % i, [128, 512]) for i in range(2)]
    ps_t = [P.ps("ps_t%d" % i, [128, 512]) for i in range(2)]
    P.dma(cws[:], C.cw[l], R=[C.cw], W=[cws])
    P.dma(bds[:], C.bd[:, :], R=[C.bd], W=[bds])
    for u in ub:
        P.op("dve", lambda e: e.memset(u[:], 0.0), W=[u])
    bg = P.sb("bg", [128, 18, 32])
    go = P.sb("go", [128, 18, 32])
    dtb = P.sb("dtb", [128, 16])
    nA = P.sb("nA", [128, 16])
    if 1 in parts:
      P.dma(bg[:], C.projT[b, :, 640:672].rearrange("(t p) f -> p t f", p=128), R=[C.projT], W=[bg])
      P.dma(dtb[:], C.dn_dt_bias[l, :].partition_broadcast(128), R=[C.dn_dt_bias], W=[dtb])
      P.dma(nA[:], C.dn_a_log[l, :].partition_broadcast(128), R=[C.dn_a_log], W=[nA])
      P.act(nA[:], nA[:], AF.Exp, R=[nA], W=[nA])
      P.ts(nA[:], nA[:], -1.0, None, ALU.mult, ALU.bypass, R=[nA], W=[nA])
      P.act(go[:, :, 0:16], bg[:, :, 0:16], AF.Sigmoid, R=[bg], W=[go])
      P.tt(bg[:, :, 16:32], bg[:, :, 16:32], bc_mid(dtb[:], 18), ALU.add, R=[bg, dtb], W=[bg])
      P.act(bg[:, :, 16:32], bg[:, :, 16:32], AF.Exp, R=[bg], W=[bg])
      P.act(bg[:, :, 16:32], bg[:, :, 16:32], AF.Ln, R=[bg], W=[bg], bias=1.0)
      P.tt(go[:, :, 16:32], bg[:, :, 16:32], bc_mid(nA[:], 18), ALU.mult, R=[bg, nA], W=[go])
      P.dma(C.gates[b].rearrange("(t p) f -> p t f", p=128), go[:], R=[go], W=[], q="act")
    r0 = FM_OFF["dqkv"]
    it = 0
    for c in (range(12) if 2 in parts else []):
        u = ub[c % 2]
        a = acc[c % 2]
        P.dma(u[:, 2:2 + TC], C.proj[b, r0 + c * 128:r0 + (c + 1) * 128, 0:TC], R=[C.proj], W=[u])
        P.dma(u[:, 6 + TC:6 + TA], C.proj[b, r0 + c * 128:r0 + (c + 1) * 128, TC:TA], R=[C.proj], W=[u])
        P.ts(a[:], u[:, 0:WV], cws[:, c, 0:1], None, ALU.mult, ALU.bypass, R=[u, cws], W=[a])
        for j in range(1, 5):
            P.stt(a[:], u[:, j:j + WV], cws[:, c, j:j + 1], a[:], ALU.mult, ALU.add, R=[u, cws, a], W=[a])
        P.act(a[:], a[:], AF.Silu, R=[a], W=[a])
        if c < 8 and 3 in parts:
            P.act(sqb[:], a[:], AF.Square, R=[a], W=[sqb])
            for k in range((WV + 511) // 512):
                n = min(512, WV - k * 512)
                ps = ps_n[k % 2]
                r = rs[k % 2]
                P.mm(ps[:, 0:n], bds[:], sqb[:, k * 512:k * 512 + n], True, True, R=[bds, sqb], W=[ps])
                P.ts(r[:, 0:n], ps[:, 0:n], 1.0, EPS, ALU.mult, ALU.add, R=[ps], W=[r])
                P.act(r[:, 0:n], r[:, 0:n], AF.Sqrt, R=[r], W=[r])
                P.op("dve", lambda e: e.reciprocal(out=r[:, 0:n], in_=r[:, 0:n]), R=[r], W=[r])
                P.stt(a[:, k * 512:k * 512 + n], a[:, k * 512:k * 512 + n], 0.125 if c < 4 else 1.0, r[:, 0:n], ALU.mult, ALU.mult, R=[a, r], W=[a])
            P.dma(C.dnf[b, c * 128:(c + 1) * 128, 0:TC], a[:, 0:TC], R=[a], W=[], q="act")
            P.dma(C.dnf[b, c * 128:(c + 1) * 128, TC:TA], a[:, TC + 4:WV], R=[a], W=[], q="act")
        for t0 in (range(0, 18, 4) if 4 in parts else []):
            nt = min(4, 18 - t0)
            ps = ps_t[it % 2]
            tb = tmb[it % 2]
            it += 1
            for k in range(nt):
                tile = t0 + k
                col = tile * 128 if tile < 2 else 4 + tile * 128
                P.tr(ps[:, k * 128:(k + 1) * 128], a[:, col:col + 128], C.ident_sb[:], R=[a, C.ident_sb], W=[ps])
            P.copy(tb[:, 0:nt, :], ps[:, 0:nt * 128].rearrange("p (t f) -> p t f", f=128), R=[ps], W=[tb], en="act" if it % 2 else "dve")
            P.dma(C.dnt[b, t0 * 128:(t0 + nt) * 128, c * 128:(c + 1) * 128].rearrange("(t p) f -> p t f", p=128), tb[:, 0:nt, :], R=[tb], W=[], q="act")


def stage_dn_scan(P, C, l, b, with_ctx_out, only_chunks=None):
    with P.scope():
        _stage_dn_scan(P, C, l, b, with_ctx_out, only_chunks)


def _stage_dn_scan(P, C, l, b, with_ctx_out, only_chunks):
    mk = P.sb("mk", [64, 6, 64])
    P.dma(mk[:], C.masks[:, :, :], R=[C.masks], W=[mk])
    LE, GE, GT, LT, I64, ONE = [mk[:, i, :] for i in range(6)]
    NBUF = 2
    def sbl(name, shape):
        return [P.sb(name + str(i), shape) for i in range(NBUF)]
    ktm = sbl("ktm", [64, 8, 64]); vtm = sbl("vtm", [64, 8, 64]); kT = sbl("kT", [64, 8, 64]); qT = sbl("qT", [64, 8, 64])
    gt = sbl("gt", [64, 32])
    sm = sbl("sm", [64, 8, 8])
    rd = sbl("rd", [64, 8, 64]); rdT = sbl("rdT", [64, 8, 64])
    decS = sbl("decS", [64, 8, 64]); decCT = sbl("decCT", [64, 8, 64])
    Ma = sbl("Ma", [64, 8, 64]); Mb = sbl("Mb", [64, 8, 64]); MTa = sbl("MTa", [64, 8, 64]); MTb = sbl("MTb", [64, 8, 64])
    Pa = sbl("Pa", [64, 8, 64]); Pb = sbl("Pb", [64, 8, 64])
    vb = sbl("vb", [64, 8, 64]); rw = sbl("rw", [64, 8, 64]); kdec = sbl("kdec", [64, 8, 64])
    u_ = sbl("u_", [64, 8, 64]); wT = sbl("wT", [64, 8, 64]); aT = sbl("aT", [64, 8, 64])
    vnew = sbl("vnew", [64, 8, 64]); ot = sbl("dno_t", [64, 8, 64]); tmp = sbl("dtmp", [64, 8, 64])
    Sl = [P.sb("Sst%d" % i, [64, 8, 64]) for i in range(2)]
    psb = [P.ps("dps%d" % i, [64, 512]) for i in range(6)]
    pssl = [P.ps("dpss%d" % i, [64, 16]) for i in range(2)]
    pc = [0]

    def nps():
        pc[0] += 1
        return psb[pc[0] % 6]

    def mmh(ps, lhs, rhs, Rl):
        for h in range(8):
            P.mm(ps[:, h * 64:(h + 1) * 64], lhs[:, h, :], rhs[:, h, :], True, True, R=Rl, W=[ps])

    def v3(t):
        return t[:].rearrange("p (h f) -> p h f", h=8)

    def stream(d):
        Minc, Mstr = (LE, GT) if d == 0 else (GE, LT)
        Sm = Mstr
        CT = Minc
        S = Sl[d]
        pss = pssl[d]
        k = d
        P.op("dve", lambda e: e.memset(S[:], 0.0), W=[S])
        order = list(range(4)) + list(range(4, 36)) if d == 0 else list(range(3, -1, -1)) + list(range(35, 3, -1))
        for cidx in order:
            if only_chunks is not None and cidx not in only_chunks:
                continue
            is_ctx = cidx < 4
            want_out = (not is_ctx) or with_ctx_out
            tok0 = cidx * 64
            P.dma(ktm[k][:], C.dnt[b, tok0:tok0 + 64, 512:1024].rearrange("t (h f) -> t h f", h=8), R=[C.dnt], W=[ktm[k]])
            P.dma(vtm[k][:], C.dnt[b, tok0:tok0 + 64, 1024:1536].rearrange("t (h f) -> t h f", h=8), R=[C.dnt], W=[vtm[k]])
            P.dma(kT[k][:], C.dnf[b, 512:1024, tok0:tok0 + 64].rearrange("(h f) t -> f h t", h=8), R=[C.dnf], W=[kT[k]])
            P.dma(qT[k][:], C.dnf[b, 0:512, tok0:tok0 + 64].rearrange("(h f) t -> f h t", h=8), R=[C.dnf], W=[qT[k]])
            P.dma(gt[k][:], C.gates[b, tok0:tok0 + 64, :], R=[C.gates], W=[gt[k]])
            beta = gt[k][:, d * 8:(d + 1) * 8]
            g = gt[k][:, 16 + d * 8:16 + (d + 1) * 8]
            s = sm[k]
            P.mm(pss[:, 0:8], Minc, g, True, True, R=[mk, gt[k]], W=[pss])
            P.mm(pss[:, 8:16], ONE, g, True, True, R=[mk, gt[k]], W=[pss])
            P.copy(s[:, 0:2, :], pss[:, 0:16].rearrange("p (a h) -> p a h", a=2), R=[pss], W=[s])
            P.act(s[:, 2:4, :], s[:, 0:2, :], AF.Exp, R=[s], W=[s])
            P.tt(s[:, 7, :], s[:, 1, :], s[:, 0, :], ALU.subtract, R=[s], W=[s])
            P.act(s[:, 4, :], s[:, 7, :], AF.Exp, R=[s], W=[s])
            P.tt(s[:, 5, :], beta, s[:, 2, :], ALU.mult, R=[s, gt[k]], W=[s])
            P.ts(s[:, 6, :], beta, -1.0, None, ALU.mult, ALU.bypass, R=[gt[k]], W=[s])
            yield
            P.tt(rd[k][:], bc_mid(Mstr, 8), bc_last(g, 64), ALU.mult, R=[mk, gt[k]], W=[rd[k]])
            P.tt(rdT[k][:], bc_mid(Minc, 8), bc_last(g, 64), ALU.mult, R=[mk, gt[k]], W=[rdT[k]])
            p1 = nps()
            P.mm(p1[:, :], Minc, rd[k][:].rearrange("p h f -> p (h f)"), True, True, R=[mk, rd[k]], W=[p1])
            P.act(decS[k][:].rearrange("p h f -> p (h f)"), p1[:, :], AF.Exp, R=[p1], W=[decS[k]])
            P.tt(decS[k][:], decS[k][:], bc_mid(Sm, 8), ALU.mult, R=[decS[k], mk], W=[decS[k]])
            yield
            p2 = nps()
            P.mm(p2[:, :], Mstr, rdT[k][:].rearrange("p h f -> p (h f)"), True, True, R=[mk, rdT[k]], W=[p2])
            P.act(decCT[k][:].rearrange("p h f -> p (h f)"), p2[:, :], AF.Exp, R=[p2], W=[decCT[k]])
            P.tt(decCT[k][:], decCT[k][:], bc_mid(CT, 8), ALU.mult, R=[decCT[k], mk], W=[decCT[k]])
            yield
            p3 = nps()
            mmh(p3, kT[k], kT[k], [kT[k]])
            MT, M, MT2, M2 = MTa[k], Ma[k], MTb[k], Mb[k]
            P.tt(MT[:], v3(p3), decS[k][:], ALU.mult, R=[p3, decS[k]], W=[MT])
            P.tt(MT[:], MT[:], bc_last(s[:, 6, :], 64), ALU.mult, R=[MT, s], W=[MT])
            yield
            p4 = nps()
            for h in range(8):
                P.tr(p4[:, h * 64:(h + 1) * 64], MT[:, h, :], I64, R=[MT, mk], W=[p4])
            P.copy(M[:], v3(p4), R=[p4], W=[M], en="act")
            yield
            Pc, Pn = Pa[k], Pb[k]
            P.tt(Pc[:], v3(p4), bc_mid(I64, 8), ALU.add, R=[p4, mk], W=[Pc])
            for lev in range(5):
                yield
                pa = nps()
                mmh(pa, M, MT, [M, MT])
                P.copy(MT2[:], v3(pa), R=[pa], W=[MT2], en="act")
                if lev < 4:
                    pb = nps()
                    mmh(pb, MT, M, [M, MT])
                    P.copy(M2[:], v3(pb), R=[pb], W=[M2], en="dve")
                yield
                pcx = nps()
                mmh(pcx, MT2, Pc, [MT2, Pc])
                P.tt(Pn[:], v3(pcx), Pc[:], ALU.add, R=[pcx, Pc], W=[Pn])
                Pc, Pn = Pn, Pc
                M, M2 = M2, M
                MT, MT2 = MT2, MT
            yield
            P.tt(vb[k][:], vtm[k][:], bc_last(beta, 64), ALU.mult, R=[vtm[k], gt[k]], W=[vb[k]])
            P.tt(rw[k][:], ktm[k][:], bc_last(s[:, 5, :], 64), ALU.mult, R=[ktm[k], s], W=[rw[k]])
            P.tt(kdec[k][:], ktm[k][:], bc_last(s[:, 4, :], 64), ALU.mult, R=[ktm[k], s], W=[kdec[k]])
            pu = nps()
            mmh(pu, Pc, vb[k], [Pc, vb[k]])
            P.copy(u_[k][:], v3(pu), R=[pu], W=[u_[k]], en="act")
            yield
            pw = nps()
            mmh(pw, rw[k], Pc, [Pc, rw[k]])
            P.copy(wT[k][:], v3(pw), R=[pw], W=[wT[k]], en="act")
            if want_out:
                pa2 = nps()
                mmh(pa2, kT[k], qT[k], [kT[k], qT[k]])
                P.tt(aT[k][:], v3(pa2), decCT[k][:], ALU.mult, R=[pa2, decCT[k]], W=[aT[k]])
            yield
            pws = nps()
            mmh(pws, wT[k], S, [wT[k], S])
            P.tt(vnew[k][:], u_[k][:], v3(pws), ALU.subtract, R=[u_[k], pws], W=[vnew[k]])
            if want_out:
                pq = nps()
                mmh(pq, qT[k], S, [qT[k], S])
                pv = nps()
                mmh(pv, aT[k], vnew[k], [aT[k], vnew[k]])
                P.tt(tmp[k][:], v3(pq), bc_last(s[:, 2, :], 64), ALU.mult, R=[pq, s], W=[tmp[k]])
                P.tt(ot[k][:], tmp[k][:], v3(pv), ALU.add, R=[tmp[k], pv], W=[ot[k]])
                P.dma(C.dno[b, d, tok0:tok0 + 64, :], ot[k][:].rearrange("p h f -> p (h f)"), R=[ot[k]], W=[], q="act")
            yield
            pk = nps()
            mmh(pk, kdec[k], vnew[k], [kdec[k], vnew[k]])
            P.tt(S[:], S[:], bc_last(s[:, 3, :], 64), ALU.mult, R=[S, s], W=[S])
            P.tt(S[:], S[:], v3(pk), ALU.add, R=[S, pk], W=[S])
            yield


    gens = [stream(0), stream(1)]
    alive = [True, True]
    while any(alive):
        for i, g in enumerate(gens):
            if alive[i]:
                try:
                    next(g)
                except StopIteration:
                    alive[i] = False


def stage_dn_out(P, C, l, b, with_ctx_out):
    with P.scope():
        _stage_dn_out(P, C, l, b, with_ctx_out)


def _stage_dn_out(P, C, l, b, with_ctx_out):
    o0 = [P.sb("o0_%d" % i, [128, 8, 64]) for i in range(2)]
    o1 = [P.sb("o1_%d" % i, [128, 8, 64]) for i in range(2)]
    zt = [P.sb("zt_%d" % i, [128, 8, 64]) for i in range(2)]
    sq = [P.sb("osq_%d" % i, [128, 8, 64]) for i in range(2)]
    ms = [P.sb("oms_%d" % i, [128, 8]) for i in range(2)]
    ng = P.sb("ong", [128, 64])
    ps = [P.ps("ops%d" % i, [128, 512]) for i in range(2)]
    oT = [P.sb("ooT%d" % i, [128, 4, 128]) for i in range(2)]
    P.dma(ng[:], C.dn_norm_g[l, :].partition_broadcast(128), R=[C.dn_norm_g], W=[ng])
    for t in range(18):
        if t < 2 and not with_ctx_out:
            continue
        k = t % 2
        rows = slice(t * 128, (t + 1) * 128)
        P.dma(o0[k][:].rearrange("p h f -> p (h f)"), C.dno[b, 0, rows, :], R=[C.dno], W=[o0[k]])
        P.dma(o1[k][:].rearrange("p h f -> p (h f)"), C.dno[b, 1, rows, :], R=[C.dno], W=[o1[k]])
        P.dma(zt[k][:].rearrange("p h f -> p (h f)"), C.projT[b, rows, 128:640], R=[C.projT], W=[zt[k]])
        P.tt(o0[k][:], o0[k][:], o1[k][:], ALU.add, R=[o0[k], o1[k]], W=[o0[k]])
        P.act(sq[k][:], o0[k][:], AF.Square, R=[o0[k]], W=[sq[k]])
        P.op("dve", lambda e: e.tensor_reduce(out=ms[k][:], in_=sq[k][:], axis=AX.X, op=ALU.add), R=[sq[k]], W=[ms[k]])
        P.ts(ms[k][:], ms[k][:], 1.0 / 64, EPS, ALU.mult, ALU.add, R=[ms[k]], W=[ms[k]])
        P.act(ms[k][:], ms[k][:], AF.Sqrt, R=[ms[k]], W=[ms[k]])
        P.op("dve", lambda e: e.reciprocal(out=ms[k][:], in_=ms[k][:]), R=[ms[k]], W=[ms[k]])
        P.tt(o0[k][:], o0[k][:], bc_last(ms[k][:], 64), ALU.mult, R=[o0[k], ms[k]], W=[o0[k]])
        P.tt(o0[k][:], o0[k][:], bc_mid(ng[:], 8), ALU.mult, R=[o0[k], ng], W=[o0[k]])
        P.act(zt[k][:], zt[k][:], AF.Silu, R=[zt[k]], W=[zt[k]])
        P.tt(o0[k][:], o0[k][:], zt[k][:], ALU.mult, R=[o0[k], zt[k]], W=[o0[k]])
        of = o0[k][:].rearrange("p h f -> p (h f)")
        for c in range(4):
            P.tr(ps[k][:, c * 128:(c + 1) * 128], of[:, c * 128:(c + 1) * 128], C.ident_sb[:], R=[o0[k], C.ident_sb], W=[ps[k]])
        P.copy(oT[k][:], ps[k][:].rearrange("p (c t) -> p c t", c=4), R=[ps[k]], W=[oT[k]], en="act")
        P.dma(C.br[b, 2, :, rows].rearrange("(c p) t -> p c t", p=128), oT[k][:], R=[oT[k]], W=[], q="act")


def host_shared(I):
    f = lambda a: np.ascontiguousarray(np.asarray(a, dtype=np.float32))
    w_fm, w_tm = host_w_in_layout(np.asarray(I["w_in"]))
    masks, bd = host_masks()
    L = DEPTH
    sh = dict(
        w_mod=f(I["w_mod"]), b_mod=f(I["b_mod"]), norm1_g=f(I["norm1_g"]), norm2_g=f(I["norm2_g"]),
        w_fm=f(w_fm), w_tm=f(w_tm), ident=np.eye(128, dtype=np.float32),
        cw=f(np.asarray(I["dn_conv_w"]).reshape(L, 5, 12, 128).transpose(0, 3, 2, 1)),
        dn_a_log=f(np.asarray(I["dn_a_log"]).reshape(L, 16)), dn_dt_bias=f(np.asarray(I["dn_dt_bias"]).reshape(L, 16)),
        dn_norm_g=f(I["dn_norm_g"]), masks=masks, bd=bd,
    )
    sh.update(host_attn(I))
    return sh


def host_core(I, core):
    b0 = core * NB
    cs = np.stack([np.asarray(I["c"])[b0], np.asarray(I["c"])[b0 + 1], np.asarray(I["c_ctx"])], 0)
    cT = np.ascontiguousarray(cs.T.reshape(8, 128, 3).transpose(1, 0, 2)).astype(np.float32)
    return dict(x=np.ascontiguousarray(np.asarray(I["x"])[b0:b0 + NB]), ctx=np.ascontiguousarray(np.asarray(I["ctx"])[b0:b0 + NB]), cT=cT)


def rope_tables_np(n_tok, rot_dim):
    rows = n_tok // 64
    row = np.broadcast_to(np.arange(rows)[:, None], (rows, 64)).reshape(-1).astype(np.float32)
    col = np.broadcast_to(np.arange(64)[None, :], (rows, 64)).reshape(-1).astype(np.float32)
    n_freq = rot_dim // 4
    inv_freq = (np.float32(10000.0) ** (-np.arange(n_freq, dtype=np.float32) / np.float32(n_freq))).astype(np.float32)
    ang = np.concatenate([row[:, None] * inv_freq, col[:, None] * inv_freq], axis=-1).astype(np.float32)
    cos, sin = np.cos(ang).astype(np.float32), np.sin(ang).astype(np.float32)
    CC = np.concatenate([cos.T, cos.T], 0)
    SS = np.concatenate([-sin.T, sin.T], 0)
    return np.ascontiguousarray(np.stack([CC, SS], 0))


def declare_attn(P, C):
    nc = P.nc
    def inp(name, shape):
        return T(nc.dram_tensor(name, list(shape), F32, kind="ExternalInput"))
    C.qg = inp("qg", [DEPTH, 128, 3])
    C.kvg = inp("kvg", [DEPTH, 128, 2])
    C.w_qn = inp("w_qn", [DEPTH, 384, 512])
    C.w_qrA = inp("w_qrA", [DEPTH, 384, 256])
    C.w_qrB = inp("w_qrB", [DEPTH, 384, 256])
    C.w_kn = inp("w_kn", [DEPTH, 256, 512])
    C.w_v = inp("w_v", [DEPTH, 256, 512])
    C.ropeM = inp("ropeM", [2, 32, TL])
    C.ropeS = inp("ropeS", [2, 64, TL])
    C.swam = inp("swam", [128, 2, 128])
    C.swa_sink = inp("swa_sink", [DEPTH, 8])


def host_attn(I):
    f = lambda a: np.ascontiguousarray(np.asarray(a, dtype=np.float32))
    L = DEPTH
    wq = np.asarray(I["mla_w_q_up"]).reshape(L, 384, 8, 96)
    wkv = np.asarray(I["mla_w_kv_up"]).reshape(L, 256, 8, 128)
    kk = np.arange(128)[:, None]
    qq = np.arange(128)[None, :]
    swam = np.stack([kk >= qq, kk <= qq], 1).astype(np.float32)
    return dict(
        qg=f(np.asarray(I["mla_q_norm_g"]).reshape(L, 3, 128).transpose(0, 2, 1)),
        kvg=f(np.asarray(I["mla_kv_norm_g"]).reshape(L, 2, 128).transpose(0, 2, 1)),
        w_qn=f(wq[..., :64].reshape(L, 384, 512)),
        w_qrA=f(wq[..., 64:96].reshape(L, 384, 256)),
        w_qrB=f(np.concatenate([wq[..., 80:96], wq[..., 64:80]], -1).reshape(L, 384, 256)),
        w_kn=f(wkv[..., :64].reshape(L, 256, 512)),
        w_v=f(wkv[..., 64:].reshape(L, 256, 512)),
        ropeM=rope_tables_np(TL, 32), ropeS=rope_tables_np(TL, 64), swam=f(swam), swa_sink=f(I["swa_sink"]),
    )


def col_blocks():
    return [(i * 512, min(512, TA - i * 512)) for i in range((TA + 511) // 512)]


def stage_mla(P, C, l, b, ctx_out):
    with P.scope():
        _stage_mla(P, C, l, b, ctx_out)


def _stage_mla(P, C, l, b, ctx_out):
    SCALE = float(96 ** -0.5)
    cqn = P.sb("cqn", [128, 3, TA]); ckvn = P.sb("ckvn", [128, 2, TA])
    sqt = P.sb("sqt", [128, 3, 512]); rs = P.sb("mrs", [128, 512])
    qg = P.sb("qg", [128, 3]); kvg = P.sb("kvg", [128, 2])
    wqn = P.sb("wqn", [128, 3, 512]); wqa = P.sb("wqa", [128, 3, 256]); wqb = P.sb("wqb", [128, 3, 256])
    wkn = P.sb("wkn", [128, 2, 512]); wv = P.sb("wv", [128, 2, 512])
    rope = P.sb("ropem", [32, 2, TL])
    krr = P.sb("krr", [32, TA]); krb = P.sb("krb", [32, TA])
    qn = P.sb("qn", [64, TA], BF16); qr = P.sb("qr", [32, TA]); qrb = P.sb("qrb", [32, TA]); kn = P.sb("kn", [64, TA], BF16)
    qr16 = P.sb("qr16", [32, TA], BF16); krr16 = P.sb("krr16", [32, TA], BF16); ones16 = P.sb("ones16", [128, 64], BF16)
    vh = P.sb("vh", [128, 18, 64], BF16); oT = P.sb("oT", [64, TA])
    pt = [P.sb("pt%d" % i, [128, 512], BF16) for i in range(3)]
    P.op("dve", lambda e: e.memset(ones16[:], 1.0), W=[ones16])
    rd = [P.sb("rdm%d" % i, [64, 512]) for i in range(2)]
    pA = [P.ps("pA%d" % i, [128, 512]) for i in range(2)]
    pO = [P.ps("pO%d" % i, [64, 512]) for i in range(2)]
    pD = [P.ps("pD%d" % i, [64, 512]) for i in range(2)]
    pX = [P.ps("pX%d" % i, [128, 512]) for i in range(2)]
    ix = [0]

    def npx():
        ix[0] += 1
        return pX[ix[0] % 2]

    P.dma(qg[:], C.qg[l], R=[C.qg], W=[qg]); P.dma(kvg[:], C.kvg[l], R=[C.kvg], W=[kvg])
    for (wt, src) in ((wqn, C.w_qn), (wqa, C.w_qrA), (wqb, C.w_qrB), (wkn, C.w_kn), (wv, C.w_v)):
        P.dma(wt[:], src.t[l].rearrange("(c p) n -> p c n", p=128), R=[src], W=[wt])
    P.dma(rope[:], C.ropeM[:, :, :].rearrange("a p t -> p a t"), R=[C.ropeM], W=[rope])
    CC, SS = rope[:, 0, :], rope[:, 1, :]
    for c in range(3):
        P.dma(cqn[:, c, :], C.proj[b, c * 128:(c + 1) * 128, :], R=[], W=[cqn])
    for c in range(2):
        P.dma(ckvn[:, c, :], C.proj[b, 384 + c * 128:384 + (c + 1) * 128, :], R=[], W=[ckvn])
    P.dma(krr[:], C.proj[b, FM_OFF["krA"]:FM_OFF["krA"] + 32, :], R=[], W=[krr])
    P.dma(krb[:], C.proj[b, FM_OFF["krB"]:FM_OFF["krB"] + 32, :], R=[], W=[krb])
    for (xt, nchunk, g, dim) in ((cqn, 3, qg, 384), (ckvn, 2, kvg, 256)):
        for (c0, n) in col_blocks():
            P.act(sqt[:, 0:nchunk, 0:n], xt[:, :, c0:c0 + n], AF.Square, R=[xt], W=[sqt])
            ps = npx()
            for c in range(nchunk):
                P.mm(ps[:, 0:n], C.ones_sb[:], sqt[:, c, 0:n], c == 0, c == nchunk - 1, R=[C.ones_sb, sqt], W=[ps])
            P.ts(rs[:, 0:n], ps[:, 0:n], 1.0 / dim, EPS, ALU.mult, ALU.add, R=[ps], W=[rs])
            P.act(rs[:, 0:n], rs[:, 0:n], AF.Sqrt, R=[rs], W=[rs])
            P.op("dve", lambda e: e.reciprocal(out=rs[:, 0:n], in_=rs[:, 0:n]), R=[rs], W=[rs])
            for c in range(nchunk):
                P.stt(xt[:, c, c0:c0 + n], xt[:, c, c0:c0 + n], g[:, c:c + 1], rs[:, 0:n], ALU.mult, ALU.mult, R=[xt, g, rs], W=[xt])
    P.tt(krr[:, TC:TA], krr[:, TC:TA], CC, ALU.mult, R=[krr, rope], W=[krr])
    P.tt(krb[:, TC:TA], krb[:, TC:TA], SS, ALU.mult, R=[krb, rope], W=[krb])
    P.tt(krr16[:, TC:TA], krr[:, TC:TA], krb[:, TC:TA], ALU.add, R=[krr, krb], W=[krr16])
    P.copy(krr16[:, 0:TC], krr[:, 0:TC], R=[krr], W=[krr16])
    ia = 0
    for h in range(8):
        for (c0, n) in col_blocks():
            ps = npx()
            for c in range(3):
                P.mm(ps[0:64, 0:n], wqn[:, c, h * 64:(h + 1) * 64], cqn[:, c, c0:c0 + n], c == 0, c == 2, R=[wqn, cqn], W=[ps])
            P.act(qn[:, c0:c0 + n], ps[0:64, 0:n], AF.Copy, R=[ps], W=[qn], scale=SCALE)
            ps = npx()
            for c in range(3):
                P.mm(ps[0:32, 0:n], wqa[:, c, h * 32:(h + 1) * 32], cqn[:, c, c0:c0 + n], c == 0, c == 2, R=[wqa, cqn], W=[ps])
            P.act(qr[:, c0:c0 + n], ps[0:32, 0:n], AF.Copy, R=[ps], W=[qr], scale=SCALE)
            ps = npx()
            for c in range(3):
                P.mm(ps[0:32, 0:n], wqb[:, c, h * 32:(h + 1) * 32], cqn[:, c, c0:c0 + n], c == 0, c == 2, R=[wqb, cqn], W=[ps])
            P.act(qrb[:, c0:c0 + n], ps[0:32, 0:n], AF.Copy, R=[ps], W=[qrb], scale=SCALE)
            ps = npx()
            for c in range(2):
                P.mm(ps[0:64, 0:n], wkn[:, c, h * 64:(h + 1) * 64], ckvn[:, c, c0:c0 + n], c == 0, c == 1, R=[wkn, ckvn], W=[ps])
            P.copy(kn[:, c0:c0 + n], ps[0:64, 0:n], R=[ps], W=[kn])
        P.tt(qr[:, TC:TA], qr[:, TC:TA], CC, ALU.mult, R=[qr, rope], W=[qr])
        P.tt(qrb[:, TC:TA], qrb[:, TC:TA], SS, ALU.mult, R=[qrb, rope], W=[qrb])
        P.tt(qr16[:, TC:TA], qr[:, TC:TA], qrb[:, TC:TA], ALU.add, R=[qr, qrb], W=[qr16])
        if ctx_out:
            P.copy(qr16[:, 0:TC], qr[:, 0:TC], R=[qr], W=[qr16])
        for t0 in range(0, 18, 8):
            nt = min(8, 18 - t0)
            ps = npx()
            for j in range(nt):
                for c in range(2):
                    P.mm(ps[:, j * 64:(j + 1) * 64], ckvn[:, c, (t0 + j) * 128:(t0 + j + 1) * 128], wv[:, c, h * 64:(h + 1) * 64], c == 0, c == 1, R=[wv, ckvn], W=[ps])
            P.copy(vh[:, t0:t0 + nt, :], ps[:, 0:nt * 64].rearrange("p (t f) -> p t f", f=64), R=[ps], W=[vh])
        qblocks = [(TC + i * 512, 512, 18) for i in range(4)]
        if ctx_out:
            qblocks.append((0, TC, 2))
        for (q0, n, nkt) in qblocks:
            po = pO[ia % 2]; pd = pD[ia % 2]; r = rd[ia % 2]
            ia += 1
            def sc_(kt):
                pa = pA[kt % 2]
                P.mm(pa[:, 0:n], kn[:, kt * 128:(kt + 1) * 128], qn[:, q0:q0 + n], True, False, R=[kn, qn], W=[pa])
                P.mm(pa[:, 0:n], krr16[:, kt * 128:(kt + 1) * 128], qr16[:, q0:q0 + n], False, True, R=[krr16, qr16], W=[pa])
            sc_(0)
            for kt in range(nkt):
                if kt + 1 < nkt:
                    sc_(kt + 1)
                pa = pA[kt % 2]
                p = pt[kt % 3]
                P.act(p[:, 0:n], pa[:, 0:n], AF.Exp, R=[pa], W=[p])
                P.mm(po[:, 0:n], vh[:, kt, :], p[:, 0:n], kt == 0, kt == nkt - 1, R=[vh, p], W=[po])
                P.mm(pd[:, 0:n], ones16[:, :], p[:, 0:n], kt == 0, kt == nkt - 1, R=[ones16, p], W=[pd])
            P.op("dve", lambda e: e.reciprocal(out=r[:, 0:n], in_=pd[:, 0:n]), R=[pd], W=[r])
            P.tt(oT[:, q0:q0 + n], po[:, 0:n], r[:, 0:n], ALU.mult, R=[po, r], W=[oT])
        c_lo = 0 if ctx_out else TC
        P.dma(C.br[b, 0, h * 64:(h + 1) * 64, c_lo:TA], oT[:, c_lo:TA], R=[oT], W=[], q="act")


def stage_swa(P, C, l, b, ctx_out):
    with P.scope():
        _stage_swa(P, C, l, b, ctx_out)


def _stage_swa(P, C, l, b, ctx_out):
    qA = P.sb("sqA", [64, 4, TA]); qB = P.sb("sqB", [64, 4, TA])
    kA = P.sb("skA", [64, TA]); kB = P.sb("skB", [64, TA])
    q16 = P.sb("sq16", [64, 4, TA], BF16); k16 = P.sb("sk16", [64, TA], BF16)
    vn32 = P.sb("svn32", [128, 18, 64]); vn = P.sb("svn", [128, 18, 64], BF16)
    ones16 = P.sb("sones16", [128, 64], BF16)
    P.op("dve", lambda e: e.memset(ones16[:], 1.0), W=[ones16])
    rope = P.sb("ropes", [64, 2, TL])
    msk = P.sb("swam", [128, 2, 128])
    snk = P.sb("snk", [64, 8])
    pt = [P.sb("spt%d" % i, [128, 4, 128], BF16) for i in range(3)]
    rd = [P.sb("srd%d" % i, [64, 4, 128]) for i in range(2)]
    ot = [P.sb("sot%d" % i, [64, 4, 128]) for i in range(2)]
    pA = [P.ps("spA%d" % i, [128, 512]) for i in range(2)]
    pO = [P.ps("spO%d" % i, [64, 512]) for i in range(2)]
    pD = [P.ps("spD%d" % i, [64, 512]) for i in range(2)]
    P.dma(rope[:], C.ropeS[:, :, :].rearrange("a p t -> p a t"), R=[C.ropeS], W=[rope])
    P.dma(msk[:], C.swam[:, :, :], R=[C.swam], W=[msk])
    P.dma(snk[:], C.swa_sink[l, :].partition_broadcast(64), R=[C.swa_sink], W=[snk])
    P.act(snk[:], snk[:], AF.Exp, R=[snk], W=[snk])
    CC, SS = rope[:, 0, :], rope[:, 1, :]
    ib = 0
    ip = 0
    for n in range(2):
        for hh in range(4):
            r0 = FM_OFF["sqA"] + (4 * n + hh) * 64
            P.dma(qA[:, hh, :], C.proj[b, r0:r0 + 64, :], R=[], W=[qA])
            r0 = FM_OFF["sqB"] + (4 * n + hh) * 64
            P.dma(qB[:, hh, :], C.proj[b, r0:r0 + 64, :], R=[], W=[qB])
        P.dma(kA[:], C.proj[b, FM_OFF["skA"] + n * 64:FM_OFF["skA"] + (n + 1) * 64, :], R=[], W=[kA])
        P.dma(kB[:], C.proj[b, FM_OFF["skB"] + n * 64:FM_OFF["skB"] + (n + 1) * 64, :], R=[], W=[kB])
        P.dma(vn32[:], C.projT[b, :, n * 64:(n + 1) * 64].rearrange("(t p) f -> p t f", p=128), R=[], W=[vn32])
        P.copy(vn[:], vn32[:], R=[vn32], W=[vn])
        P.tt(qA[:, :, TC:TA], qA[:, :, TC:TA], bc_mid(CC, 4), ALU.mult, R=[qA, rope], W=[qA])
        P.tt(qB[:, :, TC:TA], qB[:, :, TC:TA], bc_mid(SS, 4), ALU.mult, R=[qB, rope], W=[qB])
        P.tt(qA[:, :, TC:TA], qA[:, :, TC:TA], qB[:, :, TC:TA], ALU.add, R=[qA, qB], W=[qA])
        P.act(q16[:], qA[:], AF.Copy, R=[qA], W=[q16], scale=0.125)
        P.tt(kA[:, TC:TA], kA[:, TC:TA], CC, ALU.mult, R=[kA, rope], W=[kA])
        P.tt(kB[:, TC:TA], kB[:, TC:TA], SS, ALU.mult, R=[kB, rope], W=[kB])
        P.tt(k16[:, TC:TA], kA[:, TC:TA], kB[:, TC:TA], ALU.add, R=[kA, kB], W=[k16])
        P.copy(k16[:, 0:TC], kA[:, 0:TC], R=[kA], W=[k16])
        qblocks = []
        if ctx_out:
            qblocks += [(0, -1), (128, -1)]
        qblocks += [(TC + i * 128, i) for i in range(16)]
        for (q0, i) in qblocks:
            tiles = [(0, None), (1, None)]
            if i >= 0:
                if i - 1 >= 0:
                    tiles.append((2 + i - 1, 0))
                tiles.append((2 + i, None))
                if i + 1 <= 15:
                    tiles.append((2 + i + 1, 1))
            po = pO[ib % 2]; pd = pD[ib % 2]; r = rd[ib % 2]; o = ot[ib % 2]
            ib += 1
            def sc_(j):
                pa = pA[(ip + j) % 2]
                P.mm(pa[:].rearrange("p (h q) -> p h q", h=4), k16[:, tiles[j][0] * 128:(tiles[j][0] + 1) * 128], q16[:, :, q0:q0 + 128], True, True, R=[k16, q16], W=[pa])
            sc_(0)
            for ti, (kt, mi) in enumerate(tiles):
                if ti + 1 < len(tiles):
                    sc_(ti + 1)
                pa = pA[(ip + ti) % 2]; p = pt[(ip + ti) % 3]
                P.act(p[:], pa[:].rearrange("p (h q) -> p h q", h=4), AF.Exp, R=[pa], W=[p])
                if mi is not None:
                    P.tt(p[:], p[:], bc_mid(msk[:, mi, :], 4), ALU.mult, R=[p, msk], W=[p])
                pf = p[:].rearrange("p h q -> p (h q)")
                P.mm(po[:, :], vn[:, kt, :], pf, ti == 0, ti == len(tiles) - 1, R=[vn, p], W=[po])
                P.mm(pd[:, :], ones16[:, :], pf, ti == 0, ti == len(tiles) - 1, R=[ones16, p], W=[pd])
            ip += len(tiles)
            P.tt(r[:], pd[:].rearrange("p (h q) -> p h q", h=4), bc_last(snk[:, 4 * n:4 * n + 4], 128), ALU.add, R=[pd, snk], W=[r])
            P.op("dve", lambda e: e.reciprocal(out=r[:], in_=r[:]), R=[r], W=[r])
            P.tt(o[:], po[:].rearrange("p (h q) -> p h q", h=4), r[:], ALU.mult, R=[po, r], W=[o])
            P.dma(C.br[b, 1, n * 256:(n + 1) * 256, q0:q0 + 128].rearrange("(h f) t -> f h t", h=4), o[:], R=[o], W=[], q="act")


def declare_rest(P, C):
    nc = P.nc
    def inp(name, shape):
        return T(nc.dram_tensor(name, list(shape), F32, kind="ExternalInput"))
    C.w_branch = inp("w_branch", [DEPTH, 3, 512, D])
    C.w_out = inp("w_out", [DEPTH, D, D])
    C.router_w = inp("router_w", [DEPTH, D, 32])
    C.router_b = inp("router_b", [DEPTH, 32])
    C.exp_w_gu = inp("exp_w_gu", [DEPTH, 32, D, 2048])
    C.bgu = inp("bgu", [DEPTH, 128, 32, 2, 8])
    C.exp_w_dn = inp("exp_w_dn", [DEPTH, 32, D, D])
    C.exp_b_dn = inp("exp_b_dn", [DEPTH, 32, D])
    C.final_norm_g = inp("final_norm_g", [D])
    C.xres = P.dram("xres", [NB, TA, D])
    C.out = T(nc.dram_tensor("out", [NB, TL, D], F32, kind="ExternalOutput"))


def host_rest(I):
    f = lambda a: np.ascontiguousarray(np.asarray(a, dtype=np.float32))
    L = DEPTH
    bgu = np.asarray(I["exp_b_gu"]).reshape(L, 32, 8, 128, 2).transpose(0, 3, 1, 4, 2)
    return dict(w_branch=f(I["w_branch"]), w_out=f(I["w_out"]), router_w=f(I["router_w"]), router_b=f(I["router_b"]),
                exp_w_gu=f(I["exp_w_gu"]), bgu=f(bgu), exp_w_dn=f(I["exp_w_dn"]), exp_b_dn=f(I["exp_b_dn"]),
                final_norm_g=f(I["final_norm_g"]))


def stage_merge(P, C, l, b, ctx_out):
    with P.scope():
        _stage_merge(P, C, l, b, ctx_out)


def _stage_merge(P, C, l, b, ctx_out):
    wbr = P.sb("wbr", [128, 3, 4, D]); wout = P.sb("wout", [128, 8, D])
    brt = P.sb("brt", [128, 3, 4, 512])
    gch = [P.sb("gch%d" % i, [128, 512]) for i in range(3)]
    tmp = [P.sb("mtmp%d" % i, [128, 512]) for i in range(2)]
    yT = P.sb("yT", [128, 8, 512])
    g1 = [P.sb("g1bc%d" % i, [128, D]) for i in range(2)]
    xt = [P.sb("mxt%d" % i, [128, D]) for i in range(2)]
    pA = [P.ps("mpA%d" % i, [128, 512]) for i in range(3)]
    pB = [P.ps("mpB%d" % i, [128, 512]) for i in range(2)]
    P.dma(wbr[:].rearrange("p i c n -> p (i c) n"), C.w_branch.t[l].rearrange("i (c p) n -> p (i c) n", p=128), R=[C.w_branch], W=[wbr])
    P.dma(wout[:], C.w_out.t[l].rearrange("(c p) n -> p c n", p=128), R=[C.w_out], W=[wout])
    load_mod_bc(P, C, l, b, (2,), (g1[0],))
    load_mod_bc(P, C, l, 2, (2,), (g1[1],))
    ig = 0
    ix = 0
    for (t0, n) in blocks():
        if t0 < TC and not ctx_out:
            continue
        gbc = g1[1] if t0 < TC else g1[0]
        for i in range(3):
            P.dma(brt[:, i, :, 0:n], C.br[b, i, :, t0:t0 + n].rearrange("(c p) t -> p c t", p=128), R=[], W=[brt])
        for m in range(8):
            for i in range(3):
                gc = gch[ig % 3]; pa = pA[ig % 3]; tm = tmp[ig % 2]
                ig += 1
                r0 = FM_OFF["gate"] + i * D + m * 128
                P.dma(gc[:, 0:n], C.proj[b, r0:r0 + 128, t0:t0 + n], R=[], W=[gc])
                for c in range(4):
                    P.mm(pa[:, 0:n], wbr[:, i, c, m * 128:(m + 1) * 128], brt[:, i, c, 0:n], c == 0, c == 3, R=[wbr, brt], W=[pa])
                P.act(gc[:, 0:n], gc[:, 0:n], AF.Sigmoid, R=[gc], W=[gc])
                if i == 0:
                    P.tt(yT[:, m, 0:n], pa[:, 0:n], gc[:, 0:n], ALU.mult, R=[pa, gc], W=[yT])
                else:
                    P.tt(tm[:, 0:n], pa[:, 0:n], gc[:, 0:n], ALU.mult, R=[pa, gc], W=[tm])
                    P.tt(yT[:, m, 0:n], yT[:, m, 0:n], tm[:, 0:n], ALU.add, R=[yT, tm], W=[yT])
        for i in range(n // 128):
            x = xt[ix % 2]
            src_t, src_ap = token_src(C, l, b, t0 + i * 128, 128)
            P.dma(x[:], src_ap, R=[], W=[x])
            for half in range(2):
                pb = pB[(2 * ix + half) % 2]
                for m in range(8):
                    P.mm(pb[:, :], yT[:, m, i * 128:(i + 1) * 128], wout[:, m, half * 512:(half + 1) * 512], m == 0, m == 7, R=[yT, wout], W=[pb])
                tm = tmp[half]
                P.tt(tm[:], pb[:, :], gbc[:, half * 512:(half + 1) * 512], ALU.mult, R=[pb, gbc], W=[tm])
                P.tt(x[:, half * 512:(half + 1) * 512], x[:, half * 512:(half + 1) * 512], tm[:], ALU.add, R=[x, tm], W=[x])
            P.dma(C.xres[b, t0 + i * 128:t0 + (i + 1) * 128, :], x[:], R=[x], W=[], q="act")
            ix += 1


def stage_moe(P, C, l, b, ctx_out, n_exp=32):
    with P.scope():
        _stage_moe(P, C, l, b, ctx_out, n_exp)


def _stage_moe(P, C, l, b, ctx_out, n_exp):
    C.xt = [P.sb("xt%d" % i, [128, D]) for i in range(2)]
    C.ht = [P.sb("ht%d" % i, [128, D]) for i in range(2)]
    C.st = [P.sb("st%d" % i, [128, 4]) for i in range(2)]
    C.ps_tr = [P.ps("ps_tr%d" % i, [128, 1024]) for i in range(1)] * 2
    hT = P.sb("mhT", [128, 8, 512])
    hT16 = P.sb("mhT16", [128, 8, 512], BF16)
    modbc = [P.sb("modbc%d" % i, [128, D]) for i in range(3)]
    rw = P.sb("rw", [128, 8, 32]); rb = P.sb("rb", [128, 32])
    bgu = P.sb("bgu", [128, 32, 2, 8]); bdn = P.sb("bdn", [32, D])
    lg = P.sb("lg", [128, 32]); ex = P.sb("ex", [128, 32]); mk = P.sb("rmk", [128, 32]); t8 = P.sb("t8", [128, 8]); sc = P.sb("rsc", [128, 4])
    G = P.sb("G", [128, 4, 32]); GT = P.sb("GT", [32, 512])
    stg = [P.sb("stg%d" % i, [128, 8, 512]) for i in range(3)]
    wq = [P.sb("wq%d" % i, [128, 8, 512], BF16) for i in range(3)]
    wd = [P.sb("wd%d" % i, [128, 8, 512], BF16) for i in range(2)]
    actT = P.sb("actT", [128, 8, 512], BF16)
    acc = P.sb("acc", [128, 4, D])
    gs = [P.sb("gs%d" % i, [128, 512]) for i in range(2)]
    us = [P.sb("us%d" % i, [128, 512]) for i in range(2)]
    sg = [P.sb("sg%d" % i, [128, 512]) for i in range(2)]
    pR = P.ps("pR", [128, 512])
    pG = [P.ps("pG%d" % i, [128, 512]) for i in range(2)]
    pU = [P.ps("pU%d" % i, [128, 512]) for i in range(2)]
    pY = [P.ps("pY%d" % i, [128, 512]) for i in range(1)]
    P.dma(rw[:], C.router_w.t[l].rearrange("(c p) n -> p c n", p=128), R=[C.router_w], W=[rw])
    P.dma(rb[:], C.router_b[l, :].partition_broadcast(128), R=[C.router_b], W=[rb])
    P.dma(bgu[:], C.bgu[l], R=[C.bgu], W=[bgu])
    P.dma(bdn[:], C.exp_b_dn[l], R=[C.exp_b_dn], W=[bdn])
    it = 0
    ist = 0
    iq = 0
    idn = 0
    ie = 0
    last_r = None
    for (t0, n) in blocks():
        if t0 < TC and not ctx_out:
            continue
        r = 1 if t0 < TC else 0
        if r != last_r:
            load_mod_bc(P, C, l, 2 if r else b, (4, 3, 5), modbc)
            last_r = r
        A2, sh2, g2 = modbc
        nt = n // 128
        for i in range(nt):
            norm_mod_T(P, C, C.xres, C.xres[b, t0 + i * 128:t0 + (i + 1) * 128, :], A2, sh2, hT, i * 128, it)
            it += 1
        P.copy(hT16[:, :, 0:n], hT[:, :, 0:n], R=[hT], W=[hT16], en="pool")
        for i in range(nt):
            for c in range(8):
                P.mm(pR[:, 0:32], hT[:, c, i * 128:(i + 1) * 128], rw[:, c, :], c == 0, c == 7, R=[hT, rw], W=[pR])
            P.tt(lg[:], pR[:, 0:32], rb[:], ALU.add, R=[pR, rb], W=[lg])
            P.op("dve", lambda e: e.max(out=t8[:], in_=lg[:]), R=[lg], W=[t8])
            P.ts(mk[:], lg[:], t8[:, 3:4], None, ALU.is_ge, ALU.bypass, R=[lg, t8], W=[mk])
            P.ts(sc[:, 0:1], t8[:, 0:1], -1.0, None, ALU.mult, ALU.bypass, R=[t8], W=[sc])
            P.act(ex[:], lg[:], AF.Exp, R=[lg, sc], W=[ex], bias=sc[:, 0:1], scale=1.0)
            P.tt(ex[:], ex[:], mk[:], ALU.mult, R=[ex, mk], W=[ex])
            P.op("dve", lambda e: e.tensor_reduce(out=sc[:, 1:2], in_=ex[:], axis=AX.X, op=ALU.add), R=[ex], W=[sc])
            P.op("dve", lambda e: e.reciprocal(out=sc[:, 2:3], in_=sc[:, 1:2]), R=[sc], W=[sc])
            P.ts(G[:, i, :], ex[:], sc[:, 2:3], None, ALU.mult, ALU.bypass, R=[ex, sc], W=[G])
            P.tr(pR[0:32, 128:256], G[:, i, :], C.ident_sb[:], R=[G, C.ident_sb], W=[pR])
            P.copy(GT[:, i * 128:(i + 1) * 128], pR[0:32, 128:256], R=[pR], W=[GT])
        for i in range(nt):
            for half in range(2):
                P.mm(pR[:, :], GT[:, i * 128:(i + 1) * 128], bdn[:, half * 512:(half + 1) * 512], True, True, R=[GT, bdn], W=[pR])
                P.copy(acc[:, i, half * 512:(half + 1) * 512], pR[:, :], R=[pR], W=[acc])
        for e in range(n_exp):
            wgv = C.exp_w_gu.t[l, e].rearrange("(c p) n -> p c n", p=128)
            for qd in range(4):
                sgt = stg[ist % 3]
                ist += 1
                w = wq[iq % 3]
                iq += 1
                P.dma(sgt[:], wgv[:, :, qd * 512:(qd + 1) * 512], R=[C.exp_w_gu], W=[sgt])
                P.copy(w[:], sgt[:], R=[sgt], W=[w], en="act")
                for s in range(2):
                    fc = qd * 2 + s
                    pg = pG[ie % 2]; pu = pU[ie % 2]; g_ = gs[ie % 2]; u_ = us[ie % 2]; s_ = sg[ie % 2]
                    ie += 1
                    for c in range(8):
                        P.mm(pg[:, 0:n], w[:, c, s * 256:(s + 1) * 256:2], hT16[:, c, 0:n], c == 0, c == 7, R=[w, hT16], W=[pg])
                    for c in range(8):
                        P.mm(pu[:, 0:n], w[:, c, s * 256 + 1:(s + 1) * 256:2], hT16[:, c, 0:n], c == 0, c == 7, R=[w, hT16], W=[pu])
                    P.ts(g_[:, 0:n], pg[:, 0:n], bgu[:, e, 0, fc:fc + 1], 7.0, ALU.add, ALU.min, R=[pg, bgu], W=[g_])
                    P.ts(u_[:, 0:n], pu[:, 0:n], bgu[:, e, 1, fc:fc + 1], 7.0, ALU.add, ALU.min, R=[pu, bgu], W=[u_])
                    P.ts(u_[:, 0:n], u_[:, 0:n], -7.0, 1.0, ALU.max, ALU.add, R=[u_], W=[u_])
                    P.act(s_[:, 0:n], g_[:, 0:n], AF.Sigmoid, R=[g_], W=[s_], scale=1.702)
                    P.tt(g_[:, 0:n], g_[:, 0:n], s_[:, 0:n], ALU.mult, R=[g_, s_], W=[g_], en="pool")
                    P.tt(actT[:, fc, 0:n], g_[:, 0:n], u_[:, 0:n], ALU.mult, R=[g_, u_], W=[actT], en="pool")
            wdv = C.exp_w_dn.t[l, e].rearrange("(c p) n -> p c n", p=128)
            for half in range(2):
                sgt = stg[ist % 3]
                ist += 1
                w = wd[idn % 2]
                idn += 1
                P.dma(sgt[:], wdv[:, :, half * 512:(half + 1) * 512], R=[C.exp_w_dn], W=[sgt])
                P.copy(w[:], sgt[:], R=[sgt], W=[w], en="act")
                for i in range(nt):
                    py = pY[0]
                    for fc in range(8):
                        P.mm(py[:, :], actT[:, fc, i * 128:(i + 1) * 128], w[:, fc, :], fc == 0, fc == 7, R=[actT, w], W=[py])
                    a = acc[:, i, half * 512:(half + 1) * 512]
                    P.stt(a, py[:, :], G[:, i, e:e + 1], a, ALU.mult, ALU.add, R=[py, G, acc], W=[acc])
        for i in range(nt):
            x = C.xt[i % 2]
            rows = slice(t0 + i * 128, t0 + (i + 1) * 128)
            P.dma(x[:], C.xres[b, rows, :], R=[], W=[x])
            P.tt(acc[:, i, :], acc[:, i, :], g2[:], ALU.mult, R=[acc, g2], W=[acc])
            P.tt(x[:], x[:], acc[:, i, :], ALU.add, R=[x, acc], W=[x])
            P.dma(C.xres[b, rows, :], x[:], R=[x], W=[], q="act")


def stage_final(P, C, b):
    with P.scope():
        xt = [P.sb("fxt%d" % i, [128, D]) for i in range(2)]
        ht = [P.sb("fht%d" % i, [128, D]) for i in range(2)]
        st = [P.sb("fst%d" % i, [128, 4]) for i in range(2)]
        g = P.sb("fg", [128, D])
        P.dma(g[:], C.final_norm_g[:].partition_broadcast(128), R=[C.final_norm_g], W=[g])
        for i in range(TL // 128):
            x = xt[i % 2]; h = ht[i % 2]; s = st[i % 2]
            P.dma(x[:], C.xres[b, TC + i * 128:TC + (i + 1) * 128, :], R=[], W=[x])
            P.act(h[:], x[:], AF.Square, R=[x], W=[h, s], accum_out=s[:, 0:1])
            P.ts(s[:, 1:2], s[:, 0:1], 1.0 / D, EPS, ALU.mult, ALU.add, R=[s], W=[s])
            P.act(s[:, 2:3], s[:, 1:2], AF.Sqrt, R=[s], W=[s])
            P.op("dve", lambda e: e.reciprocal(out=s[:, 3:4], in_=s[:, 2:3]), R=[s], W=[s])
            P.stt(h[:], x[:], s[:, 3:4], g[:], ALU.mult, ALU.mult, R=[x, s, g], W=[h])
            P.dma(C.out[b, i * 128:(i + 1) * 128, :], h[:], R=[h], W=[], q="act", is_output=True)


def build_program(debug=False):
    P = Prog(debug=debug)
    C = Ctx()
    declare_io(P, C); declare_dn(P, C); declare_attn(P, C); declare_rest(P, C)
    stage_consts(P, C)
    for l in range(DEPTH):
        ctx_out = l < DEPTH - 1
        stage_mod(P, C, l)
        stage_proj(P, C, l)
        for b in range(NB):
            stage_mla(P, C, l, b, ctx_out)
            stage_swa(P, C, l, b, ctx_out)
            stage_dn_prep(P, C, l, b)
            stage_dn_scan(P, C, l, b, ctx_out)
            stage_dn_out(P, C, l, b, ctx_out)
            stage_merge(P, C, l, b, ctx_out)
            stage_moe(P, C, l, b, ctx_out)
    for b in range(NB):
        stage_final(P, C, b)
    P.finish()
    return P, C


_CACHE = {}


def kernel(**inputs):
    I = {k: np.asarray(v) for k, v in inputs.items()}
    if "prog" not in _CACHE:
        _CACHE["prog"] = build_program()
    P, C = _CACHE["prog"]
    sh = host_shared(I)
    sh.update(host_rest(I))
    in_maps = []
    for core in range(NCORES):
        m = dict(sh)
        m.update(host_core(I, core))
        in_maps.append(m)
    res = run_bass_kernel_spmd(P.nc, in_maps, core_ids=list(range(NCORES)))
    out = np.concatenate([np.asarray(r["out"]) for r in res.results], axis=0)
    return out.astype(np.float32)
```

```python
import numpy as np
import concourse.bass as bass
import concourse.mybir as mybir
from concourse.bass_utils import run_bass_kernel_spmd
from concourse.alu_op_type import AluOpType as ALU

F32 = mybir.dt.float32
BF16 = mybir.dt.bfloat16
F32R = mybir.dt.float32r
F32R_ON = True
AF = mybir.ActivationFunctionType
AX = mybir.AxisListType

D = 1024
DEPTH = 2
NB = 2
TC = 256
TL = 2048
TA = TC + TL
EPS = 1e-6
NCORES = 8


class Buf:
    __slots__ = ("w", "r")

    def __init__(self):
        self.w = None
        self.r = []


class T:
    def __init__(self, t, psum=False):
        self.t = t
        self.b = Buf()
        self.psum = psum

    def __getitem__(self, k):
        return self.t[k]


class Prog:
    def __init__(self, debug=False, as_input=()):
        self.as_input = set(as_input)
        self.nc = bass.Bass("TRN2", target_bir_lowering=False)
        nc = self.nc
        self.debug = debug
        self.eng = {"pe": nc.tensor, "act": nc.scalar, "dve": nc.vector, "pool": nc.gpsimd, "sp": nc.sync}
        self.esem = {k: nc.alloc_semaphore("sem_" + k) for k in ("pe", "act", "dve", "pool")}
        self.ecnt = {k: 0 for k in self.esem}
        self.waited = {k: {} for k in self.eng}
        self.dsems = [nc.alloc_semaphore("dsem%d" % i) for i in range(48)]
        self.dcnt = [0] * len(self.dsems)
        self.dnext = 0
        self.semid = {}
        self.n_inst = 0
        self.out_events = []
        self.scopes = []
        self.uid = 0

    def sb(self, name, shape, dt=F32):
        self.uid += 1
        name = "%s_%d" % (name, self.uid)
        if self.scopes:
            return T(self.scopes[-1].enter_context(self.nc.sbuf_tensor(name, list(shape), dt)))
        return T(self.nc.alloc_sbuf_tensor(name, list(shape), dt))

    def ps(self, name, shape, dt=F32):
        self.uid += 1
        name = "%s_%d" % (name, self.uid)
        if self.scopes:
            return T(self.scopes[-1].enter_context(self.nc.psum_tensor(name, list(shape), dt)), psum=True)
        return T(self.nc.alloc_psum_tensor(name, list(shape), dt), psum=True)

    def scope(self):
        return _Scope(self)

    def barrier(self):
        evs = [(self.dsems[i], self.dcnt[i]) for i in range(len(self.dsems)) if self.dcnt[i] > 0]
        evs += [(self.esem[k], self.ecnt[k]) for k in self.esem if self.ecnt[k] > 0]
        for en in self.eng:
            self._wait(en, evs)

    def dram(self, name, shape, dt=F32, kind="Internal"):
        if self.debug and kind == "Internal":
            kind = "ExternalInput" if name in self.as_input else "ExternalOutput"
        return T(self.nc.dram_tensor(name, list(shape), dt, kind=kind))

    def _wait(self, en, evs):
        e = self.eng[en]
        w = self.waited[en]
        best = {}
        for ev in evs:
            if ev is None:
                continue
            sem, val = ev
            k = id(sem)
            if w.get(k, 0) >= val:
                continue
            if k not in best or best[k][1] < val:
                best[k] = (sem, val)
        for k, (sem, val) in best.items():
            e.wait_ge(sem, val)
            w[k] = val

    def _deps(self, en, R, W):
        evs = []
        for b in R:
            evs.append(b.b.w)
            if b.psum:
                own = self.esem.get(en)
                evs.extend(ev for ev in b.b.r if ev[0] is not own)
        for b in W:
            evs.append(b.b.w)
            evs.extend(b.b.r)
        if en == "pe":
            s = self.esem["pe"]
            evs = [ev for ev in evs if ev is not None and ev[0] is not s]
        return evs

    def _commit(self, ev, R, W):
        for b in R:
            b.b.r.append(ev)
            if len(b.b.r) > 6:
                d = {}
                for s, v in b.b.r:
                    if id(s) not in d or d[id(s)][1] < v:
                        d[id(s)] = (s, v)
                b.b.r = list(d.values())
        for b in W:
            b.b.w = ev
            b.b.r = []

    def op(self, en, fn, R=(), W=()):
        self._wait(en, self._deps(en, R, W))
        inst = fn(self.eng[en])
        self.ecnt[en] += 1
        inst.then_inc(self.esem[en], 1)
        self._commit((self.esem[en], self.ecnt[en]), R, W)
        self.n_inst += 1
        return inst

    def dma(self, out, in_, R=(), W=(), q="sp", is_output=False, **kw):
        i = self.dnext
        self.dnext = (self.dnext + 1) % len(self.dsems)
        sem = self.dsems[i]
        evs = self._deps(q, R, W)
        if self.dcnt[i] > 0:
            evs.append((sem, self.dcnt[i]))
        self._wait(q, evs)
        inst = self.eng[q].dma_start(out=out, in_=in_, **kw)
        self.dcnt[i] += 16
        inst.then_inc(sem, 16)
        ev = (sem, self.dcnt[i])
        self._commit(ev, R, W)
        if is_output:
            self.out_events.append(ev)
        self.n_inst += 1
        return inst

    def finish(self):
        evs = [(self.dsems[i], self.dcnt[i]) for i in range(len(self.dsems)) if self.dcnt[i] > 0]
        evs += [(self.esem[k], self.ecnt[k]) for k in self.esem if self.ecnt[k] > 0]
        self._wait("sp", evs)

    def mm(self, out, lhsT, rhs, start, stop, R, W, fast=False):
        if fast and F32R_ON and lhsT.dtype == F32 and rhs.dtype == F32:
            lhsT = lhsT.bitcast(F32R)
            rhs = rhs.bitcast(F32R)
        return self.op("pe", lambda e: e.matmul(out, lhsT, rhs, start=start, stop=stop), R, W)

    def tr(self, out, in_, ident, R, W):
        return self.op("pe", lambda e: e.transpose(out, in_, ident), R, W)

    def act(self, out, in_, func, R, W, en="act", **kw):
        return self.op(en, lambda e: e.activation(out=out, in_=in_, func=func, **kw), R, W)

    def tt(self, out, in0, in1, op, R, W, en="dve"):
        return self.op(en, lambda e: e.tensor_tensor(out=out, in0=in0, in1=in1, op=op), R, W)

    def ts(self, out, in0, s1, s2, op0, op1, R, W, en="dve"):
        return self.op(en, lambda e: e.tensor_scalar(out=out, in0=in0, scalar1=s1, scalar2=s2, op0=op0, op1=op1), R, W)

    def stt(self, out, in0, scalar, in1, op0, op1, R, W):
        return self.op("dve", lambda e: e.scalar_tensor_tensor(out=out, in0=in0, scalar=scalar, in1=in1, op0=op0, op1=op1), R, W)

    def copy(self, out, in_, R, W, en="dve"):
        if en == "act":
            return self.op("act", lambda e: e.copy(out=out, in_=in_), R, W)
        return self.op(en, lambda e: e.tensor_copy(out=out, in_=in_), R, W)


class _Scope:
    def __init__(self, P):
        self.P = P

    def __enter__(self):
        import contextlib
        self.es = contextlib.ExitStack()
        self.P.scopes.append(self.es)
        return self

    def __exit__(self, *a):
        self.P.barrier()
        self.P.scopes.pop()
        self.es.close()
        return False


NFM = 6592
NTM = 672
FM_OFF = dict(cq=0, ckv=384, sqA=640, sqB=1152, skA=1664, skB=1792, dqkv=1920, gate=3456, krA=6528, krB=6560)
TM_OFF = dict(sv=0, dz=128, dbeta=640, da=656)
IN_OFF = dict(cq=0, ckv=384, kr=640, sq=672, sk=1184, sv=1312, dqkv=1440, dz=2976, dbeta=3488, da=3504, gate=3520)


def host_w_in_layout(w_in):
    o = IN_OFF
    idx = []
    idx += list(range(o["cq"], o["cq"] + 384))
    idx += list(range(o["ckv"], o["ckv"] + 256))
    idx += list(range(o["sq"], o["sq"] + 512))
    for h in range(8):
        b = o["sq"] + 64 * h
        idx += list(range(b + 32, b + 64)) + list(range(b, b + 32))
    idx += list(range(o["sk"], o["sk"] + 128))
    for h in range(2):
        b = o["sk"] + 64 * h
        idx += list(range(b + 32, b + 64)) + list(range(b, b + 32))
    idx += list(range(o["dqkv"], o["dqkv"] + 1536))
    idx += list(range(o["gate"], o["gate"] + 3072))
    idx += list(range(o["kr"], o["kr"] + 32))
    idx += list(range(o["kr"] + 16, o["kr"] + 32)) + list(range(o["kr"], o["kr"] + 16))
    assert len(idx) == NFM
    w_fm = np.ascontiguousarray(w_in[:, :, idx])
    idt = list(range(o["sv"], o["sv"] + 128)) + list(range(o["dz"], o["dz"] + 512)) + list(range(o["dbeta"], o["dbeta"] + 32))
    w_tm = np.ascontiguousarray(w_in[:, :, idt])
    return w_fm, w_tm


class Ctx:
    pass


def declare_io(P, C):
    nc = P.nc
    def inp(name, shape):
        return T(nc.dram_tensor(name, list(shape), F32, kind="ExternalInput"))
    C.x = inp("x", [NB, TL, D])
    C.ctx = inp("ctx", [NB, TC, D])
    C.cT = inp("cT", [128, 8, 3])
    C.w_mod = inp("w_mod", [DEPTH, D, 6 * D])
    C.b_mod = inp("b_mod", [DEPTH, 6 * D])
    C.norm1_g = inp("norm1_g", [DEPTH, D])
    C.norm2_g = inp("norm2_g", [DEPTH, D])
    C.w_fm = inp("w_fm", [DEPTH, D, NFM])
    C.w_tm = inp("w_tm", [DEPTH, D, NTM])
    C.ident = inp("ident", [128, 128])
    C.mod = P.dram("mod", [DEPTH, 3, 6 * D])
    C.proj = P.dram("proj", [NB, NFM, TA])
    C.projT = P.dram("projT", [NB, TA, NTM])


def stage_consts(P, C):
    C.ident_sb = P.sb("ident_sb", [128, 128])
    P.dma(C.ident_sb[:], C.ident[:, :], R=[C.ident], W=[C.ident_sb])
    C.ones_sb = P.sb("ones_sb", [128, 128])
    P.op("dve", lambda e: e.memset(C.ones_sb[:], 1.0), W=[C.ones_sb])


def stage_mod(P, C, l):
    with P.scope():
        _stage_mod(P, C, l)


def _stage_mod(P, C, l):
    C.scT = P.sb("scT", [128, 8, 3])
    C.modrow = P.sb("modrow", [3, 6 * D])
    C.wm = [P.sb("wm%d" % i, [128, 8, 512]) for i in range(2)]
    C.bm = P.sb("bm", [3, 6 * D])
    C.gbc = P.sb("gbc", [3, 2, D])
    C.ps_mod = [P.ps("ps_mod%d" % i, [128, 512]) for i in range(2)]
    P.dma(C.scT[:], C.cT[:, :, :], R=[C.cT], W=[C.scT])
    P.act(C.scT[:], C.scT[:], AF.Silu, R=[C.scT], W=[C.scT])
    P.dma(C.bm[:], C.b_mod[l, :].partition_broadcast(3), R=[C.b_mod], W=[C.bm])
    P.dma(C.gbc[:, 0, :], C.norm1_g[l, :].partition_broadcast(3), R=[C.norm1_g], W=[C.gbc])
    P.dma(C.gbc[:, 1, :], C.norm2_g[l, :].partition_broadcast(3), R=[C.norm2_g], W=[C.gbc])
    wv = C.w_mod.t[l].rearrange("(c p) n -> p c n", p=128)
    for j in range(12):
        wt = C.wm[j % 2]
        ps = C.ps_mod[j % 2]
        P.dma(wt[:], wv[:, :, j * 512:(j + 1) * 512], R=[C.w_mod], W=[wt])
        for c in range(8):
            P.mm(ps[0:3, :], C.scT[:, c, :], wt[:, c, :], c == 0, c == 7, R=[C.scT, wt], W=[ps])
        P.tt(C.modrow[:, j * 512:(j + 1) * 512], ps[0:3, :], C.bm[:, j * 512:(j + 1) * 512], ALU.add, R=[ps, C.bm], W=[C.modrow])
    for slot, gi in ((1, 0), (4, 1)):
        sl = C.modrow[:, slot * D:(slot + 1) * D]
        P.stt(sl, sl, 1.0, C.gbc[:, gi, :], ALU.add, ALU.mult, R=[C.modrow, C.gbc], W=[C.modrow])
    P.dma(C.mod[l], C.modrow[:], R=[C.modrow], W=[C.mod])


def token_src(C, l, b, t0, n):
    if l == 0:
        if t0 < TC:
            return C.ctx, C.ctx[b, t0:t0 + n, :]
        return C.x, C.x[b, t0 - TC:t0 - TC + n, :]
    return C.xres, C.xres[b, t0:t0 + n, :]


def blocks():
    out = [(0, TC)]
    for i in range(TL // 512):
        out.append((TC + i * 512, 512))
    return out


def load_mod_bc(P, C, l, row, slots, dst):
    for s, d in zip(slots, dst):
        P.dma(d[:], C.mod[l, row, s * D:(s + 1) * D].partition_broadcast(128), R=[C.mod], W=[d])


def norm_mod_T(P, C, src_t, src_ap, A_bc, sh_bc, hT, col0, it):
    xt = C.xt[it % 2]
    ht = C.ht[it % 2]
    st = C.st[it % 2]
    ps = C.ps_tr[it % 2]
    P.dma(xt[:], src_ap, R=[src_t], W=[xt])
    P.act(ht[:], xt[:], AF.Square, R=[xt], W=[ht, st], accum_out=st[:, 0:1])
    P.ts(st[:, 1:2], st[:, 0:1], 1.0 / D, EPS, ALU.mult, ALU.add, R=[st], W=[st])
    P.act(st[:, 2:3], st[:, 1:2], AF.Sqrt, R=[st], W=[st])
    P.op("dve", lambda e: e.reciprocal(out=st[:, 3:4], in_=st[:, 2:3]), R=[st], W=[st])
    P.stt(ht[:], xt[:], st[:, 3:4], A_bc[:], ALU.mult, ALU.mult, R=[xt, st, A_bc], W=[ht])
    P.tt(ht[:], ht[:], sh_bc[:], ALU.add, R=[ht, sh_bc], W=[ht])
    for c in range(8):
        P.tr(ps[:, c * 128:(c + 1) * 128], ht[:, c * 128:(c + 1) * 128], C.ident_sb[:], R=[ht, C.ident_sb], W=[ps])
    P.copy(hT[:, :, col0:col0 + 128], ps[:].rearrange("p (c t) -> p c t", c=8), R=[ps], W=[hT], en="act" if it % 2 else "dve")


def stage_proj(P, C, l, do_blocks=None):
    with P.scope():
        _stage_proj(P, C, l, do_blocks)


def _stage_proj(P, C, l, do_blocks=None):
    C.xt = [P.sb("xt%d" % i, [128, D]) for i in range(2)]
    C.ht = [P.sb("ht%d" % i, [128, D]) for i in range(2)]
    C.st = [P.sb("st%d" % i, [128, 4]) for i in range(2)]
    C.ps_tr = [P.ps("ps_tr%d" % i, [128, 1024]) for i in range(2)]
    C.hT = [P.sb("hT%d" % i, [128, 8, 512], BF16) for i in range(2)]
    C.A_bc = [P.sb("A_bc%d" % i, [128, D]) for i in range(3)]
    C.sh_bc = [P.sb("sh_bc%d" % i, [128, D]) for i in range(3)]
    C.wfm32 = [P.sb("wfm32_%d" % i, [128, 8, 512]) for i in range(2)]
    C.wfm = [P.sb("wfm%d" % i, [128, 8, 512], BF16) for i in range(2)]
    C.wtm = P.sb("wtm", [128, 8, NTM], BF16)
    C.ps_mm = [P.ps("ps_mm%d" % i, [128, 512]) for i in range(2)]
    C.ot = [P.sb("ot%d" % i, [128, 512]) for i in range(3)]
    for row in range(3):
        load_mod_bc(P, C, l, row, (1, 0), (C.A_bc[row], C.sh_bc[row]))
    wtv = C.w_tm.t[l].rearrange("(c p) n -> p c n", p=128)
    for (q0, qw) in ((0, 512), (512, NTM - 512)):
        P.dma(C.wfm32[0][:, :, 0:qw], wtv[:, :, q0:q0 + qw], R=[C.w_tm], W=[C.wfm32[0]])
        P.copy(C.wtm[:, :, q0:q0 + qw], C.wfm32[0][:, :, 0:qw], R=[C.wfm32[0]], W=[C.wtm], en="act")
    wv = C.w_fm.t[l].rearrange("(c p) n -> p c n", p=128)
    it = 0
    ib = 0
    ig = 0
    io = 0
    for b in range(NB):
        for (t0, n) in blocks():
            if do_blocks is not None and (b, t0) not in do_blocks:
                continue
            row = 2 if t0 < TC else b
            hT = C.hT[ib % 2]
            ib += 1
            for i in range(n // 128):
                src_t, src_ap = token_src(C, l, b, t0 + i * 128, 128)
                norm_mod_T(P, C, src_t, src_ap, C.A_bc[row], C.sh_bc[row], hT, i * 128, it)
                it += 1
            for g in range((NFM + 511) // 512):
                c0 = g * 512
                cw = min(512, NFM - c0)
                wt = C.wfm[ig % 2]
                w32 = C.wfm32[ig % 2]
                P.dma(w32[:, :, 0:cw], wv[:, :, c0:c0 + cw], R=[C.w_fm], W=[w32])
                P.copy(wt[:, :, 0:cw], w32[:, :, 0:cw], R=[w32], W=[wt], en="act" if ig % 2 else "pool")
                ig += 1
                for j in range((cw + 127) // 128):
                    m = min(128, cw - j * 128)
                    ps = C.ps_mm[io % 2]
                    ot = C.ot[io % 3]
                    for c in range(8):
                        P.mm(ps[0:m, 0:n], wt[:, c, j * 128:j * 128 + m], hT[:, c, 0:n], c == 0, c == 7, R=[wt, hT], W=[ps])
                    P.copy(ot[0:m, 0:n], ps[0:m, 0:n], R=[ps], W=[ot], en="act" if io % 2 else "dve")
                    r0 = c0 + j * 128
                    P.dma(C.proj[b, r0:r0 + m, t0:t0 + n], ot[0:m, 0:n], R=[ot], W=[], q="act")
                    io += 1
            for i in range(n // 128):
                for (q0, qw) in ((0, 512), (512, NTM - 512)):
                    ps = C.ps_mm[io % 2]
                    ot = C.ot[io % 3]
                    for c in range(8):
                        P.mm(ps[:, 0:qw], hT[:, c, i * 128:(i + 1) * 128], C.wtm[:, c, q0:q0 + qw], c == 0, c == 7, R=[C.wtm, hT], W=[ps])
                    P.copy(ot[:, 0:qw], ps[:, 0:qw], R=[ps], W=[ot], en="act" if io % 2 else "dve")
                    P.dma(C.projT[b, t0 + i * 128:t0 + (i + 1) * 128, q0:q0 + qw], ot[:, 0:qw], R=[ot], W=[], q="act")
                    io += 1


DN_STOP = 0
def bc_mid(ap, n):
    return ap.unsqueeze(1).broadcast_to([ap.shape[0], n, ap.shape[1]])


def bc_last(ap, n):
    return ap.unsqueeze(2).broadcast_to([ap.shape[0], ap.shape[1], n])


def declare_dn(P, C):
    nc = P.nc
    def inp(name, shape):
        return T(nc.dram_tensor(name, list(shape), F32, kind="ExternalInput"))
    C.cw = inp("cw", [DEPTH, 128, 12, 5])
    C.dn_a_log = inp("dn_a_log", [DEPTH, 16])
    C.dn_dt_bias = inp("dn_dt_bias", [DEPTH, 16])
    C.dn_norm_g = inp("dn_norm_g", [DEPTH, 64])
    C.masks = inp("masks", [64, 6, 64])
    C.bd = inp("bd", [128, 128])
    C.dnf = P.dram("dnf", [NB, 1024, TA])
    C.dnt = P.dram("dnt", [NB, TA, 1536])
    C.gates = P.dram("gates", [NB, TA, 32])
    C.dno = P.dram("dno", [NB, 2, TA, 512])
    C.br = P.dram("br", [NB, 3, 512, TA])


def host_masks():
    a = np.arange(64)[:, None]
    b = np.arange(64)[None, :]
    m = np.stack([a <= b, a >= b, a > b, a < b, a == b, np.ones((64, 64), bool)], 1).astype(np.float32)
    bd = np.kron(np.eye(2), np.ones((64, 64))).astype(np.float32)
    return np.ascontiguousarray(m), bd


def stage_dn_prep(P, C, l, b, parts=(1, 2, 3, 4)):
    with P.scope():
        _stage_dn_prep(P, C, l, b, parts)


def _stage_dn_prep(P, C, l, b, parts=(1, 2, 3, 4)):
    WV = TA + 4
    ub = [P.sb("ub%d" % i, [128, TA + 8]) for i in range(2)]
    acc = [P.sb("acc%d" % i, [128, WV]) for i in range(2)]
    sqb = P.sb("sqb", [128, WV])
    rs = [P.sb("rs%d" % i, [128, 512]) for i in range(2)]
    cws = P.sb("cws", [128, 12, 5])
    bds = P.sb("bds", [128, 128])
    tmb = [P.sb("tmb%d" % i, [128, 4, 128]) for i in range(2)]
    ps_n = [P.ps("ps_n%d" % i, [128, 512]) for i in range(2)]
    ps_t = [P.ps("ps_t%d" % i, [128, 512]) for i in range(2)]
    P.dma(cws[:], C.cw[l], R=[C.cw], W=[cws])
    P.dma(bds[:], C.bd[:, :], R=[C.bd], W=[bds])
    for u in ub:
        P.op("dve", lambda e: e.memset(u[:], 0.0), W=[u])
    bg = P.sb("bg", [128, 18, 32])
    go = P.sb("go", [128, 18, 32])
    dtb = P.sb("dtb", [128, 16])
    nA = P.sb("nA", [128, 16])
    if 1 in parts:
      P.dma(bg[:], C.projT[b, :, 640:672].rearrange("(t p) f -> p t f", p=128), R=[C.projT], W=[bg])
      P.dma(dtb[:], C.dn_dt_bias[l, :].partition_broadcast(128), R=[C.dn_dt_bias], W=[dtb])
      P.dma(nA[:], C.dn_a_log[l, :].partition_broadcast(128), R=[C.dn_a_log], W=[nA])
      P.act(nA[:], nA[:], AF.Exp, R=[nA], W=[nA])
      P.ts(nA[:], nA[:], -1.0, None, ALU.mult, ALU.bypass, R=[nA], W=[nA])
      P.act(go[:, :, 0:16], bg[:, :, 0:16], AF.Sigmoid, R=[bg], W=[go])
      P.tt(bg[:, :, 16:32], bg[:, :, 16:32], bc_mid(dtb[:], 18), ALU.add, R=[bg, dtb], W=[bg])
      P.act(bg[:, :, 16:32], bg[:, :, 16:32], AF.Exp, R=[bg], W=[bg])
      P.act(bg[:, :, 16:32], bg[:, :, 16:32], AF.Ln, R=[bg], W=[bg], bias=1.0)
      P.tt(go[:, :, 16:32], bg[:, :, 16:32], bc_mid(nA[:], 18), ALU.mult, R=[bg, nA], W=[go])
      P.dma(C.gates[b].rearrange("(t p) f -> p t f", p=128), go[:], R=[go], W=[], q="act")
    r0 = FM_OFF["dqkv"]
    it = 0
    for c in (range(12) if 2 in parts else []):
        u = ub[c % 2]
        a = acc[c % 2]
        P.dma(u[:, 2:2 + TC], C.proj[b, r0 + c * 128:r0 + (c + 1) * 128, 0:TC], R=[C.proj], W=[u])
        P.dma(u[:, 6 + TC:6 + TA], C.proj[b, r0 + c * 128:r0 + (c + 1) * 128, TC:TA], R=[C.proj], W=[u])
        P.ts(a[:], u[:, 0:WV], cws[:, c, 0:1], None, ALU.mult, ALU.bypass, R=[u, cws], W=[a])
        for j in range(1, 5):
            P.stt(a[:], u[:, j:j + WV], cws[:, c, j:j + 1], a[:], ALU.mult, ALU.add, R=[u, cws, a], W=[a])
        P.act(a[:], a[:], AF.Silu, R=[a], W=[a])
        if c < 8 and 3 in parts:
            P.act(sqb[:], a[:], AF.Square, R=[a], W=[sqb])
            for k in range((WV + 511) // 512):
                n = min(512, WV - k * 512)
                ps = ps_n[k % 2]
                r = rs[k % 2]
                P.mm(ps[:, 0:n], bds[:], sqb[:, k * 512:k * 512 + n], True, True, R=[bds, sqb], W=[ps])
                P.ts(r[:, 0:n], ps[:, 0:n], 1.0, EPS, ALU.mult, ALU.add, R=[ps], W=[r])
                P.act(r[:, 0:n], r[:, 0:n], AF.Sqrt, R=[r], W=[r])
                P.op("dve", lambda e: e.reciprocal(out=r[:, 0:n], in_=r[:, 0:n]), R=[r], W=[r])
                P.stt(a[:, k * 512:k * 512 + n], a[:, k * 512:k * 512 + n], 0.125 if c < 4 else 1.0, r[:, 0:n], ALU.mult, ALU.mult, R=[a, r], W=[a])
            P.dma(C.dnf[b, c * 128:(c + 1) * 128, 0:TC], a[:, 0:TC], R=[a], W=[], q="act")
            P.dma(C.dnf[b, c * 128:(c + 1) * 128, TC:TA], a[:, TC + 4:WV], R=[a], W=[], q="act")
        for t0 in (range(0, 18, 4) if 4 in parts else []):
            nt = min(4, 18 - t0)
            ps = ps_t[it % 2]
            tb = tmb[it % 2]
            it += 1
            for k in range(nt):
                tile = t0 + k
                col = tile * 128 if tile < 2 else 4 + tile * 128
                P.tr(ps[:, k * 128:(k + 1) * 128], a[:, col:col + 128], C.ident_sb[:], R=[a, C.ident_sb], W=[ps])
            P.copy(tb[:, 0:nt, :], ps[:, 0:nt * 128].rearrange("p (t f) -> p t f", f=128), R=[ps], W=[tb], en="act" if it % 2 else "dve")
            P.dma(C.dnt[b, t0 * 128:(t0 + nt) * 128, c * 128:(c + 1) * 128].rearrange("(t p) f -> p t f", p=128), tb[:, 0:nt, :], R=[tb], W=[], q="act")


def stage_dn_scan(P, C, l, b, with_ctx_out, only_chunks=None):
    with P.scope():
        _stage_dn_scan(P, C, l, b, with_ctx_out, only_chunks)


def _stage_dn_scan(P, C, l, b, with_ctx_out, only_chunks):
    mk = P.sb("mk", [64, 6, 64])
    P.dma(mk[:], C.masks[:, :, :], R=[C.masks], W=[mk])
    LE, GE, GT, LT, I64, ONE = [mk[:, i, :] for i in range(6)]
    NBUF = 2
    def sbl(name, shape):
        return [P.sb(name + str(i), shape) for i in range(NBUF)]
    ktm = sbl("ktm", [64, 8, 64]); vtm = sbl("vtm", [64, 8, 64]); kT = sbl("kT", [64, 8, 64]); qT = sbl("qT", [64, 8, 64])
    gt = sbl("gt", [64, 32])
    sm = sbl("sm", [64, 8, 8])
    rd = sbl("rd", [64, 8, 64]); rdT = sbl("rdT", [64, 8, 64])
    decS = sbl("decS", [64, 8, 64]); decCT = sbl("decCT", [64, 8, 64])
    Ma = sbl("Ma", [64, 8, 64]); Mb = sbl("Mb", [64, 8, 64]); MTa = sbl("MTa", [64, 8, 64]); MTb = sbl("MTb", [64, 8, 64])
    Pa = sbl("Pa", [64, 8, 64]); Pb = sbl("Pb", [64, 8, 64])
    vb = sbl("vb", [64, 8, 64]); rw = sbl("rw", [64, 8, 64]); kdec = sbl("kdec", [64, 8, 64])
    u_ = sbl("u_", [64, 8, 64]); wT = sbl("wT", [64, 8, 64]); aT = sbl("aT", [64, 8, 64])
    vnew = sbl("vnew", [64, 8, 64]); ot = sbl("dno_t", [64, 8, 64]); tmp = sbl("dtmp", [64, 8, 64])
    Sl = [P.sb("Sst%d" % i, [64, 8, 64]) for i in range(2)]
    psb = [P.ps("dps%d" % i, [64, 512]) for i in range(6)]
    pssl = [P.ps("dpss%d" % i, [64, 16]) for i in range(2)]
    pc = [0]

    def nps():
        pc[0] += 1
        return psb[pc[0] % 6]

    def r32(ap):
        return ap.bitcast(F32R) if F32R_ON else ap

    zero = P.sb("dzero", [64, 8, 64])
    P.op("dve", lambda e: e.memset(zero[:], 0.0), W=[zero])

    def mmh(ps, lhs, rhs, Rl, fast=True):
        for h in range(8):
            P.mm(ps[:, h * 64:(h + 1) * 64], lhs[:, h, :], rhs[:, h, :], True, True, R=Rl, W=[ps], fast=fast)

    def v3(t):
        return t[:].rearrange("p (h f) -> p h f", h=8)

    def stream(d):
        Minc, Mstr = (LE, GT) if d == 0 else (GE, LT)
        Sm = Mstr
        CT = Minc
        S = Sl[d]
        pss = pssl[d]
        k = d
        P.copy(r32(S[:]), zero[:], R=[zero], W=[S])
        order = list(range(4)) + list(range(4, 36)) if d == 0 else list(range(3, -1, -1)) + list(range(35, 3, -1))
        for cidx in order:
            if only_chunks is not None and cidx not in only_chunks:
                continue
            is_ctx = cidx < 4
            want_out = (not is_ctx) or with_ctx_out
            tok0 = cidx * 64
            P.dma(ktm[k][:], C.dnt[b, tok0:tok0 + 64, 512:1024].rearrange("t (h f) -> t h f", h=8), R=[C.dnt], W=[ktm[k]])
            P.dma(vtm[k][:], C.dnt[b, tok0:tok0 + 64, 1024:1536].rearrange("t (h f) -> t h f", h=8), R=[C.dnt], W=[vtm[k]])
            P.dma(kT[k][:], C.dnf[b, 512:1024, tok0:tok0 + 64].rearrange("(h f) t -> f h t", h=8), R=[C.dnf], W=[kT[k]])
            P.dma(qT[k][:], C.dnf[b, 0:512, tok0:tok0 + 64].rearrange("(h f) t -> f h t", h=8), R=[C.dnf], W=[qT[k]])
            P.dma(gt[k][:], C.gates[b, tok0:tok0 + 64, :], R=[C.gates], W=[gt[k]])
            beta = gt[k][:, d * 8:(d + 1) * 8]
            g = gt[k][:, 16 + d * 8:16 + (d + 1) * 8]
            s = sm[k]
            P.mm(pss[:, 0:8], Minc, g, True, True, R=[mk, gt[k]], W=[pss])
            P.mm(pss[:, 8:16], ONE, g, True, True, R=[mk, gt[k]], W=[pss])
            P.copy(s[:, 0:2, :], pss[:, 0:16].rearrange("p (a h) -> p a h", a=2), R=[pss], W=[s])
            P.act(s[:, 2:4, :], s[:, 0:2, :], AF.Exp, R=[s], W=[s])
            P.tt(s[:, 7, :], s[:, 1, :], s[:, 0, :], ALU.subtract, R=[s], W=[s])
            P.act(s[:, 4, :], s[:, 7, :], AF.Exp, R=[s], W=[s])
            P.tt(s[:, 5, :], beta, s[:, 2, :], ALU.mult, R=[s, gt[k]], W=[s])
            P.ts(s[:, 6, :], beta, -1.0, None, ALU.mult, ALU.bypass, R=[gt[k]], W=[s])
            yield
            P.tt(rd[k][:], bc_mid(Mstr, 8), bc_last(g, 64), ALU.mult, R=[mk, gt[k]], W=[rd[k]])
            P.tt(rdT[k][:], bc_mid(Minc, 8), bc_last(g, 64), ALU.mult, R=[mk, gt[k]], W=[rdT[k]])
            p1 = nps()
            P.mm(p1[:, :], Minc, rd[k][:].rearrange("p h f -> p (h f)"), True, True, R=[mk, rd[k]], W=[p1])
            P.act(decS[k][:].rearrange("p h f -> p (h f)"), p1[:, :], AF.Exp, R=[p1], W=[decS[k]])
            P.tt(decS[k][:], decS[k][:], bc_mid(Sm, 8), ALU.mult, R=[decS[k], mk], W=[decS[k]])
            yield
            p2 = nps()
            P.mm(p2[:, :], Mstr, rdT[k][:].rearrange("p h f -> p (h f)"), True, True, R=[mk, rdT[k]], W=[p2])
            P.act(decCT[k][:].rearrange("p h f -> p (h f)"), p2[:, :], AF.Exp, R=[p2], W=[decCT[k]])
            P.tt(decCT[k][:], decCT[k][:], bc_mid(CT, 8), ALU.mult, R=[decCT[k], mk], W=[decCT[k]])
            yield
            p3 = nps()
            mmh(p3, kT[k], kT[k], [kT[k]], fast=False)
            MT, M, MT2, M2 = MTa[k], Ma[k], MTb[k], Mb[k]
            P.tt(r32(MT[:]), v3(p3), decS[k][:], ALU.mult, R=[p3, decS[k]], W=[MT])
            P.tt(r32(MT[:]), MT[:], bc_last(s[:, 6, :], 64), ALU.mult, R=[MT, s], W=[MT])
            yield
            p4 = nps()
            for h in range(8):
                P.tr(p4[:, h * 64:(h + 1) * 64], MT[:, h, :], I64, R=[MT, mk], W=[p4])
            P.copy(r32(M[:]), v3(p4), R=[p4], W=[M], en="act")
            yield
            Pc, Pn = Pa[k], Pb[k]
            P.tt(r32(Pc[:]), v3(p4), bc_mid(I64, 8), ALU.add, R=[p4, mk], W=[Pc])
            for lev in range(5):
                yield
                pa = nps()
                mmh(pa, M, MT, [M, MT])
                P.copy(r32(MT2[:]), v3(pa), R=[pa], W=[MT2], en="act")
                if lev < 4:
                    pb = nps()
                    mmh(pb, MT, M, [M, MT])
                    P.copy(r32(M2[:]), v3(pb), R=[pb], W=[M2], en="dve")
                yield
                pcx = nps()
                mmh(pcx, MT2, Pc, [MT2, Pc])
                P.tt(r32(Pn[:]), v3(pcx), Pc[:], ALU.add, R=[pcx, Pc], W=[Pn])
                Pc, Pn = Pn, Pc
                M, M2 = M2, M
                MT, MT2 = MT2, MT
            yield
            P.tt(r32(vb[k][:]), vtm[k][:], bc_last(beta, 64), ALU.mult, R=[vtm[k], gt[k]], W=[vb[k]])
            P.tt(r32(rw[k][:]), ktm[k][:], bc_last(s[:, 5, :], 64), ALU.mult, R=[ktm[k], s], W=[rw[k]])
            P.tt(r32(kdec[k][:]), ktm[k][:], bc_last(s[:, 4, :], 64), ALU.mult, R=[ktm[k], s], W=[kdec[k]])
            pu = nps()
            mmh(pu, Pc, vb[k], [Pc, vb[k]])
            P.copy(u_[k][:], v3(pu), R=[pu], W=[u_[k]], en="act")
            yield
            pw = nps()
            mmh(pw, rw[k], Pc, [Pc, rw[k]])
            P.copy(r32(wT[k][:]), v3(pw), R=[pw], W=[wT[k]], en="act")
            if want_out:
                pa2 = nps()
                mmh(pa2, kT[k], qT[k], [kT[k], qT[k]], fast=False)
                P.tt(r32(aT[k][:]), v3(pa2), decCT[k][:], ALU.mult, R=[pa2, decCT[k]], W=[aT[k]])
            yield
            pws = nps()
            mmh(pws, wT[k], S, [wT[k], S])
            P.tt(r32(vnew[k][:]), u_[k][:], v3(pws), ALU.subtract, R=[u_[k], pws], W=[vnew[k]])
            if want_out:
                pq = nps()
                mmh(pq, qT[k], S, [qT[k], S], fast=False)
                pv = nps()
                mmh(pv, aT[k], vnew[k], [aT[k], vnew[k]])
                P.tt(tmp[k][:], v3(pq), bc_last(s[:, 2, :], 64), ALU.mult, R=[pq, s], W=[tmp[k]])
                P.tt(ot[k][:], tmp[k][:], v3(pv), ALU.add, R=[tmp[k], pv], W=[ot[k]])
                P.dma(C.dno[b, d, tok0:tok0 + 64, :], ot[k][:].rearrange("p h f -> p (h f)"), R=[ot[k]], W=[], q="act")
            yield
            pk = nps()
            mmh(pk, kdec[k], vnew[k], [kdec[k], vnew[k]])
            P.tt(r32(S[:]), S[:], bc_last(s[:, 3, :], 64), ALU.mult, R=[S, s], W=[S])
            P.tt(r32(S[:]), S[:], v3(pk), ALU.add, R=[S, pk], W=[S])
            yield


    gens = [stream(0), stream(1)]
    alive = [True, True]
    while any(alive):
        for i, g in enumerate(gens):
            if alive[i]:
                try:
                    next(g)
                except StopIteration:
                    alive[i] = False


def stage_dn_out(P, C, l, b, with_ctx_out):
    with P.scope():
        _stage_dn_out(P, C, l, b, with_ctx_out)


def _stage_dn_out(P, C, l, b, with_ctx_out):
    o0 = [P.sb("o0_%d" % i, [128, 8, 64]) for i in range(2)]
    o1 = [P.sb("o1_%d" % i, [128, 8, 64]) for i in range(2)]
    zt = [P.sb("zt_%d" % i, [128, 8, 64]) for i in range(2)]
    sq = [P.sb("osq_%d" % i, [128, 8, 64]) for i in range(2)]
    ms = [P.sb("oms_%d" % i, [128, 8]) for i in range(2)]
    ng = P.sb("ong", [128, 64])
    ps = [P.ps("ops%d" % i, [128, 512]) for i in range(2)]
    oT = [P.sb("ooT%d" % i, [128, 4, 128]) for i in range(2)]
    P.dma(ng[:], C.dn_norm_g[l, :].partition_broadcast(128), R=[C.dn_norm_g], W=[ng])
    for t in range(18):
        if t < 2 and not with_ctx_out:
            continue
        k = t % 2
        rows = slice(t * 128, (t + 1) * 128)
        P.dma(o0[k][:].rearrange("p h f -> p (h f)"), C.dno[b, 0, rows, :], R=[C.dno], W=[o0[k]])
        P.dma(o1[k][:].rearrange("p h f -> p (h f)"), C.dno[b, 1, rows, :], R=[C.dno], W=[o1[k]])
        P.dma(zt[k][:].rearrange("p h f -> p (h f)"), C.projT[b, rows, 128:640], R=[C.projT], W=[zt[k]])
        P.tt(o0[k][:], o0[k][:], o1[k][:], ALU.add, R=[o0[k], o1[k]], W=[o0[k]])
        P.act(sq[k][:], o0[k][:], AF.Square, R=[o0[k]], W=[sq[k]])
        P.op("dve", lambda e: e.tensor_reduce(out=ms[k][:], in_=sq[k][:], axis=AX.X, op=ALU.add), R=[sq[k]], W=[ms[k]])
        P.ts(ms[k][:], ms[k][:], 1.0 / 64, EPS, ALU.mult, ALU.add, R=[ms[k]], W=[ms[k]])
        P.act(ms[k][:], ms[k][:], AF.Sqrt, R=[ms[k]], W=[ms[k]])
        P.op("dve", lambda e: e.reciprocal(out=ms[k][:], in_=ms[k][:]), R=[ms[k]], W=[ms[k]])
        P.tt(o0[k][:], o0[k][:], bc_last(ms[k][:], 64), ALU.mult, R=[o0[k], ms[k]], W=[o0[k]])
        P.tt(o0[k][:], o0[k][:], bc_mid(ng[:], 8), ALU.mult, R=[o0[k], ng], W=[o0[k]])
        P.act(zt[k][:], zt[k][:], AF.Silu, R=[zt[k]], W=[zt[k]])
        P.tt(o0[k][:], o0[k][:], zt[k][:], ALU.mult, R=[o0[k], zt[k]], W=[o0[k]])
        of = o0[k][:].rearrange("p h f -> p (h f)")
        for c in range(4):
            P.tr(ps[k][:, c * 128:(c + 1) * 128], of[:, c * 128:(c + 1) * 128], C.ident_sb[:], R=[o0[k], C.ident_sb], W=[ps[k]])
        P.copy(oT[k][:], ps[k][:].rearrange("p (c t) -> p c t", c=4), R=[ps[k]], W=[oT[k]], en="act")
        P.dma(C.br[b, 2, :, rows].rearrange("(c p) t -> p c t", p=128), oT[k][:], R=[oT[k]], W=[], q="act")


def host_shared(I):
    f = lambda a: np.ascontiguousarray(np.asarray(a, dtype=np.float32))
    w_fm, w_tm = host_w_in_layout(np.asarray(I["w_in"]))
    masks, bd = host_masks()
    L = DEPTH
    sh = dict(
        w_mod=f(I["w_mod"]), b_mod=f(I["b_mod"]), norm1_g=f(I["norm1_g"]), norm2_g=f(I["norm2_g"]),
        w_fm=f(w_fm), w_tm=f(w_tm), ident=np.eye(128, dtype=np.float32),
        cw=f(np.asarray(I["dn_conv_w"]).reshape(L, 5, 12, 128).transpose(0, 3, 2, 1)),
        dn_a_log=f(np.asarray(I["dn_a_log"]).reshape(L, 16)), dn_dt_bias=f(np.asarray(I["dn_dt_bias"]).reshape(L, 16)),
        dn_norm_g=f(I["dn_norm_g"]), masks=masks, bd=bd,
    )
    sh.update(host_attn(I))
    return sh


def host_core(I, core):
    b0 = core * NB
    cs = np.stack([np.asarray(I["c"])[b0], np.asarray(I["c"])[b0 + 1], np.asarray(I["c_ctx"])], 0)
    cT = np.ascontiguousarray(cs.T.reshape(8, 128, 3).transpose(1, 0, 2)).astype(np.float32)
    return dict(x=np.ascontiguousarray(np.asarray(I["x"])[b0:b0 + NB]), ctx=np.ascontiguousarray(np.asarray(I["ctx"])[b0:b0 + NB]), cT=cT)


def rope_tables_np(n_tok, rot_dim):
    rows = n_tok // 64
    row = np.broadcast_to(np.arange(rows)[:, None], (rows, 64)).reshape(-1).astype(np.float32)
    col = np.broadcast_to(np.arange(64)[None, :], (rows, 64)).reshape(-1).astype(np.float32)
    n_freq = rot_dim // 4
    inv_freq = (np.float32(10000.0) ** (-np.arange(n_freq, dtype=np.float32) / np.float32(n_freq))).astype(np.float32)
    ang = np.concatenate([row[:, None] * inv_freq, col[:, None] * inv_freq], axis=-1).astype(np.float32)
    cos, sin = np.cos(ang).astype(np.float32), np.sin(ang).astype(np.float32)
    CC = np.concatenate([cos.T, cos.T], 0)
    SS = np.concatenate([-sin.T, sin.T], 0)
    return np.ascontiguousarray(np.stack([CC, SS], 0))


def declare_attn(P, C):
    nc = P.nc
    def inp(name, shape):
        return T(nc.dram_tensor(name, list(shape), F32, kind="ExternalInput"))
    C.qg = inp("qg", [DEPTH, 128, 3])
    C.kvg = inp("kvg", [DEPTH, 128, 2])
    C.w_qn = inp("w_qn", [DEPTH, 384, 512])
    C.w_qrA = inp("w_qrA", [DEPTH, 384, 256])
    C.w_qrB = inp("w_qrB", [DEPTH, 384, 256])
    C.w_kn = inp("w_kn", [DEPTH, 256, 512])
    C.w_v = inp("w_v", [DEPTH, 256, 512])
    C.ropeM = inp("ropeM", [2, 32, TL])
    C.ropeS = inp("ropeS", [2, 64, TL])
    C.swam = inp("swam", [128, 2, 128])
    C.swa_sink = inp("swa_sink", [DEPTH, 8])


def host_attn(I):
    f = lambda a: np.ascontiguousarray(np.asarray(a, dtype=np.float32))
    L = DEPTH
    wq = np.asarray(I["mla_w_q_up"]).reshape(L, 384, 8, 96)
    wkv = np.asarray(I["mla_w_kv_up"]).reshape(L, 256, 8, 128)
    kk = np.arange(128)[:, None]
    qq = np.arange(128)[None, :]
    swam = np.stack([kk >= qq, kk <= qq], 1).astype(np.float32)
    return dict(
        qg=f(np.asarray(I["mla_q_norm_g"]).reshape(L, 3, 128).transpose(0, 2, 1)),
        kvg=f(np.asarray(I["mla_kv_norm_g"]).reshape(L, 2, 128).transpose(0, 2, 1)),
        w_qn=f(wq[..., :64].reshape(L, 384, 512)),
        w_qrA=f(wq[..., 64:96].reshape(L, 384, 256)),
        w_qrB=f(np.concatenate([wq[..., 80:96], wq[..., 64:80]], -1).reshape(L, 384, 256)),
        w_kn=f(wkv[..., :64].reshape(L, 256, 512)),
        w_v=f(wkv[..., 64:].reshape(L, 256, 512)),
        ropeM=rope_tables_np(TL, 32), ropeS=rope_tables_np(TL, 64), swam=f(swam), swa_sink=f(I["swa_sink"]),
    )


def col_blocks():
    return [(i * 512, min(512, TA - i * 512)) for i in range((TA + 511) // 512)]


def stage_mla(P, C, l, b, ctx_out):
    with P.scope():
        _stage_mla(P, C, l, b, ctx_out)


def _stage_mla(P, C, l, b, ctx_out):
    SCALE = float(96 ** -0.5)
    cqn = P.sb("cqn", [128, 3, TA]); ckvn = P.sb("ckvn", [128, 2, TA])
    sqt = P.sb("sqt", [128, 3, 512]); rs = P.sb("mrs", [128, 512])
    qg = P.sb("qg", [128, 3]); kvg = P.sb("kvg", [128, 2])
    wqn = P.sb("wqn", [128, 3, 512]); wqa = P.sb("wqa", [128, 3, 256]); wqb = P.sb("wqb", [128, 3, 256])
    wkn = P.sb("wkn", [128, 2, 512]); wv = P.sb("wv", [128, 2, 512])
    rope = P.sb("ropem", [32, 2, TL])
    krr = P.sb("krr", [32, TA]); krb = P.sb("krb", [32, TA])
    qn = P.sb("qn", [64, TA], BF16); qr = P.sb("qr", [32, TA]); qrb = P.sb("qrb", [32, TA]); kn = P.sb("kn", [64, TA], BF16)
    qr16 = P.sb("qr16", [32, TA], BF16); krr16 = P.sb("krr16", [32, TA], BF16); ones16 = P.sb("ones16", [128, 64], BF16)
    vh = P.sb("vh", [128, 18, 64], BF16); oT = P.sb("oT", [64, TA])
    pt = [P.sb("pt%d" % i, [128, 512], BF16) for i in range(3)]
    P.op("dve", lambda e: e.memset(ones16[:], 1.0), W=[ones16])
    rd = [P.sb("rdm%d" % i, [64, 512]) for i in range(2)]
    pA = [P.ps("pA%d" % i, [128, 512]) for i in range(2)]
    pO = [P.ps("pO%d" % i, [64, 512]) for i in range(2)]
    pD = [P.ps("pD%d" % i, [64, 512]) for i in range(2)]
    pX = [P.ps("pX%d" % i, [128, 512]) for i in range(2)]
    ix = [0]

    def npx():
        ix[0] += 1
        return pX[ix[0] % 2]

    P.dma(qg[:], C.qg[l], R=[C.qg], W=[qg]); P.dma(kvg[:], C.kvg[l], R=[C.kvg], W=[kvg])
    for (wt, src) in ((wqn, C.w_qn), (wqa, C.w_qrA), (wqb, C.w_qrB), (wkn, C.w_kn), (wv, C.w_v)):
        P.dma(wt[:], src.t[l].rearrange("(c p) n -> p c n", p=128), R=[src], W=[wt])
    P.dma(rope[:], C.ropeM[:, :, :].rearrange("a p t -> p a t"), R=[C.ropeM], W=[rope])
    CC, SS = rope[:, 0, :], rope[:, 1, :]
    for c in range(3):
        P.dma(cqn[:, c, :], C.proj[b, c * 128:(c + 1) * 128, :], R=[], W=[cqn])
    for c in range(2):
        P.dma(ckvn[:, c, :], C.proj[b, 384 + c * 128:384 + (c + 1) * 128, :], R=[], W=[ckvn])
    P.dma(krr[:], C.proj[b, FM_OFF["krA"]:FM_OFF["krA"] + 32, :], R=[], W=[krr])
    P.dma(krb[:], C.proj[b, FM_OFF["krB"]:FM_OFF["krB"] + 32, :], R=[], W=[krb])
    for (xt, nchunk, g, dim) in ((cqn, 3, qg, 384), (ckvn, 2, kvg, 256)):
        for (c0, n) in col_blocks():
            P.act(sqt[:, 0:nchunk, 0:n], xt[:, :, c0:c0 + n], AF.Square, R=[xt], W=[sqt])
            ps = npx()
            for c in range(nchunk):
                P.mm(ps[:, 0:n], C.ones_sb[:], sqt[:, c, 0:n], c == 0, c == nchunk - 1, R=[C.ones_sb, sqt], W=[ps])
            P.ts(rs[:, 0:n], ps[:, 0:n], 1.0 / dim, EPS, ALU.mult, ALU.add, R=[ps], W=[rs])
            P.act(rs[:, 0:n], rs[:, 0:n], AF.Sqrt, R=[rs], W=[rs])
            P.op("dve", lambda e: e.reciprocal(out=rs[:, 0:n], in_=rs[:, 0:n]), R=[rs], W=[rs])
            for c in range(nchunk):
                P.stt(xt[:, c, c0:c0 + n], xt[:, c, c0:c0 + n], g[:, c:c + 1], rs[:, 0:n], ALU.mult, ALU.mult, R=[xt, g, rs], W=[xt])
    P.tt(krr[:, TC:TA], krr[:, TC:TA], CC, ALU.mult, R=[krr, rope], W=[krr])
    P.tt(krb[:, TC:TA], krb[:, TC:TA], SS, ALU.mult, R=[krb, rope], W=[krb])
    P.tt(krr16[:, TC:TA], krr[:, TC:TA], krb[:, TC:TA], ALU.add, R=[krr, krb], W=[krr16])
    P.copy(krr16[:, 0:TC], krr[:, 0:TC], R=[krr], W=[krr16])
    ia = 0
    for h in range(8):
        for (c0, n) in col_blocks():
            ps = npx()
            for c in range(3):
                P.mm(ps[0:64, 0:n], wqn[:, c, h * 64:(h + 1) * 64], cqn[:, c, c0:c0 + n], c == 0, c == 2, R=[wqn, cqn], W=[ps])
            P.act(qn[:, c0:c0 + n], ps[0:64, 0:n], AF.Copy, R=[ps], W=[qn], scale=SCALE)
            ps = npx()
            for c in range(3):
                P.mm(ps[0:32, 0:n], wqa[:, c, h * 32:(h + 1) * 32], cqn[:, c, c0:c0 + n], c == 0, c == 2, R=[wqa, cqn], W=[ps])
            P.act(qr[:, c0:c0 + n], ps[0:32, 0:n], AF.Copy, R=[ps], W=[qr], scale=SCALE)
            ps = npx()
            for c in range(3):
                P.mm(ps[0:32, 0:n], wqb[:, c, h * 32:(h + 1) * 32], cqn[:, c, c0:c0 + n], c == 0, c == 2, R=[wqb, cqn], W=[ps])
            P.act(qrb[:, c0:c0 + n], ps[0:32, 0:n], AF.Copy, R=[ps], W=[qrb], scale=SCALE)
            ps = npx()
            for c in range(2):
                P.mm(ps[0:64, 0:n], wkn[:, c, h * 64:(h + 1) * 64], ckvn[:, c, c0:c0 + n], c == 0, c == 1, R=[wkn, ckvn], W=[ps])
            P.copy(kn[:, c0:c0 + n], ps[0:64, 0:n], R=[ps], W=[kn])
        P.tt(qr[:, TC:TA], qr[:, TC:TA], CC, ALU.mult, R=[qr, rope], W=[qr])
        P.tt(qrb[:, TC:TA], qrb[:, TC:TA], SS, ALU.mult, R=[qrb, rope], W=[qrb])
        P.tt(qr16[:, TC:TA], qr[:, TC:TA], qrb[:, TC:TA], ALU.add, R=[qr, qrb], W=[qr16])
        if ctx_out:
            P.copy(qr16[:, 0:TC], qr[:, 0:TC], R=[qr], W=[qr16])
        for t0 in range(0, 18, 8):
            nt = min(8, 18 - t0)
            ps = npx()
            for j in range(nt):
                for c in range(2):
                    P.mm(ps[:, j * 64:(j + 1) * 64], ckvn[:, c, (t0 + j) * 128:(t0 + j + 1) * 128], wv[:, c, h * 64:(h + 1) * 64], c == 0, c == 1, R=[wv, ckvn], W=[ps])
            P.copy(vh[:, t0:t0 + nt, :], ps[:, 0:nt * 64].rearrange("p (t f) -> p t f", f=64), R=[ps], W=[vh])
        qblocks = [(TC + i * 512, 512, 18) for i in range(4)]
        if ctx_out:
            qblocks.append((0, TC, 2))
        for (q0, n, nkt) in qblocks:
            po = pO[ia % 2]; pd = pD[ia % 2]; r = rd[ia % 2]
            ia += 1
            def sc_(kt):
                pa = pA[kt % 2]
                P.mm(pa[:, 0:n], kn[:, kt * 128:(kt + 1) * 128], qn[:, q0:q0 + n], True, False, R=[kn, qn], W=[pa])
                P.mm(pa[:, 0:n], krr16[:, kt * 128:(kt + 1) * 128], qr16[:, q0:q0 + n], False, True, R=[krr16, qr16], W=[pa])
            sc_(0)
            for kt in range(nkt):
                if kt + 1 < nkt:
                    sc_(kt + 1)
                pa = pA[kt % 2]
                p = pt[kt % 3]
                P.act(p[:, 0:n], pa[:, 0:n], AF.Exp, R=[pa], W=[p])
                P.mm(po[:, 0:n], vh[:, kt, :], p[:, 0:n], kt == 0, kt == nkt - 1, R=[vh, p], W=[po])
                P.mm(pd[:, 0:n], ones16[:, :], p[:, 0:n], kt == 0, kt == nkt - 1, R=[ones16, p], W=[pd])
            P.op("dve", lambda e: e.reciprocal(out=r[:, 0:n], in_=pd[:, 0:n]), R=[pd], W=[r])
            P.tt(oT[:, q0:q0 + n], po[:, 0:n], r[:, 0:n], ALU.mult, R=[po, r], W=[oT])
        c_lo = 0 if ctx_out else TC
        P.dma(C.br[b, 0, h * 64:(h + 1) * 64, c_lo:TA], oT[:, c_lo:TA], R=[oT], W=[], q="act")


def stage_swa(P, C, l, b, ctx_out):
    with P.scope():
        _stage_swa(P, C, l, b, ctx_out)


def _stage_swa(P, C, l, b, ctx_out):
    qA = P.sb("sqA", [64, 4, TA]); qB = P.sb("sqB", [64, 4, TA])
    kA = P.sb("skA", [64, TA]); kB = P.sb("skB", [64, TA])
    q16 = P.sb("sq16", [64, 4, TA], BF16); k16 = P.sb("sk16", [64, TA], BF16)
    vn32 = P.sb("svn32", [128, 18, 64]); vn = P.sb("svn", [128, 18, 64], BF16)
    ones16 = P.sb("sones16", [128, 64], BF16)
    P.op("dve", lambda e: e.memset(ones16[:], 1.0), W=[ones16])
    rope = P.sb("ropes", [64, 2, TL])
    msk = P.sb("swam", [128, 2, 128])
    snk = P.sb("snk", [64, 8])
    pt = [P.sb("spt%d" % i, [128, 4, 128], BF16) for i in range(3)]
    rd = [P.sb("srd%d" % i, [64, 4, 128]) for i in range(2)]
    ot = [P.sb("sot%d" % i, [64, 4, 128]) for i in range(2)]
    pA = [P.ps("spA%d" % i, [128, 512]) for i in range(2)]
    pO = [P.ps("spO%d" % i, [64, 512]) for i in range(2)]
    pD = [P.ps("spD%d" % i, [64, 512]) for i in range(2)]
    P.dma(rope[:], C.ropeS[:, :, :].rearrange("a p t -> p a t"), R=[C.ropeS], W=[rope])
    P.dma(msk[:], C.swam[:, :, :], R=[C.swam], W=[msk])
    P.dma(snk[:], C.swa_sink[l, :].partition_broadcast(64), R=[C.swa_sink], W=[snk])
    P.act(snk[:], snk[:], AF.Exp, R=[snk], W=[snk])
    CC, SS = rope[:, 0, :], rope[:, 1, :]
    ib = 0
    ip = 0
    for n in range(2):
        for hh in range(4):
            r0 = FM_OFF["sqA"] + (4 * n + hh) * 64
            P.dma(qA[:, hh, :], C.proj[b, r0:r0 + 64, :], R=[], W=[qA])
            r0 = FM_OFF["sqB"] + (4 * n + hh) * 64
            P.dma(qB[:, hh, :], C.proj[b, r0:r0 + 64, :], R=[], W=[qB])
        P.dma(kA[:], C.proj[b, FM_OFF["skA"] + n * 64:FM_OFF["skA"] + (n + 1) * 64, :], R=[], W=[kA])
        P.dma(kB[:], C.proj[b, FM_OFF["skB"] + n * 64:FM_OFF["skB"] + (n + 1) * 64, :], R=[], W=[kB])
        P.dma(vn32[:], C.projT[b, :, n * 64:(n + 1) * 64].rearrange("(t p) f -> p t f", p=128), R=[], W=[vn32])
        P.copy(vn[:], vn32[:], R=[vn32], W=[vn])
        P.tt(qA[:, :, TC:TA], qA[:, :, TC:TA], bc_mid(CC, 4), ALU.mult, R=[qA, rope], W=[qA])
        P.tt(qB[:, :, TC:TA], qB[:, :, TC:TA], bc_mid(SS, 4), ALU.mult, R=[qB, rope], W=[qB])
        P.tt(qA[:, :, TC:TA], qA[:, :, TC:TA], qB[:, :, TC:TA], ALU.add, R=[qA, qB], W=[qA])
        P.act(q16[:], qA[:], AF.Copy, R=[qA], W=[q16], scale=0.125)
        P.tt(kA[:, TC:TA], kA[:, TC:TA], CC, ALU.mult, R=[kA, rope], W=[kA])
        P.tt(kB[:, TC:TA], kB[:, TC:TA], SS, ALU.mult, R=[kB, rope], W=[kB])
        P.tt(k16[:, TC:TA], kA[:, TC:TA], kB[:, TC:TA], ALU.add, R=[kA, kB], W=[k16])
        P.copy(k16[:, 0:TC], kA[:, 0:TC], R=[kA], W=[k16])
        qblocks = []
        if ctx_out:
            qblocks += [(0, -1), (128, -1)]
        qblocks += [(TC + i * 128, i) for i in range(16)]
        for (q0, i) in qblocks:
            tiles = [(0, None), (1, None)]
            if i >= 0:
                if i - 1 >= 0:
                    tiles.append((2 + i - 1, 0))
                tiles.append((2 + i, None))
                if i + 1 <= 15:
                    tiles.append((2 + i + 1, 1))
            po = pO[ib % 2]; pd = pD[ib % 2]; r = rd[ib % 2]; o = ot[ib % 2]
            ib += 1
            def sc_(j):
                pa = pA[(ip + j) % 2]
                P.mm(pa[:].rearrange("p (h q) -> p h q", h=4), k16[:, tiles[j][0] * 128:(tiles[j][0] + 1) * 128], q16[:, :, q0:q0 + 128], True, True, R=[k16, q16], W=[pa])
            sc_(0)
            for ti, (kt, mi) in enumerate(tiles):
                if ti + 1 < len(tiles):
                    sc_(ti + 1)
                pa = pA[(ip + ti) % 2]; p = pt[(ip + ti) % 3]
                P.act(p[:], pa[:].rearrange("p (h q) -> p h q", h=4), AF.Exp, R=[pa], W=[p])
                if mi is not None:
                    P.tt(p[:], p[:], bc_mid(msk[:, mi, :], 4), ALU.mult, R=[p, msk], W=[p])
                pf = p[:].rearrange("p h q -> p (h q)")
                P.mm(po[:, :], vn[:, kt, :], pf, ti == 0, ti == len(tiles) - 1, R=[vn, p], W=[po])
                P.mm(pd[:, :], ones16[:, :], pf, ti == 0, ti == len(tiles) - 1, R=[ones16, p], W=[pd])
            ip += len(tiles)
            P.tt(r[:], pd[:].rearrange("p (h q) -> p h q", h=4), bc_last(snk[:, 4 * n:4 * n + 4], 128), ALU.add, R=[pd, snk], W=[r])
            P.op("dve", lambda e: e.reciprocal(out=r[:], in_=r[:]), R=[r], W=[r])
            P.tt(o[:], po[:].rearrange("p (h q) -> p h q", h=4), r[:], ALU.mult, R=[po, r], W=[o])
            P.dma(C.br[b, 1, n * 256:(n + 1) * 256, q0:q0 + 128].rearrange("(h f) t -> f h t", h=4), o[:], R=[o], W=[], q="act")


def declare_rest(P, C):
    nc = P.nc
    def inp(name, shape):
        return T(nc.dram_tensor(name, list(shape), F32, kind="ExternalInput"))
    C.w_branch = inp("w_branch", [DEPTH, 3, 512, D])
    C.w_out = inp("w_out", [DEPTH, D, D])
    C.router_w = inp("router_w", [DEPTH, D, 32])
    C.router_b = inp("router_b", [DEPTH, 32])
    C.exp_w_gu = inp("exp_w_gu", [DEPTH, 32, D, 2048])
    C.bgu = inp("bgu", [DEPTH, 128, 32, 2, 8])
    C.exp_w_dn = inp("exp_w_dn", [DEPTH, 32, D, D])
    C.exp_b_dn = inp("exp_b_dn", [DEPTH, 32, D])
    C.final_norm_g = inp("final_norm_g", [D])
    C.xres = P.dram("xres", [NB, TA, D])
    C.out = T(nc.dram_tensor("out", [NB, TL, D], F32, kind="ExternalOutput"))


def host_rest(I):
    f = lambda a: np.ascontiguousarray(np.asarray(a, dtype=np.float32))
    L = DEPTH
    bgu = np.asarray(I["exp_b_gu"]).reshape(L, 32, 8, 128, 2).transpose(0, 3, 1, 4, 2)
    return dict(w_branch=f(I["w_branch"]), w_out=f(I["w_out"]), router_w=f(I["router_w"]), router_b=f(I["router_b"]),
                exp_w_gu=f(I["exp_w_gu"]), bgu=f(bgu), exp_w_dn=f(I["exp_w_dn"]), exp_b_dn=f(I["exp_b_dn"]),
                final_norm_g=f(I["final_norm_g"]))


def stage_merge(P, C, l, b, ctx_out):
    with P.scope():
        _stage_merge(P, C, l, b, ctx_out)


def _stage_merge(P, C, l, b, ctx_out):
    wbr = P.sb("wbr", [128, 3, 4, D]); wout = P.sb("wout", [128, 8, D])
    brt = P.sb("brt", [128, 3, 4, 512])
    gch = [P.sb("gch%d" % i, [128, 512]) for i in range(3)]
    tmp = [P.sb("mtmp%d" % i, [128, 512]) for i in range(2)]
    yT = P.sb("yT", [128, 8, 512])
    g1 = [P.sb("g1bc%d" % i, [128, D]) for i in range(2)]
    xt = [P.sb("mxt%d" % i, [128, D]) for i in range(2)]
    pA = [P.ps("mpA%d" % i, [128, 512]) for i in range(3)]
    pB = [P.ps("mpB%d" % i, [128, 512]) for i in range(2)]
    P.dma(wbr[:].rearrange("p i c n -> p (i c) n"), C.w_branch.t[l].rearrange("i (c p) n -> p (i c) n", p=128), R=[C.w_branch], W=[wbr])
    P.dma(wout[:], C.w_out.t[l].rearrange("(c p) n -> p c n", p=128), R=[C.w_out], W=[wout])
    load_mod_bc(P, C, l, b, (2,), (g1[0],))
    load_mod_bc(P, C, l, 2, (2,), (g1[1],))
    ig = 0
    ix = 0
    for (t0, n) in blocks():
        if t0 < TC and not ctx_out:
            continue
        gbc = g1[1] if t0 < TC else g1[0]
        for i in range(3):
            P.dma(brt[:, i, :, 0:n], C.br[b, i, :, t0:t0 + n].rearrange("(c p) t -> p c t", p=128), R=[], W=[brt])
        for m in range(8):
            for i in range(3):
                gc = gch[ig % 3]; pa = pA[ig % 3]; tm = tmp[ig % 2]
                ig += 1
                r0 = FM_OFF["gate"] + i * D + m * 128
                P.dma(gc[:, 0:n], C.proj[b, r0:r0 + 128, t0:t0 + n], R=[], W=[gc])
                for c in range(4):
                    P.mm(pa[:, 0:n], wbr[:, i, c, m * 128:(m + 1) * 128], brt[:, i, c, 0:n], c == 0, c == 3, R=[wbr, brt], W=[pa])
                P.act(gc[:, 0:n], gc[:, 0:n], AF.Sigmoid, R=[gc], W=[gc])
                if i == 0:
                    P.tt(yT[:, m, 0:n], pa[:, 0:n], gc[:, 0:n], ALU.mult, R=[pa, gc], W=[yT])
                else:
                    P.tt(tm[:, 0:n], pa[:, 0:n], gc[:, 0:n], ALU.mult, R=[pa, gc], W=[tm])
                    P.tt(yT[:, m, 0:n], yT[:, m, 0:n], tm[:, 0:n], ALU.add, R=[yT, tm], W=[yT])
        for i in range(n // 128):
            x = xt[ix % 2]
            src_t, src_ap = token_src(C, l, b, t0 + i * 128, 128)
            P.dma(x[:], src_ap, R=[], W=[x])
            for half in range(2):
                pb = pB[(2 * ix + half) % 2]
                for m in range(8):
                    P.mm(pb[:, :], yT[:, m, i * 128:(i + 1) * 128], wout[:, m, half * 512:(half + 1) * 512], m == 0, m == 7, R=[yT, wout], W=[pb])
                tm = tmp[half]
                P.tt(tm[:], pb[:, :], gbc[:, half * 512:(half + 1) * 512], ALU.mult, R=[pb, gbc], W=[tm])
                P.tt(x[:, half * 512:(half + 1) * 512], x[:, half * 512:(half + 1) * 512], tm[:], ALU.add, R=[x, tm], W=[x])
            P.dma(C.xres[b, t0 + i * 128:t0 + (i + 1) * 128, :], x[:], R=[x], W=[], q="act")
            ix += 1


def stage_moe(P, C, l, b, ctx_out, n_exp=32):
    with P.scope():
        _stage_moe(P, C, l, b, ctx_out, n_exp)


def _stage_moe(P, C, l, b, ctx_out, n_exp):
    C.xt = [P.sb("xt%d" % i, [128, D]) for i in range(2)]
    C.ht = [P.sb("ht%d" % i, [128, D]) for i in range(2)]
    C.st = [P.sb("st%d" % i, [128, 4]) for i in range(2)]
    C.ps_tr = [P.ps("ps_tr%d" % i, [128, 1024]) for i in range(1)] * 2
    hT = P.sb("mhT", [128, 8, 512])
    hT16 = P.sb("mhT16", [128, 8, 512], BF16)
    modbc = [P.sb("modbc%d" % i, [128, D]) for i in range(3)]
    rw = P.sb("rw", [128, 8, 32]); rb = P.sb("rb", [128, 32])
    bgu = P.sb("bgu", [128, 32, 2, 8]); bdn = P.sb("bdn", [32, D])
    lg = P.sb("lg", [128, 32]); ex = P.sb("ex", [128, 32]); mk = P.sb("rmk", [128, 32]); t8 = P.sb("t8", [128, 8]); sc = P.sb("rsc", [128, 4])
    G = P.sb("G", [128, 4, 32]); GT = P.sb("GT", [32, 512])
    stg = [P.sb("stg%d" % i, [128, 8, 512]) for i in range(3)]
    wq = [P.sb("wq%d" % i, [128, 8, 512], BF16) for i in range(3)]
    wd = [P.sb("wd%d" % i, [128, 8, 512], BF16) for i in range(2)]
    actT = P.sb("actT", [128, 8, 512], BF16)
    acc = P.sb("acc", [128, 4, D])
    gs = [P.sb("gs%d" % i, [128, 512]) for i in range(2)]
    us = [P.sb("us%d" % i, [128, 512]) for i in range(2)]
    sg = [P.sb("sg%d" % i, [128, 512]) for i in range(2)]
    pR = C.ps_tr[0]
    pG = [P.ps("pG%d" % i, [128, 512]) for i in range(2)]
    pU = [P.ps("pU%d" % i, [128, 512]) for i in range(2)]
    pY = [P.ps("pY%d" % i, [128, 512]) for i in range(2)]
    P.dma(rw[:], C.router_w.t[l].rearrange("(c p) n -> p c n", p=128), R=[C.router_w], W=[rw])
    P.dma(rb[:], C.router_b[l, :].partition_broadcast(128), R=[C.router_b], W=[rb])
    P.dma(bgu[:], C.bgu[l], R=[C.bgu], W=[bgu])
    P.dma(bdn[:], C.exp_b_dn[l], R=[C.exp_b_dn], W=[bdn])
    it = 0
    ist = 0
    iq = 0
    idn = 0
    ie = 0
    last_r = None
    for (t0, n) in blocks():
        if t0 < TC and not ctx_out:
            continue
        r = 1 if t0 < TC else 0
        if r != last_r:
            load_mod_bc(P, C, l, 2 if r else b, (4, 3, 5), modbc)
            last_r = r
        A2, sh2, g2 = modbc
        nt = n // 128
        for i in range(nt):
            norm_mod_T(P, C, C.xres, C.xres[b, t0 + i * 128:t0 + (i + 1) * 128, :], A2, sh2, hT, i * 128, it)
            it += 1
        P.copy(hT16[:, :, 0:n], hT[:, :, 0:n], R=[hT], W=[hT16], en="pool")
        for i in range(nt):
            for c in range(8):
                P.mm(pR[:, 0:32], hT[:, c, i * 128:(i + 1) * 128], rw[:, c, :], c == 0, c == 7, R=[hT, rw], W=[pR])
            P.tt(lg[:], pR[:, 0:32], rb[:], ALU.add, R=[pR, rb], W=[lg])
            P.op("dve", lambda e: e.max(out=t8[:], in_=lg[:]), R=[lg], W=[t8])
            P.ts(mk[:], lg[:], t8[:, 3:4], None, ALU.is_ge, ALU.bypass, R=[lg, t8], W=[mk])
            P.ts(sc[:, 0:1], t8[:, 0:1], -1.0, None, ALU.mult, ALU.bypass, R=[t8], W=[sc])
            P.act(ex[:], lg[:], AF.Exp, R=[lg, sc], W=[ex], bias=sc[:, 0:1], scale=1.0)
            P.tt(ex[:], ex[:], mk[:], ALU.mult, R=[ex, mk], W=[ex])
            P.op("dve", lambda e: e.tensor_reduce(out=sc[:, 1:2], in_=ex[:], axis=AX.X, op=ALU.add), R=[ex], W=[sc])
            P.op("dve", lambda e: e.reciprocal(out=sc[:, 2:3], in_=sc[:, 1:2]), R=[sc], W=[sc])
            P.ts(G[:, i, :], ex[:], sc[:, 2:3], None, ALU.mult, ALU.bypass, R=[ex, sc], W=[G])
            P.tr(pR[0:32, 128:256], G[:, i, :], C.ident_sb[:], R=[G, C.ident_sb], W=[pR])
            P.copy(GT[:, i * 128:(i + 1) * 128], pR[0:32, 128:256], R=[pR], W=[GT])
        for i in range(nt):
            for half in range(2):
                P.mm(pR[:, 0:512], GT[:, i * 128:(i + 1) * 128], bdn[:, half * 512:(half + 1) * 512], True, True, R=[GT, bdn], W=[pR])
                P.copy(acc[:, i, half * 512:(half + 1) * 512], pR[:, 0:512], R=[pR], W=[acc])
        for e in range(n_exp):
            wgv = C.exp_w_gu.t[l, e].rearrange("(c p) n -> p c n", p=128)
            for qd in range(4):
                sgt = stg[ist % 3]
                ist += 1
                w = wq[iq % 3]
                iq += 1
                P.dma(sgt[:], wgv[:, :, qd * 512:(qd + 1) * 512], R=[C.exp_w_gu], W=[sgt])
                for s in range(2):
                    P.copy(w[:, :, s * 256:(s + 1) * 256].rearrange("p c (t j) -> p c t j", t=2),
                           sgt[:, :, s * 256:(s + 1) * 256].rearrange("p c (j t) -> p c t j", t=2), R=[sgt], W=[w], en="act")
                for s in range(2):
                    fc = qd * 2 + s
                    pg = pG[ie % 2]; pu = pU[ie % 2]; g_ = gs[ie % 2]; u_ = us[ie % 2]; s_ = sg[ie % 2]
                    ie += 1
                    for c in range(8):
                        P.mm(pg[:, 0:n], w[:, c, s * 256:s * 256 + 128], hT16[:, c, 0:n], c == 0, c == 7, R=[w, hT16], W=[pg])
                    for c in range(8):
                        P.mm(pu[:, 0:n], w[:, c, s * 256 + 128:(s + 1) * 256], hT16[:, c, 0:n], c == 0, c == 7, R=[w, hT16], W=[pu])
                    P.ts(g_[:, 0:n], pg[:, 0:n], bgu[:, e, 0, fc:fc + 1], 7.0, ALU.add, ALU.min, R=[pg, bgu], W=[g_])
                    P.ts(u_[:, 0:n], pu[:, 0:n], bgu[:, e, 1, fc:fc + 1], 7.0, ALU.add, ALU.min, R=[pu, bgu], W=[u_])
                    P.ts(u_[:, 0:n], u_[:, 0:n], -7.0, 1.0, ALU.max, ALU.add, R=[u_], W=[u_])
                    P.act(s_[:, 0:n], g_[:, 0:n], AF.Sigmoid, R=[g_], W=[s_], scale=1.702)
                    P.tt(g_[:, 0:n], g_[:, 0:n], s_[:, 0:n], ALU.mult, R=[g_, s_], W=[g_], en="pool")
                    P.tt(actT[:, fc, 0:n], g_[:, 0:n], u_[:, 0:n], ALU.mult, R=[g_, u_], W=[actT], en="pool")
            wdv = C.exp_w_dn.t[l, e].rearrange("(c p) n -> p c n", p=128)
            for half in range(2):
                sgt = stg[ist % 3]
                ist += 1
                w = wd[idn % 2]
                idn += 1
                P.dma(sgt[:], wdv[:, :, half * 512:(half + 1) * 512], R=[C.exp_w_dn], W=[sgt])
                P.copy(w[:], sgt[:], R=[sgt], W=[w], en="act")
                for i in range(nt):
                    py = pY[(idn + i) % 2]
                    for fc in range(8):
                        P.mm(py[:, :], actT[:, fc, i * 128:(i + 1) * 128], w[:, fc, :], fc == 0, fc == 7, R=[actT, w], W=[py])
                    a = acc[:, i, half * 512:(half + 1) * 512]
                    P.stt(a, py[:, :], G[:, i, e:e + 1], a, ALU.mult, ALU.add, R=[py, G, acc], W=[acc])
        for i in range(nt):
            x = C.xt[i % 2]
            rows = slice(t0 + i * 128, t0 + (i + 1) * 128)
            P.dma(x[:], C.xres[b, rows, :], R=[], W=[x])
            P.tt(acc[:, i, :], acc[:, i, :], g2[:], ALU.mult, R=[acc, g2], W=[acc])
            P.tt(x[:], x[:], acc[:, i, :], ALU.add, R=[x, acc], W=[x])
            P.dma(C.xres[b, rows, :], x[:], R=[x], W=[], q="act")


def stage_final(P, C, b):
    with P.scope():
        xt = [P.sb("fxt%d" % i, [128, D]) for i in range(2)]
        ht = [P.sb("fht%d" % i, [128, D]) for i in range(2)]
        st = [P.sb("fst%d" % i, [128, 4]) for i in range(2)]
        g = P.sb("fg", [128, D])
        P.dma(g[:], C.final_norm_g[:].partition_broadcast(128), R=[C.final_norm_g], W=[g])
        for i in range(TL // 128):
            x = xt[i % 2]; h = ht[i % 2]; s = st[i % 2]
            P.dma(x[:], C.xres[b, TC + i * 128:TC + (i + 1) * 128, :], R=[], W=[x])
            P.act(h[:], x[:], AF.Square, R=[x], W=[h, s], accum_out=s[:, 0:1])
            P.ts(s[:, 1:2], s[:, 0:1], 1.0 / D, EPS, ALU.mult, ALU.add, R=[s], W=[s])
            P.act(s[:, 2:3], s[:, 1:2], AF.Sqrt, R=[s], W=[s])
            P.op("dve", lambda e: e.reciprocal(out=s[:, 3:4], in_=s[:, 2:3]), R=[s], W=[s])
            P.stt(h[:], x[:], s[:, 3:4], g[:], ALU.mult, ALU.mult, R=[x, s, g], W=[h])
            P.dma(C.out[b, i * 128:(i + 1) * 128, :], h[:], R=[h], W=[], q="act", is_output=True)


def build_program(debug=False):
    P = Prog(debug=debug)
    C = Ctx()
    declare_io(P, C); declare_dn(P, C); declare_attn(P, C); declare_rest(P, C)
    stage_consts(P, C)
    for l in range(DEPTH):
        ctx_out = l < DEPTH - 1
        stage_mod(P, C, l)
        stage_proj(P, C, l)
        for b in range(NB):
            stage_mla(P, C, l, b, ctx_out)
            stage_swa(P, C, l, b, ctx_out)
            stage_dn_prep(P, C, l, b)
            stage_dn_scan(P, C, l, b, ctx_out)
            stage_dn_out(P, C, l, b, ctx_out)
            stage_merge(P, C, l, b, ctx_out)
            stage_moe(P, C, l, b, ctx_out)
    for b in range(NB):
        stage_final(P, C, b)
    P.finish()
    return P, C


_CACHE = {}


def kernel(**inputs):
    I = {k: np.asarray(v) for k, v in inputs.items()}
    if "prog" not in _CACHE:
        _CACHE["prog"] = build_program()
    P, C = _CACHE["prog"]
    sh = host_shared(I)
    sh.update(host_rest(I))
    in_maps = []
    for core in range(NCORES):
        m = dict(sh)
        m.update(host_core(I, core))
        in_maps.append(m)
    res = run_bass_kernel_spmd(P.nc, in_maps, core_ids=list(range(NCORES)))
    out = np.concatenate([np.asarray(r["out"]) for r in res.results], axis=0)
    return out.astype(np.float32)
```
